# Optimizing a Trainium2 kernel written in Bass

```python
import math
import jax, jax.numpy as jnp
from jax import lax
import numpy as np

D_MODEL = 2048
BATCH = 8
SEQ = 2048
DEPTH = 4

HEAD_DIM = 64
MIX_WIDTH = D_MODEL
N_HEADS_A = (MIX_WIDTH // 2) // HEAD_DIM
N_HEADS_B = (MIX_WIDTH // 2) // HEAD_DIM
WIDTH_A = N_HEADS_A * HEAD_DIM
WIDTH_B = N_HEADS_B * HEAD_DIM
IN_WIDTH = 3 * WIDTH_A + 3 * WIDTH_B
DILATED_PATTERNS = ((128, 1), (512, 4), (2048, 16))
Q_BLOCK = 128
N_EXPERTS = 32
TOP_K = 4
D_FF = D_MODEL // 2
SWIGLU_LIMIT = 7.0
SWIGLU_ALPHA = 1.702
EXPERT_BLOCK = 256
DEEPNORM_ALPHA = (2.0 * DEPTH) ** 0.25
DEEPNORM_BETA = (8.0 * DEPTH) ** -0.25
LN_EPS = 1e-5
RMS_EPS = 1e-6

kernel_name = "hybrid_dilated_stickbreak_moe_deepnorm"


def alibi_slopes(n_heads):
    return jnp.asarray([2.0 ** (-8.0 * (h + 1) / n_heads) for h in range(n_heads)], dtype=jnp.float32)


def layer_norm(x, g, b):
    x32 = x.astype(jnp.float32)
    mu = jnp.mean(x32, axis=-1, keepdims=True)
    var = jnp.mean(jnp.square(x32 - mu), axis=-1, keepdims=True)
    y = (x32 - mu) * lax.rsqrt(var + LN_EPS)
    return (y * g.astype(jnp.float32) + b.astype(jnp.float32)).astype(x.dtype)


def rms_norm(x, g):
    x32 = x.astype(jnp.float32)
    y = x32 * lax.rsqrt(jnp.mean(jnp.square(x32), axis=-1, keepdims=True) + RMS_EPS)
    return (y * g.astype(jnp.float32)).astype(x.dtype)


def dilated_pattern(q, k, v, window, dilation, slopes):
    B, S, H, Dh = q.shape
    n_back = window // dilation
    L = S // dilation
    nblk = -(-L // Q_BLOCK)
    Lp = nblk * Q_BLOCK

    def to_sub(a):
        a = a.reshape(B, L, dilation, H, Dh)
        return jnp.pad(a, ((0, 0), (0, Lp - L), (0, 0), (0, 0), (0, 0)))

    def band(a):
        a = jnp.pad(to_sub(a), ((0, 0), (Q_BLOCK, 0), (0, 0), (0, 0), (0, 0)))
        a = a.reshape(B, nblk + 1, Q_BLOCK, dilation, H, Dh)
        return jnp.concatenate([a[:, :-1], a[:, 1:]], axis=2)

    qs = to_sub(q).reshape(B, nblk, Q_BLOCK, dilation, H, Dh)
    ks, vs = band(k), band(v)
    s = jnp.einsum('bnqchd,bnkchd->bnchqk', qs, ks,
                   preferred_element_type=jnp.float32) * (1.0 / math.sqrt(Dh))
    qi = jnp.arange(Q_BLOCK)[:, None]
    kj = jnp.arange(2 * Q_BLOCK)[None, :]
    delta = qi + Q_BLOCK - kj
    blk = jnp.arange(nblk)[:, None, None]
    valid = (delta >= 0) & (delta <= n_back) & (blk * Q_BLOCK + kj - Q_BLOCK >= 0)
    bias = -slopes[:, None, None] * (dilation * delta).astype(jnp.float32)[None]
    s = jnp.where(valid[None, :, None, None], s + bias[None, None, None], -jnp.inf)
    lse = jax.nn.logsumexp(s, axis=-1)
    p = jnp.exp(s - lse[..., None])
    o = jnp.einsum('bnchqk,bnkchd->bnqchd', p.astype(v.dtype), vs)
    o = o.reshape(B, Lp * dilation, H, Dh)[:, :S]
    lse = lse.transpose(0, 1, 4, 2, 3).reshape(B, Lp * dilation, H)[:, :S]
    return o, lse


def dilated_attention(q, k, v):
    slopes = alibi_slopes(q.shape[2])
    outs, lses = [], []
    for window, dilation in DILATED_PATTERNS:
        o, lse = dilated_pattern(q, k, v, window, dilation, slopes)
        outs.append(o)
        lses.append(lse)
    w = jax.nn.softmax(jnp.stack(lses, axis=0), axis=0)
    o = jnp.sum(w[..., None] * jnp.stack(outs, axis=0).astype(jnp.float32), axis=0)
    return o.astype(q.dtype)


def stick_breaking_attention(q, k, v):
    B, S, H, Dh = q.shape
    scale = 1.0 / math.sqrt(Dh)
    outs = []
    for n in range(S // Q_BLOCK):
        t0, t1 = n * Q_BLOCK, (n + 1) * Q_BLOCK
        z = jnp.einsum('bqhd,bkhd->bhqk', q[:, t0:t1], k[:, :t1],
                       preferred_element_type=jnp.float32) * scale
        valid = jnp.arange(t1)[None, :] < (t0 + jnp.arange(Q_BLOCK))[:, None]
        log_1mb = jnp.where(valid, jax.nn.log_sigmoid(-z), 0.0)
        log_stick = lax.cumsum(log_1mb, axis=3, reverse=True) - log_1mb
        a = jnp.where(valid, jnp.exp(jax.nn.log_sigmoid(z) + log_stick), 0.0)
        outs.append(jnp.einsum('bhqk,bkhd->bqhd', a.astype(v.dtype), v[:, :t1]))
    return jnp.concatenate(outs, axis=1)


def clamped_swiglu(h):
    gate, up = h[..., :D_FF], h[..., D_FF:]
    gate = jnp.minimum(gate, SWIGLU_LIMIT)
    up = jnp.clip(up, -SWIGLU_LIMIT, SWIGLU_LIMIT)
    return (up + 1.0) * (gate * jax.nn.sigmoid(gate * SWIGLU_ALPHA))


def moe_ffn(x, w_router, b_router, w_gate_up, b_gate_up, w_down, b_down):
    B, S, D = x.shape
    xt = x.reshape(-1, D)
    T = xt.shape[0]
    n_assign = T * TOP_K
    logits = (xt @ w_router + b_router).astype(jnp.float32)
    top_val, top_idx = lax.top_k(logits, TOP_K)
    gates = jax.nn.softmax(top_val, axis=-1).astype(x.dtype)
    e_flat = top_idx.reshape(-1)
    order = jnp.argsort(e_flat)
    e_sorted = e_flat[order]
    tok_sorted = order // TOP_K
    gate_sorted = gates.reshape(-1)[order]
    counts = jnp.bincount(e_flat, length=N_EXPERTS)
    padded = (counts + EXPERT_BLOCK - 1) // EXPERT_BLOCK * EXPERT_BLOCK
    start = jnp.cumsum(counts) - counts
    pend = jnp.cumsum(padded)
    pstart = pend - padded
    dest = pstart[e_sorted] + (jnp.arange(n_assign) - start[e_sorted])
    n_blocks = -(-n_assign // EXPERT_BLOCK) + N_EXPERTS
    block_expert = jnp.minimum(
        jnp.searchsorted(pend, jnp.arange(n_blocks) * EXPERT_BLOCK, side='right'),
        N_EXPERTS - 1).astype(jnp.int32)
    x_buf = jnp.zeros((n_blocks * EXPERT_BLOCK, D), x.dtype).at[dest].set(xt[tok_sorted])

    def expert_block(args):
        xb, e = args
        h = xb @ w_gate_up[e] + b_gate_up[e]
        return clamped_swiglu(h) @ w_down[e] + b_down[e]

    y_buf = lax.map(expert_block, (x_buf.reshape(n_blocks, EXPERT_BLOCK, D), block_expert))
    y_rows = y_buf.reshape(-1, D)[dest] * gate_sorted[:, None]
    y = jnp.zeros_like(xt).at[tok_sorted].add(y_rows)
    return y.reshape(B, S, D)


def setup_inputs(seed: int = 0) -> dict:
    key = jax.random.key(seed)
    ks = jax.random.split(key, 20)
    f32 = jnp.float32
    std_in = D_MODEL ** -0.5
    x = jax.random.normal(ks[0], (BATCH, SEQ, D_MODEL), f32)
    col_scale = jnp.concatenate([
        jnp.ones((2 * WIDTH_A,), f32), jnp.full((WIDTH_A,), DEEPNORM_BETA, f32),
        jnp.ones((2 * WIDTH_B,), f32), jnp.full((WIDTH_B,), DEEPNORM_BETA, f32)])
    w_in = jax.random.normal(ks[1], (DEPTH, D_MODEL, IN_WIDTH), f32) * std_in * col_scale
    g_mix_a = 1.0 + 0.02 * jax.random.normal(ks[2], (DEPTH, WIDTH_A), f32)
    g_mix_b = 1.0 + 0.02 * jax.random.normal(ks[3], (DEPTH, WIDTH_B), f32)
    w_out = jax.random.normal(ks[4], (DEPTH, MIX_WIDTH, D_MODEL), f32) * (MIX_WIDTH ** -0.5) * DEEPNORM_BETA
    ln1_g = 1.0 + 0.02 * jax.random.normal(ks[5], (DEPTH, D_MODEL), f32)
    ln1_b = 0.02 * jax.random.normal(ks[6], (DEPTH, D_MODEL), f32)
    w_router = jax.random.normal(ks[7], (DEPTH, D_MODEL, N_EXPERTS), f32) * std_in
    b_router = 0.01 * jax.random.normal(ks[8], (DEPTH, N_EXPERTS), f32)
    w_gate_up = jax.random.normal(ks[9], (DEPTH, N_EXPERTS, D_MODEL, 2 * D_FF), f32) * std_in
    b_gate_up = 0.02 * jax.random.normal(ks[10], (DEPTH, N_EXPERTS, 2 * D_FF), f32)
    w_down = jax.random.normal(ks[11], (DEPTH, N_EXPERTS, D_FF, D_MODEL), f32) * (D_FF ** -0.5) * DEEPNORM_BETA
    b_down = 0.02 * jax.random.normal(ks[12], (DEPTH, N_EXPERTS, D_MODEL), f32)
    ln2_g = 1.0 + 0.02 * jax.random.normal(ks[13], (DEPTH, D_MODEL), f32)
    ln2_b = 0.02 * jax.random.normal(ks[14], (DEPTH, D_MODEL), f32)
    return {"x": x, "w_in": w_in, "g_mix_a": g_mix_a, "g_mix_b": g_mix_b, "w_out": w_out,
            "ln1_g": ln1_g, "ln1_b": ln1_b, "w_router": w_router, "b_router": b_router,
            "w_gate_up": w_gate_up, "b_gate_up": b_gate_up, "w_down": w_down, "b_down": b_down,
            "ln2_g": ln2_g, "ln2_b": ln2_b}


def reference(x, w_in, g_mix_a, g_mix_b, w_out, ln1_g, ln1_b, w_router, b_router,
              w_gate_up, b_gate_up, w_down, b_down, ln2_g, ln2_b):
    B, S, _ = x.shape
    splits = np.cumsum([WIDTH_A, WIDTH_A, WIDTH_A, WIDTH_B, WIDTH_B])
    for l in range(DEPTH):
        h = x @ w_in[l]
        qa, ka, va, qb, kb, vb = jnp.split(h, splits, axis=-1)
        heads_a = lambda t: t.reshape(B, S, N_HEADS_A, HEAD_DIM)
        heads_b = lambda t: t.reshape(B, S, N_HEADS_B, HEAD_DIM)
        oa = dilated_attention(heads_a(qa), heads_a(ka), heads_a(va)).reshape(B, S, WIDTH_A)
        ob = stick_breaking_attention(heads_b(qb), heads_b(kb), heads_b(vb)).reshape(B, S, WIDTH_B)
        mix = jnp.concatenate([rms_norm(oa, g_mix_a[l]), rms_norm(ob, g_mix_b[l])], axis=-1)
        x = layer_norm(DEEPNORM_ALPHA * x + mix @ w_out[l], ln1_g[l], ln1_b[l])
        y = moe_ffn(x, w_router[l], b_router[l], w_gate_up[l], b_gate_up[l], w_down[l], b_down[l])
        x = layer_norm(DEEPNORM_ALPHA * x + y, ln2_g[l], ln2_b[l])
    return x
```

```python
import math
from contextlib import ExitStack

import numpy as np
import ml_dtypes

import concourse.bass as bass
import concourse.mybir as mybir
from concourse.bass_utils import run_bass_kernel_spmd

F32 = mybir.dt.float32
BF16 = mybir.dt.bfloat16
I32 = mybir.dt.int32
AF = mybir.ActivationFunctionType
ALU = mybir.AluOpType
AX = mybir.AxisListType

NL = 4
S = 2048
D = 2048
HD = 64
NH = 16
WA = 1024
NE = 32
CAP = 384
NSLOT = NE * CAP
DFF = 1024
ALPHA = (2.0 * NL) ** 0.25
PAT = ((128, 1), (512, 4), (2048, 16))
BIGD = 30000.0
NEGM = -30000.0
SAME_ENG_SYNC = False


class Buf:
    __slots__ = ("name", "w", "r", "dkey", "excl")

    def __init__(self, name, excl=False):
        self.name = name
        self.w = None
        self.r = {}
        self.dkey = None
        self.excl = excl


class Prog:
    def __init__(self, nc, es, n_dsem=90):
        self.nc = nc
        self.E = {"pe": nc.tensor, "act": nc.scalar, "dve": nc.vector, "pool": nc.gpsimd, "sp": nc.sync}
        self.semobj = {}
        self.cnt = {}
        for k in ["pe", "act", "dve", "pool"]:
            self.semobj[("e", k)] = es.enter_context(nc.semaphore("e_" + k))
            self.cnt[k] = 0
        self.seen = {k: {} for k in self.E}
        self.dfree = []
        self.dval = {}
        for i in range(n_dsem):
            key = ("d", i)
            self.semobj[key] = es.enter_context(nc.semaphore("d%d" % i))
            self.dval[key] = 0
            self.dfree.append(key)
        self.stage_bufs = []
        self.defer = set()

    def buf(self, name):
        b = Buf(name)
        self.stage_bufs.append(b)
        return b

    def _deps(self, eng, reads, writes):
        deps = {}
        me = ("e", eng)

        def add(k, v):
            if v > deps.get(k, 0):
                deps[k] = v

        for b in reads:
            if b.w is not None:
                add(*b.w)
        for b in writes:
            if b.w is not None:
                add(*b.w)
            for k, v in b.r.items():
                if k != me:
                    add(k, v)
        out = []
        seen = self.seen[eng]
        for k, v in deps.items():
            if k == me and eng == "pe":
                continue
            if k[0] == "e":
                assert v <= self.cnt[k[1]], ("wait on not-yet-emitted inc", eng, k, v, self.cnt[k[1]])
            if seen.get(k, 0) < v:
                seen[k] = v
                out.append((k, v))
        return out

    def _emit_waits(self, eng, waits):
        e = self.E[eng]
        for k, v in waits:
            e.wait_ge(self.semobj[k], v)

    def op(self, eng, fn, reads=(), writes=(), inc=True):
        if any(b.excl for b in reads):
            writes = list(writes) + [b for b in reads if b.excl]
            reads = [b for b in reads if not b.excl]
        self._emit_waits(eng, self._deps(eng, reads, writes))
        ins = fn(self.E[eng])
        if inc:
            self.cnt[eng] += 1
            ins.then_inc(self.semobj[("e", eng)], 1)
            tok = (("e", eng), self.cnt[eng])
        else:
            tok = (("e", eng), self.cnt[eng] + 1)
        for b in reads:
            if b.r.get(tok[0], 0) < tok[1]:
                b.r[tok[0]] = tok[1]
        for b in writes:
            b.w = tok
            b.r = {}
        return tok

    def dma(self, q, fn, reads=(), writes=()):
        self._emit_waits(q, self._deps(q, reads, writes))
        wb = writes[0]
        if wb.dkey is None:
            assert self.dfree, "out of DMA semaphores"
            wb.dkey = self.dfree.pop()
        key = wb.dkey
        ins = fn(self.E[q])
        self.dval[key] += 16
        ins.then_inc(self.semobj[key], 16)
        tok = (key, self.dval[key])
        for b in reads:
            if b.r.get(tok[0], 0) < tok[1]:
                b.r[tok[0]] = tok[1]
        for b in writes:
            b.w = tok
            b.r = {}
        return tok

    def barrier(self, engines=("pe", "act", "dve", "pool", "sp")):
        allv = {}
        for k in ["pe", "act", "dve", "pool"]:
            if self.cnt[k] > 0:
                allv[("e", k)] = self.cnt[k]
        for key, v in self.dval.items():
            if v > 0 and key not in self.defer:
                allv[key] = v
        for eng in engines:
            seen = self.seen[eng]
            for k, v in allv.items():
                if k == ("e", eng):
                    continue
                if seen.get(k, 0) < v:
                    seen[k] = v
                    self.E[eng].wait_ge(self.semobj[k], v)

    def end_stage(self):
        self.barrier()
        for b in self.stage_bufs:
            if b.dkey is not None:
                self.dfree.append(b.dkey)
                b.dkey = None
        self.stage_bufs = []


def make_consts():
    c = {}
    k = np.arange(128)[:, None]
    q = np.arange(128)[None, :]
    cur = np.where(q >= k, (q - k).astype(np.float32), BIGD)
    prv = np.where(k >= q, (q + 128 - k).astype(np.float32), BIGD)
    c["c_dm"] = np.concatenate([cur, prv], axis=1).astype(np.float32)
    c["c_identf"] = np.eye(128, dtype=np.float32)
    c["c_identb"] = np.eye(128, dtype=np.float32).astype(ml_dtypes.bfloat16)
    qq = np.arange(512)[None, :]
    neg = np.stack([np.where(128 * o + k < qq, 0.0, NEGM) for o in range(4)], axis=1)
    c["c_neg"] = neg.astype(ml_dtypes.bfloat16)
    kp = np.arange(128)[:, None]
    kk = np.arange(128)[None, :]
    c["c_negtri"] = np.where(kp >= kk, -1.0, 0.0).astype(ml_dtypes.bfloat16)
    c["c_negones"] = np.full((128, 128), -1.0).astype(ml_dtypes.bfloat16)
    c["c_ones"] = np.ones((128, 128), np.float32).astype(ml_dtypes.bfloat16)
    c["c_tstrict"] = np.where(kp < kk, 1.0, 0.0).astype(ml_dtypes.bfloat16)
    sel = np.zeros((128, 64), np.float32)
    sel[64, :] = 1.0
    c["c_sel65"] = sel
    c["c_onesf"] = np.ones((128, 8), np.float32)
    c["c_ecap"] = np.tile((np.arange(NE) * CAP).astype(np.float32)[None, :], (128, 1))
    return c


CONST_SPECS = {
    "c_dm": ([128, 256], F32), "c_identf": ([128, 128], F32), "c_identb": ([128, 128], BF16),
    "c_neg": ([128, 4, 512], BF16), "c_negtri": ([128, 128], BF16), "c_negones": ([128, 128], BF16),
    "c_ones": ([128, 128], BF16), "c_tstrict": ([128, 128], BF16), "c_sel65": ([128, 64], F32),
    "c_onesf": ([128, 8], F32), "c_ecap": ([128, NE], F32),
}


class K:
    pass


def build(n_layers=NL, stages=("attn", "oproj", "moe", "ln2"), debug=()):
    nc = bass.Bass("TRN2", target_bir_lowering=False)
    k = K()
    k.nc = nc
    k.debug = set(debug)
    dt = nc.dram_tensor
    k.x_in = dt("x", [S, D], F32, kind="ExternalInput").ap()
    k.w_in = dt("w_in", [NL, D, 6144], F32, kind="ExternalInput").ap()
    k.w_out = dt("w_out", [NL, D, D], F32, kind="ExternalInput").ap()
    k.gmix = dt("gmix", [NL, 64, 32], F32, kind="ExternalInput").ap()
    k.ln1_g = dt("ln1_g", [NL, D], F32, kind="ExternalInput").ap()
    k.ln1_b = dt("ln1_b", [NL, D], F32, kind="ExternalInput").ap()
    k.ln2_g = dt("ln2_g", [NL, D], F32, kind="ExternalInput").ap()
    k.ln2_b = dt("ln2_b", [NL, D], F32, kind="ExternalInput").ap()
    k.w_router = dt("w_router", [NL, D, NE], F32, kind="ExternalInput").ap()
    k.b_router = dt("b_router", [NL, NE], F32, kind="ExternalInput").ap()
    if "moe" in stages:
        k.w_gu = dt("w_gate_up", [NL, NE, D, 2 * DFF], F32, kind="ExternalInput").ap()
        k.w_dn = dt("w_down", [NL, NE, DFF, D], F32, kind="ExternalInput").ap()
    k.b_gu = dt("b_gu", [NL, 128, NE, 16], F32, kind="ExternalInput").ap()
    k.b_dn = dt("b_down", [NL, NE, D], F32, kind="ExternalInput").ap()
    k.cdram = {n: dt(n, shp, d, kind="ExternalInput").ap() for n, (shp, d) in CONST_SPECS.items()}
    k.out = dt("out", [S, D], F32, kind="ExternalOutput").ap()
    k.xres = dt("xres", [S, D], F32, kind="Internal").ap()
    k.x1res = dt("x1res", [S, D], F32, kind="Internal").ap()
    k.mixT = dt("mixT", [16, 128, S], BF16, kind="Internal").ap()
    k.XS = dt("XS", [NSLOT, D], BF16, kind="Internal").ap()
    k.YS = dt("YS", [NSLOT, D], F32, kind="Internal").ap()
    k.WB1 = dt("WB1", [NE * 4, 128, 16, 512], BF16, kind="Internal").ap()
    k.dbg = {}
    if "mix" in k.debug:
        k.dbg["mixT_o"] = dt("mixT_o", [16, 128, S], BF16, kind="ExternalOutput").ap()
        k.dbg["ss_o"] = dt("ss_o", [128, 32], F32, kind="ExternalOutput").ap()
    if "x1" in k.debug:
        k.dbg["x1_o"] = dt("x1_o", [S, D], F32, kind="ExternalOutput").ap()
        k.dbg["idx_o"] = dt("idx_o", [128, 64], I32, kind="ExternalOutput").ap()
        k.dbg["gk_o"] = dt("gk_o", [128, 64], F32, kind="ExternalOutput").ap()
    if "xs" in k.debug:
        k.dbg["xs_o"] = dt("xs_o", [NSLOT, D], BF16, kind="ExternalOutput").ap()
    if "ys" in k.debug:
        k.dbg["ys_o"] = dt("ys_o", [NSLOT, D], F32, kind="ExternalOutput").ap()

    with ExitStack() as es:
        p = Prog(nc, es)
        k.p = p
        sb = lambda name, shape, d: es.enter_context(nc.sbuf_tensor(name, shape, d))
        ps = lambda name, shape, d: es.enter_context(nc.psum_tensor(name, shape, d))
        k.xT = sb("xT", [128, 16, S], BF16)
        k.b_xT = Buf("xT")
        k.cs = {}
        k.b_c = Buf("consts")
        for n, (shp, d) in CONST_SPECS.items():
            k.cs[n] = sb("s_" + n, shp, d)
        k.ss = sb("ss", [128, 32], F32)
        k.b_ss = Buf("ss")
        k.idx = sb("idx", [128, 16, 4], I32)
        k.gk = sb("gk", [128, 16, 4], F32)
        k.b_route = Buf("route")
        k.b_WB = Buf("WB1")
        k.b_WB.dkey = p.dfree.pop()
        p.defer.add(k.b_WB.dkey)
        k.has_moe = "moe" in stages
        k.F = [ps("F%d" % i, [128, 512], F32) for i in range(7)]
        k.b_F = [Buf("F%d" % i, excl=True) for i in range(7)]
        k.PX = ps("PX", [128, 512], F32)
        k.PT = k.PX[:, 0:256].bitcast(BF16)
        k.b_PT = Buf("PX", excl=True)
        k.PS = k.PX[:, 256:512]
        k.b_PS = k.b_PT
        print("PT shape", k.PT.shape, "PS shape", k.PS.shape)

        k.bc_reg = nc.gpsimd.alloc_register("bc_reg")
        nc.gpsimd.reg_mov(k.bc_reg, NSLOT - 1)
        first = True
        for n in CONST_SPECS:
            src = k.cdram[n]
            dst = k.cs[n]
            if len(CONST_SPECS[n][0]) == 3:
                p.dma("sp", lambda e, d_=dst, s_=src: e.dma_start(out=d_[:, :, :], in_=s_[:, :, :]), writes=[k.b_c])
            else:
                p.dma("sp", lambda e, d_=dst, s_=src: e.dma_start(out=d_[:, :], in_=s_[:, :]), writes=[k.b_c])
        print("sbuf bytes remaining after persistent:", nc.sbuf_bytes_remaining)

        stage_prologue(k)
        xcur = k.x_in
        for l in range(n_layers):
            last = (l == n_layers - 1)
            if "attn" in stages:
                stage_attn(k, l)
            if "mix" in k.debug and l == 0:
                dump_dram(k, k.dbg["mixT_o"].rearrange("c p t -> (c p) t"), k.mixT.rearrange("c p t -> (c p) t"), 2048, BF16, 2048)
                dump_sbuf(k, k.dbg["ss_o"], k.ss, k.b_ss)
            if "oproj" in stages:
                stage_oproj(k, l, xcur)
            if "x1" in k.debug and l == 0:
                dump_dram(k, k.dbg["x1_o"], k.x1res, 2048, F32, 2048)
                dump_sbuf(k, k.dbg["idx_o"], k.idx[:, :, :].rearrange("p a b -> p (a b)"), k.b_route, raw=True)
                dump_sbuf(k, k.dbg["gk_o"], k.gk[:, :, :].rearrange("p a b -> p (a b)"), k.b_route, raw=True)
            if "xs" in k.debug and l == 0:
                dump_dram(k, k.dbg["xs_o"], k.XS, NSLOT, BF16, 2048)
            if "moe" in stages:
                stage_moe(k, l)
            if "ys" in k.debug and l == 0:
                dump_dram(k, k.dbg["ys_o"], k.YS, NSLOT, F32, 2048)
            if "ln2" in stages:
                stage_ln2(k, l, k.out if last else k.xres, last)
            xcur = k.xres
        p.barrier()
    return nc


def dump_dram(k, dst, src, rows, dtp, cols):
    p = k.p
    nc = k.nc
    p.barrier()
    k.ndump = getattr(k, "ndump", 0) + 1
    with nc.sbuf_tensor("dump_t%d" % k.ndump, [128, cols], dtp) as t:
        bt = Buf("dump_t")
        bo = Buf("dump_o")
        for r0 in range(0, rows, 128):
            p.dma("sp", lambda e, r0=r0: e.dma_start(out=t[:, :], in_=src[r0:r0 + 128, :]), writes=[bt])
            p.dma("sp", lambda e, r0=r0: e.dma_start(out=dst[r0:r0 + 128, :], in_=t[:, :]), reads=[bt], writes=[bo])
        p.barrier()
        if bt.dkey is not None:
            p.dfree.append(bt.dkey)
        if bo.dkey is not None:
            p.dfree.append(bo.dkey)


def dump_sbuf(k, dst, src, b, raw=False):
    p = k.p
    p.barrier()
    bo = Buf("dump_o2")
    if raw:
        p.dma("sp", lambda e: e.dma_start(out=dst[:, :], in_=src), reads=[b], writes=[bo])
    else:
        p.dma("sp", lambda e: e.dma_start(out=dst[:, :], in_=src[:, :]), reads=[b], writes=[bo])
    p.barrier()
    p.dfree.append(bo.dkey)


def transpose_block(k, src, b_src, tb, xT_dst=True, f32_dst=None, b_f32=None, fbanks=(0, 1), evac=("act", "dve")):
    p = k.p
    identf = k.cs["c_identf"]
    for g in range(4):
        fb = fbanks[g % len(fbanks)]
        F = k.F[fb]
        bF = k.b_F[fb]
        for i in range(4):
            c = 4 * g + i
            p.op("pe", lambda e, c=c, i=i, F=F: e.transpose(F[:, i * 128:(i + 1) * 128], src[:, c * 128:(c + 1) * 128], identf[:, :]),
                 reads=[b_src, k.b_c], writes=[bF], inc=(i == 3))
        Fv = F[:, :].rearrange("p (a t) -> p a t", a=4)
        if xT_dst:
            eng = evac[g % len(evac)]
            if eng == "act":
                p.op("act", lambda e, g=g, Fv=Fv: e.activation(out=k.xT[:, 4 * g:4 * g + 4, tb * 128:(tb + 1) * 128], in_=Fv, func=AF.Copy),
                     reads=[bF], writes=[k.b_xT])
            else:
                p.op("dve", lambda e, g=g, Fv=Fv: e.tensor_copy(k.xT[:, 4 * g:4 * g + 4, tb * 128:(tb + 1) * 128], Fv),
                     reads=[bF], writes=[k.b_xT])
        if f32_dst is not None:
            p.op("dve", lambda e, g=g, Fv=Fv: e.tensor_copy(f32_dst[:, 4 * g:4 * g + 4, :], Fv), reads=[bF], writes=[b_f32])


def stage_prologue(k):
    p = k.p
    nc = k.nc
    with ExitStack() as es:
        xt = [es.enter_context(nc.sbuf_tensor("pro_x%d" % i, [128, D], F32)) for i in range(3)]
        bx = [p.buf("pro_x%d" % i) for i in range(3)]
        zt = es.enter_context(nc.sbuf_tensor("pro_z", [128, D], BF16))
        bz = p.buf("pro_z")
        bxs = p.buf("XS_init")
        p.op("pool", lambda e: e.memset(zt[:, :], 0.0), writes=[bz])
        for r0 in range(0, NSLOT, 128):
            p.dma("pool", lambda e, r0=r0: e.dma_start(out=k.XS[r0:r0 + 128, :], in_=zt[:, :]), reads=[bz], writes=[bxs])
        for tb in range(16):
            t = xt[tb % 3]
            b = bx[tb % 3]
            p.dma("sp", lambda e, t=t, tb=tb: e.dma_start(out=t[:, :], in_=k.x_in[tb * 128:(tb + 1) * 128, :]), writes=[b])
            transpose_block(k, t, b, tb)
        p.end_stage()


def stage_attn(k, l):
    p = k.p
    nc = k.nc
    cs = k.cs
    with ExitStack() as es:
        sb = lambda name, shape, d: es.enter_context(nc.sbuf_tensor("L%d_" % l + name, shape, d))
        NSET = 2
        wsl = [[sb("a_w%d_%d" % (s, j), [128, 16, 128], BF16) for j in range(3)] for s in range(NSET)]
        b_wsl = [[p.buf("a_w%d_%d" % (s, j)) for j in range(3)] for s in range(NSET)]
        qkvT = [[sb("a_qkv%d_%d" % (s, j), [128, S], BF16) for j in range(3)] for s in range(NSET)]
        b_qkvT = [[p.buf("a_qkv%d_%d" % (s, j)) for j in range(3)] for s in range(NSET)]
        Vr = [[sb("a_vr%d_%d" % (s, r), [128, 16, 2, 65], BF16) for r in range(3)] for s in range(NSET)]
        b_Vr = [[p.buf("a_vr%d_%d" % (s, r)) for r in range(3)] for s in range(NSET)]
        acc = [sb("a_acc%d" % i, [128, S], F32) for i in range(2)]
        b_acc = [p.buf("a_acc%d" % i) for i in range(2)]
        NW = 4
        Pe = [sb("a_pe%d" % i, [128, 256], F32) for i in range(NW)]
        b_Pe = [p.buf("a_pe%d" % i) for i in range(NW)]
        Pm = [sb("a_pm%d" % i, [128, 256], BF16) for i in range(NW)]
        b_Pm = [p.buf("a_pm%d" % i) for i in range(NW)]
        Md = [sb("a_md%d" % i, [128, 256], F32) for i in range(2)]
        b_Md = [p.buf("a_md%d" % i) for i in range(2)]
        e32 = [sb("b_e%d" % i, [128, 512], F32) for i in range(2)]
        b_e32 = [p.buf("b_e%d" % i) for i in range(2)]
        spt = [sb("b_sp%d" % i, [128, 512], BF16) for i in range(3)]
        b_spt = [p.buf("b_sp%d" % i) for i in range(3)]
        ss32 = [sb("b_ss%d" % i, [128, 512], F32) for i in range(2)]
        b_ss32 = [p.buf("b_ss%d" % i) for i in range(2)]
        ssb = [sb("b_ssb%d" % i, [128, 512], BF16) for i in range(3)]
        b_ssb = [p.buf("b_ssb%d" % i) for i in range(3)]
        at = [sb("b_a%d" % i, [128, 512], BF16) for i in range(3)]
        b_at = [p.buf("b_a%d" % i) for i in range(3)]
        oaf = [sb("f_oaf%d" % i, [64, 512], F32) for i in range(2)]
        b_oaf = [p.buf("f_oaf%d" % i) for i in range(2)]
        oab = [sb("f_oab%d" % i, [64, 512], BF16) for i in range(2)]
        b_oab = [p.buf("f_oab%d" % i) for i in range(2)]
        sq = [sb("f_sq%d" % i, [64, 512], F32) for i in range(2)]
        b_sq = [p.buf("f_sq%d" % i) for i in range(2)]
        b_mixT = p.buf("mixT")
        gmt = sb("a_gmt", [64, 32], F32)
        b_gmt = p.buf("a_gmt")
        p.dma("sp", lambda e: e.dma_start(out=gmt[:, :], in_=k.gmix[l, :, :]), writes=[b_gmt])
        print("attn stage: sbuf bytes remaining:", nc.sbuf_bytes_remaining)

        for s in range(NSET):
            for r in range(3):
                p.op("pool", lambda e, s=s, r=r: e.memset(Vr[s][r][:, :, :, 64:65], 1.0), writes=[b_Vr[s][r]])
        p.op("pool", lambda e: e.memset(k.ss[:, :], 0.0), writes=[k.b_ss])

        st = {"fin": 0, "pe": 0, "pm": 0, "md": 0}

        def col0(pi, j):
            base = 0 if pi < 8 else 3 * WA
            return base + j * WA + (pi % 8) * 128

        def bg_pair(pi):
            s = pi % NSET
            for j in range(3):
                c0 = col0(pi, j)
                src = k.w_in[l].rearrange("(c p) n -> p c n", p=128)[:, :, c0:c0 + 128]
                p.dma("pool", lambda e, s=s, j=j, src=src: e.dma_start(out=wsl[s][j][:, :, :], in_=src), writes=[b_wsl[s][j]])
            if k.has_moe:
                for e_ in (2 * pi, 2 * pi + 1):
                    for s_ in range(4):
                        gu, half = s_ % 2, s_ // 2
                        c0 = gu * DFF + 512 * half
                        srcw = k.w_gu[l, e_].rearrange("(c p) n -> p c n", p=128)[:, :, c0:c0 + 512]
                        for hc in range(2):
                            p.dma("pool", lambda e, e_=e_, s_=s_, hc=hc, srcw=srcw: e.dma_start(out=k.WB1[4 * e_ + s_, :, 8 * hc:8 * hc + 8, :], in_=srcw[:, 8 * hc:8 * hc + 8, :]),
                                  writes=[k.b_WB])
            yield
            gi = 0
            for j in range(3):
                for n in range(4):
                    fb = gi % 2
                    gi += 1
                    F = k.F[fb]
                    bF = k.b_F[fb]
                    for c in range(16):
                        p.op("pe", lambda e, c=c, F=F, j=j, n=n: e.matmul(F[:, :], lhsT=wsl[s][j][:, c, :], rhs=k.xT[:, c, n * 512:(n + 1) * 512],
                                                                        start=(c == 0), stop=(c == 15)),
                             reads=[b_wsl[s][j], k.b_xT], writes=[bF], inc=(c == 15))
                        if c % 4 == 3:
                            yield
                    scale = 0.125 if j == 0 else 1.0
                    if gi % 2 == 0:
                        p.op("act", lambda e, F=F, j=j, n=n, scale=scale: e.activation(out=qkvT[s][j][:, n * 512:(n + 1) * 512], in_=F[:, :], func=AF.Copy, scale=scale),
                             reads=[bF], writes=[b_qkvT[s][j]])
                    else:
                        p.op("dve", lambda e, F=F, j=j, n=n, scale=scale: e.tensor_scalar(out=qkvT[s][j][:, n * 512:(n + 1) * 512], in0=F[:, :], scalar1=scale, scalar2=None, op0=ALU.mult),
                             reads=[bF], writes=[b_qkvT[s][j]])
                    yield
            npat = 3 if pi < 8 else 1
            vT = qkvT[s][2]
            for r in range(npat):
                d = PAT[r][1]
                nblk = S // d // 128
                vv = vT[:, :].rearrange("p (i d) -> p d i", d=d)
                for g in range(4):
                    for i in range(4):
                        B = 4 * g + i
                        c, n = B // nblk, B % nblk
                        p.op("pe", lambda e, i=i, c=c, n=n, vv=vv: e.transpose(k.PT[:, i * 128:(i + 1) * 128], vv[:, c, n * 128:(n + 1) * 128], cs["c_identb"][:, :]),
                             reads=[b_qkvT[s][2], k.b_c], writes=[k.b_PT], inc=(i == 3))
                    p.op("dve", lambda e, g=g, r=r: e.tensor_copy(Vr[s][r][:, 4 * g:4 * g + 4, :, 0:64],
                                                                 k.PT[:, :].rearrange("p (a h d) -> p a h d", a=4, h=2)),
                         reads=[k.b_PT], writes=[b_Vr[s][r]])
                    yield

        def run_all(gen):
            for _ in gen:
                pass

        def step(gen):
            if gen is not None:
                try:
                    next(gen)
                except StopIteration:
                    return None
            return gen

        def finalize(h_glob, n, src_kind, accb=None, b_accb=None, Fsrc=None, b_Fsrc=None):
            i = st["fin"] % 2
            st["fin"] += 1
            if src_kind == "A":
                F6 = k.F[6]
                p.op("pe", lambda e: e.matmul(F6[0:64, :], lhsT=cs["c_sel65"][0:65, :], rhs=accb[0:65, n * 512:(n + 1) * 512], start=True, stop=True),
                     reads=[b_accb, k.b_c], writes=[k.b_F[6]])
                p.op("dve", lambda e: e.reciprocal(out=sq[i][:, :], in_=F6[0:64, :]), reads=[k.b_F[6]], writes=[b_sq[i]])
                p.op("dve", lambda e: e.tensor_tensor(out=oaf[i][:, :], in0=accb[0:64, n * 512:(n + 1) * 512], in1=sq[i][:, :], op=ALU.mult),
                     reads=[b_accb, b_sq[i]], writes=[b_oaf[i]])
            else:
                p.op("dve", lambda e: e.tensor_copy(oaf[i][:, :], Fsrc[0:64, :]), reads=[b_Fsrc], writes=[b_oaf[i]])
            p.op("dve", lambda e: e.tensor_scalar(out=oab[i][:, :], in0=oaf[i][:, :], scalar1=gmt[:, h_glob:h_glob + 1], scalar2=None, op0=ALU.mult),
                 reads=[b_oaf[i], b_gmt], writes=[b_oab[i]])
            p.op("act", lambda e: e.activation(out=sq[i][:, :], in_=oaf[i][:, :], func=AF.Square), reads=[b_oaf[i]], writes=[b_sq[i]])
            for tb in range(4):
                p.op("pe", lambda e, tb=tb: e.matmul(k.PS[:, tb:tb + 1], lhsT=sq[i][:, tb * 128:(tb + 1) * 128], rhs=cs["c_onesf"][0:64, 0:1], start=True, stop=True),
                     reads=[b_sq[i], k.b_c], writes=[k.b_PS], inc=(tb == 3))
            off = 0 if h_glob < 16 else 16
            p.op("dve", lambda e: e.tensor_tensor(out=k.ss[:, off + 4 * n:off + 4 * n + 4], in0=k.ss[:, off + 4 * n:off + 4 * n + 4], in1=k.PS[:, 0:4], op=ALU.add),
                 reads=[k.b_PS, k.b_ss], writes=[k.b_ss])
            ch, ph = h_glob // 2, (h_glob % 2) * 64
            p.dma("sp", lambda e: e.dma_start(out=k.mixT[ch, ph:ph + 64, n * 512:(n + 1) * 512], in_=oab[i][:, :]), reads=[b_oab[i]], writes=[b_mixT])

        def attn_A(pi, bg):
            s = pi % NSET
            qT, kT = qkvT[s][0], qkvT[s][1]
            bq, bk = b_qkvT[s][0], b_qkvT[s][1]
            for hh in range(2):
                h = 2 * pi + hh
                slope = 2.0 ** (-8.0 * (h + 1) / NH)
                accb, b_accb = acc[hh], b_acc[hh]
                r0, r1 = hh * 64, hh * 64 + 64
                for r in range(3):
                    d = PAT[r][1]
                    nblk = S // d // 128
                    mi = st["md"] % 2
                    st["md"] += 1
                    p.op("act", lambda e, mi=mi, d=d: e.activation(out=Md[mi][:, :], in_=cs["c_dm"][:, :], func=AF.Exp, scale=-slope * d),
                         reads=[k.b_c], writes=[b_Md[mi]])
                    qv = qT[r0:r1, :].rearrange("p (i d) -> p d i", d=d)
                    kv = kT[r0:r1, :].rearrange("p (i d) -> p d i", d=d)
                    av = accb[0:65, :].rearrange("p (i d) -> p d i", d=d)
                    tiles = [(B // nblk, B % nblk) for B in range(16)]
                    pendq = []
                    for t in range(18):
                        cur = None
                        if t < 16:
                            c, m = tiles[t]
                            ncols = 256 if m + 1 < nblk else 128
                            sbk = 2 + (t % 2)
                            FS = k.F[sbk]
                            p.op("pe", lambda e, c=c, m=m, ncols=ncols, FS=FS: e.matmul(FS[:, 0:ncols], lhsT=kv[:, c, m * 128:(m + 1) * 128],
                                                                                      rhs=qv[:, c, m * 128:m * 128 + ncols], start=True, stop=True),
                                 reads=[bq, bk], writes=[k.b_F[sbk]])
                            wi = st["pe"] % NW
                            st["pe"] += 1
                            p.op("act", lambda e, wi=wi, ncols=ncols, FS=FS: e.activation(out=Pe[wi][:, 0:ncols], in_=FS[:, 0:ncols], func=AF.Exp),
                                 reads=[k.b_F[sbk]], writes=[b_Pe[wi]])
                            p.op("dve", lambda e, wi=wi, ncols=ncols, mi=mi: e.tensor_tensor(out=Pm[wi][:, 0:ncols], in0=Pe[wi][:, 0:ncols], in1=Md[mi][:, 0:ncols], op=ALU.mult),
                                 reads=[b_Pe[wi], b_Md[mi]], writes=[b_Pm[wi]])
                            cur = (t, c, m, ncols, wi)
                        if cur is not None:
                            pendq.append(cur)
                        if t >= 2 or t >= 16:
                            tt, c, m, ncols, wi = pendq.pop(0)
                            B = tt
                            g = B // 4
                            ob = 4 + (g % 2)
                            FO = k.F[ob]
                            lhs = Vr[s][r][:, B, hh, :]
                            p.op("pe", lambda e, FO=FO, lhs=lhs, wi=wi, B=B, m=m: e.matmul(FO[0:65, (B % 4) * 128:(B % 4) * 128 + 128], lhsT=lhs, rhs=Pm[wi][:, 0:128],
                                                                                       start=(m == 0), stop=True),
                                 reads=[b_Vr[s][r], b_Pm[wi]], writes=[k.b_F[ob]], inc=True)
                            if ncols == 256:
                                B2 = B + 1
                                ob2 = 4 + ((B2 // 4) % 2)
                                FO2 = k.F[ob2]
                                p.op("pe", lambda e, FO2=FO2, lhs=lhs, wi=wi, B2=B2: e.matmul(FO2[0:65, (B2 % 4) * 128:(B2 % 4) * 128 + 128], lhsT=lhs, rhs=Pm[wi][:, 128:256],
                                                                                          start=True, stop=False),
                                     reads=[b_Vr[s][r], b_Pm[wi]], writes=[k.b_F[ob2]], inc=True)
                            if B % 4 == 3:
                                if d == 1:
                                    dst = av[:, 0, g * 512:(g + 1) * 512]
                                    srcv = FO[0:65, :]
                                elif d == 4:
                                    dst = av[:, g, 0:512]
                                    srcv = FO[0:65, :]
                                else:
                                    dst = av[:, 4 * g:4 * g + 4, 0:128]
                                    srcv = FO[0:65, :].rearrange("p (a t) -> p a t", a=4)
                                if r == 0:
                                    p.op("dve", lambda e, dst=dst, srcv=srcv: e.tensor_copy(dst, srcv), reads=[k.b_F[ob]], writes=[b_accb])
                                else:
                                    p.op("dve", lambda e, dst=dst, srcv=srcv: e.tensor_tensor(out=dst, in0=dst, in1=srcv, op=ALU.add), reads=[k.b_F[ob], b_accb], writes=[b_accb])
                            bg = step(bg)
                    assert not pendq
                for n in range(4):
                    finalize(h, n, "A", accb=accb, b_accb=b_accb)
                    bg = step(bg)
            return bg

        def attn_B(pi, bg):
            s = pi % NSET
            qT, kT = qkvT[s][0], qkvT[s][1]
            bq, bk = b_qkvT[s][0], b_qkvT[s][1]
            V0, bV0 = Vr[s][0], b_Vr[s][0]
            tiles = []
            for hh in range(2):
                for m in range(4):
                    for j in range(4 * m + 3, -1, -1):
                        tiles.append((hh, m, j))
            N = len(tiles)
            info = {}

            def PEz(t):
                hh, m, j = tiles[t]
                r0, r1 = hh * 64, hh * 64 + 64
                zb = 2 + (t % 4)
                diag = j >= 4 * m
                p.op("pe", lambda e: e.matmul(k.F[zb][:, :], lhsT=kT[r0:r1, j * 128:(j + 1) * 128], rhs=qT[r0:r1, m * 512:(m + 1) * 512], start=True, stop=False),
                     reads=[bq, bk], writes=[k.b_F[zb]], inc=not diag)
                if diag:
                    p.op("pe", lambda e: e.matmul(k.F[zb][:, :], lhsT=cs["c_identb"][:, :], rhs=cs["c_neg"][:, j - 4 * m, :], start=False, stop=False),
                         reads=[k.b_c], writes=[k.b_F[zb]])

            def ACTe(t):
                zb = 2 + (t % 4)
                ei = t % 2
                p.op("act", lambda e: e.activation(out=e32[ei][:, :], in_=k.F[zb][:, :], func=AF.Exp), reads=[k.b_F[zb]], writes=[b_e32[ei]])

            def ACTsp(t):
                ei = t % 2
                si = t % 3
                p.op("act", lambda e: e.activation(out=spt[si][:, :], in_=e32[ei][:, :], func=AF.Ln, bias=1.0), reads=[b_e32[ei]], writes=[b_spt[si]])

            def SSupd(t):
                hh, m, j = tiles[t]
                if j == 0:
                    return
                first = (j == 4 * m + 3)
                gidx = (hh * 4 + m) % 2
                si = t % 3
                if first:
                    p.op("dve", lambda e: e.tensor_copy(ss32[gidx][:, :], spt[si][:, :]), reads=[b_spt[si]], writes=[b_ss32[gidx]])
                else:
                    p.op("dve", lambda e: e.tensor_tensor(out=ss32[gidx][:, :], in0=ss32[gidx][:, :], in1=spt[si][:, :], op=ALU.add),
                         reads=[b_spt[si], b_ss32[gidx]], writes=[b_ss32[gidx]])
                p.op("dve", lambda e: e.tensor_copy(ssb[si][:, :], ss32[gidx][:, :]), reads=[b_ss32[gidx]], writes=[b_ssb[si]])

            def PEB(t):
                hh, m, j = tiles[t]
                bb = 2 + (t % 4)
                si = t % 3
                first = (j == 4 * m + 3)
                FB = k.F[bb]
                p.op("pe", lambda e: e.matmul(FB[:, :], lhsT=cs["c_negtri"][:, :], rhs=spt[si][:, :], start=False, stop=first),
                     reads=[k.b_c, b_spt[si]], writes=[k.b_F[bb]], inc=first)
                if not first:
                    sp_prev = (t - 1) % 3
                    p.op("pe", lambda e: e.matmul(FB[:, :], lhsT=cs["c_negones"][:, :], rhs=ssb[sp_prev][:, :], start=False, stop=True),
                         reads=[k.b_c, b_ssb[sp_prev]], writes=[k.b_F[bb]], inc=True)

            def ACTa(t):
                bb = 2 + (t % 4)
                ai = t % 3
                p.op("act", lambda e: e.activation(out=at[ai][:, :], in_=k.F[bb][:, :], func=AF.Exp), reads=[k.b_F[bb]], writes=[b_at[ai]])

            def PEav(t):
                hh, m, j = tiles[t]
                ai = t % 3
                first = (j == 4 * m + 3)
                p.op("pe", lambda e: e.matmul(k.F[6][0:64, :], lhsT=V0[:, j, hh, 0:64], rhs=at[ai][:, :], start=first, stop=(j == 0)),
                     reads=[bV0, b_at[ai]], writes=[k.b_F[6]], inc=True)
                if j == 0:
                    finalize(16 + 2 * (pi - 8) + hh, m, "B", Fsrc=k.F[6], b_Fsrc=k.b_F[6])

            for t in range(-1, N + 1):
                if t + 1 < N:
                    PEz(t + 1)
                    ACTe(t + 1)
                if 0 <= t < N:
                    PEB(t)
                if t + 1 < N:
                    ACTsp(t + 1)
                    SSupd(t + 1)
                if 0 <= t < N:
                    ACTa(t)
                if t >= 1:
                    PEav(t - 1)
                    bg = step(bg)
            return bg

        import os
        lim = int(os.environ.get("ATTN_LIMIT", "99"))
        bg = bg_pair(0)
        run_all(bg)
        for pi in range(16):
            if lim == 0 or (lim == 1 and pi >= 1) or (lim == 2 and pi != 8):
                continue
            if lim == 2:
                run_all(bg_pair(8))
            nxt = bg_pair(pi + 1) if pi + 1 < 16 else None
            if pi < 8:
                nxt = attn_A(pi, nxt)
            else:
                nxt = attn_B(pi, nxt)
            if nxt is not None:
                run_all(nxt)
        p.end_stage()


def ln_block(k, sbufs, u, b_u, dst, b_dst, lng, lnb, b_ln):
    p = k.p
    st, b_st = sbufs["st"], sbufs["b_st"]
    p.op("dve", lambda e: e.reduce_sum(out=st[:, 0:1], in_=u[:, :], axis=AX.X), reads=[b_u], writes=[b_st])
    p.op("dve", lambda e: e.memset(st[:, 1:2], 0.0), writes=[b_st])
    p.op("act", lambda e: e.activation(out=dst[:, :], in_=u[:, :], func=AF.Square, accum_out=st[:, 1:2]), reads=[b_u], writes=[b_dst, b_st])
    p.op("dve", lambda e: e.tensor_scalar(out=st[:, 2:3], in0=st[:, 0:1], scalar1=1.0 / D, scalar2=None, op0=ALU.mult), reads=[b_st], writes=[b_st])
    p.op("dve", lambda e: e.tensor_tensor(out=st[:, 3:4], in0=st[:, 2:3], in1=st[:, 2:3], op=ALU.mult), reads=[b_st], writes=[b_st])
    p.op("dve", lambda e: e.scalar_tensor_tensor(out=st[:, 4:5], in0=st[:, 1:2], scalar=1.0 / D, in1=st[:, 3:4], op0=ALU.mult, op1=ALU.subtract),
         reads=[b_st], writes=[b_st])
    p.op("dve", lambda e: e.tensor_scalar(out=st[:, 4:5], in0=st[:, 4:5], scalar1=1e-5, scalar2=None, op0=ALU.add), reads=[b_st], writes=[b_st])
    p.op("act", lambda e: e.activation(out=st[:, 5:6], in_=st[:, 4:5], func=AF.Sqrt), reads=[b_st], writes=[b_st])
    p.op("dve", lambda e: e.reciprocal(out=st[:, 6:7], in_=st[:, 5:6]), reads=[b_st], writes=[b_st])
    p.op("dve", lambda e: e.tensor_scalar(out=dst[:, :], in0=u[:, :], scalar1=st[:, 2:3], scalar2=st[:, 6:7], op0=ALU.subtract, op1=ALU.mult),
         reads=[b_u, b_st], writes=[b_dst])
    p.op("dve", lambda e: e.tensor_tensor(out=dst[:, :], in0=dst[:, :], in1=lng[:, :], op=ALU.mult), reads=[b_dst, b_ln], writes=[b_dst])
    p.op("dve", lambda e: e.tensor_tensor(out=dst[:, :], in0=dst[:, :], in1=lnb[:, :], op=ALU.add), reads=[b_dst, b_ln], writes=[b_dst])


def stage_oproj(k, l, xcur):
    p = k.p
    nc = k.nc
    cs = k.cs
    with ExitStack() as es:
        sb = lambda name, shape, d: es.enter_context(nc.sbuf_tensor("L%d_" % l + name, shape, d))
        wout = k.xT
        b_wout = k.b_xT
        gm = sb("o_gm", [128, 16], F32)
        lng = sb("o_lng", [128, D], F32)
        lnb = sb("o_lnb", [128, D], F32)
        wr = sb("o_wr", [128, 16, NE], F32)
        brt = sb("o_brt", [128, NE], F32)
        b_par = p.buf("o_par")
        mixt = [sb("o_mixt%d" % i, [128, 16, 128], BF16) for i in range(2)]
        b_mixt = [p.buf("o_mixt%d" % i) for i in range(2)]
        xr = [sb("o_xr%d" % i, [128, D], F32) for i in range(2)]
        b_xr = [p.buf("o_xr%d" % i) for i in range(2)]
        u = sb("o_u", [128, D], F32)
        b_u = p.buf("o_u")
        x1f = [sb("o_x1f%d" % i, [128, D], F32) for i in range(2)]
        b_x1f = [p.buf("o_x1f%d" % i) for i in range(2)]
        x1b = [sb("o_x1b%d" % i, [128, D], BF16) for i in range(2)]
        b_x1b = [p.buf("o_x1b%d" % i) for i in range(2)]
        x1T = sb("o_x1T", [128, 16, 128], F32)
        b_x1T = p.buf("o_x1T")
        selb = sb("o_selb", [128, 16, NE], BF16)
        b_selb = p.buf("o_selb")
        st = sb("o_st", [128, 8], F32)
        rr = sb("o_rr", [128, 4], F32)
        b_rr = p.buf("o_rr")
        sm = {n: sb("o_" + n, [128, NE], F32) for n in ["lg", "sel", "ex", "G", "oh", "tmp", "pos"]}
        b_sm = p.buf("o_sm")
        mx8 = sb("o_mx8", [128, 8], F32)
        pk = sb("o_pk", [128, 8], F32)
        b_x1res = p.buf("x1res")
        b_XS = p.buf("XS")
        b_mixT = p.buf("mixT_r")
        lnsb = {"st": st, "b_st": p.buf("o_st")}
        print("oproj stage: sbuf bytes remaining:", nc.sbuf_bytes_remaining)

        p.dma("sp", lambda e: e.dma_start(out=lng[:, :], in_=k.ln1_g[l:l + 1, :].broadcast_to([128, D])), writes=[b_par])
        p.dma("sp", lambda e: e.dma_start(out=lnb[:, :], in_=k.ln1_b[l:l + 1, :].broadcast_to([128, D])), writes=[b_par])
        p.dma("sp", lambda e: e.dma_start(out=wr[:, :, :], in_=k.w_router[l].rearrange("(c p) n -> p c n", p=128)), writes=[b_par])
        p.dma("sp", lambda e: e.dma_start(out=brt[:, :], in_=k.b_router[l:l + 1, :].broadcast_to([128, NE])), writes=[b_par])
        for c in range(16):
            p.dma("pool", lambda e, c=c: e.dma_start(out=wout[:, c, :], in_=k.w_out[l, c * 128:(c + 1) * 128, :]), writes=[b_wout])

        ssv = k.ss[:, :].rearrange("p (a b) -> p a b", a=2)
        for tb in range(16):
            i2 = tb % 2
            t0, t1 = tb * 128, (tb + 1) * 128
            p.dma("sp", lambda e: e.dma_start(out=mixt[i2][:, :, :], in_=k.mixT[:, :, t0:t1].rearrange("c p t -> p c t")), reads=[b_mixT], writes=[b_mixt[i2]])
            p.dma("sp", lambda e: e.dma_start(out=xr[i2][:, :], in_=xcur[t0:t1, :]), writes=[b_xr[i2]])
            p.op("dve", lambda e: e.tensor_scalar(out=rr[:, 0:2], in0=ssv[:, :, tb], scalar1=1.0 / WA, scalar2=1e-6, op0=ALU.mult, op1=ALU.add),
                 reads=[k.b_ss], writes=[b_rr])
            p.op("act", lambda e: e.activation(out=rr[:, 0:2], in_=rr[:, 0:2], func=AF.Sqrt), writes=[b_rr])
            p.op("dve", lambda e: e.reciprocal(out=rr[:, 2:4], in_=rr[:, 0:2]), writes=[b_rr])
            p.op("act", lambda e: e.activation(out=u[:, :], in_=xr[i2][:, :], func=AF.Copy, scale=ALPHA), reads=[b_xr[i2]], writes=[b_u])
            for part in range(2):
                for nb in range(4):
                    for c in range(8):
                        cc = 8 * part + c
                        p.op("pe", lambda e, nb=nb, cc=cc, c=c: e.matmul(k.F[nb][:, :], lhsT=mixt[i2][:, cc, :], rhs=wout[:, cc, nb * 512:(nb + 1) * 512],
                                                                      start=(c == 0), stop=(c == 7)),
                             reads=[b_mixt[i2], b_wout], writes=[k.b_F[nb]], inc=(c == 7))
                for nb in range(4):
                    p.op("dve", lambda e, nb=nb, part=part: e.scalar_tensor_tensor(out=u[:, nb * 512:(nb + 1) * 512], in0=k.F[nb][:, :], scalar=rr[:, 2 + part:3 + part],
                                                                                 in1=u[:, nb * 512:(nb + 1) * 512], op0=ALU.mult, op1=ALU.add),
                         reads=[k.b_F[nb], b_rr], writes=[b_u])
            xf, b_xf = x1f[i2], b_x1f[i2]
            ln_block(k, lnsb, u, b_u, xf, b_xf, lng, lnb, b_par)
            p.op("act", lambda e: e.activation(out=x1b[i2][:, :], in_=xf[:, :], func=AF.Copy), reads=[b_xf], writes=[b_x1b[i2]])
            p.dma("sp", lambda e: e.dma_start(out=k.x1res[t0:t1, :], in_=xf[:, :]), reads=[b_xf], writes=[b_x1res])
            transpose_block(k, xf, b_xf, tb, xT_dst=False, f32_dst=x1T, b_f32=b_x1T, fbanks=(4, 5))
            for c in range(16):
                p.op("pe", lambda e, c=c: e.matmul(k.F[6][:, 0:NE], lhsT=x1T[:, c, :], rhs=wr[:, c, :], start=(c == 0), stop=(c == 15)),
                     reads=[b_x1T, b_par], writes=[k.b_F[6]], inc=(c == 15))
            lg, sel, ex, G, oh, tmp, pos = [sm[n] for n in ["lg", "sel", "ex", "G", "oh", "tmp", "pos"]]
            W = [b_sm]
            p.op("dve", lambda e: e.tensor_tensor(out=lg[:, :], in0=k.F[6][:, 0:NE], in1=brt[:, :], op=ALU.add), reads=[k.b_F[6], b_par], writes=W)
            p.op("dve", lambda e: e.max(out=mx8[:, :], in_=lg[:, :]), writes=W)
            p.op("dve", lambda e: e.tensor_scalar(out=sel[:, :], in0=lg[:, :], scalar1=mx8[:, 3:4], scalar2=None, op0=ALU.is_ge), writes=W)
            p.op("dve", lambda e: e.tensor_copy(selb[:, tb, :], sel[:, :]), reads=W, writes=[b_selb])
            p.op("dve", lambda e: e.tensor_scalar(out=pk[:, 4:5], in0=mx8[:, 0:1], scalar1=-1.0, scalar2=None, op0=ALU.mult), writes=W)
            p.op("act", lambda e: e.activation(out=ex[:, :], in_=lg[:, :], func=AF.Exp, bias=pk[:, 4:5]), writes=W)
            p.op("dve", lambda e: e.tensor_tensor(out=ex[:, :], in0=ex[:, :], in1=sel[:, :], op=ALU.mult), writes=W)
            p.op("dve", lambda e: e.reduce_sum(out=pk[:, 5:6], in_=ex[:, :], axis=AX.X), writes=W)
            p.op("dve", lambda e: e.reciprocal(out=pk[:, 6:7], in_=pk[:, 5:6]), writes=W)
            p.op("dve", lambda e: e.tensor_scalar(out=G[:, :], in0=ex[:, :], scalar1=pk[:, 6:7], scalar2=None, op0=ALU.mult), writes=W)
            for b2 in range(tb + 1):
                lhs = cs["c_ones"] if b2 < tb else cs["c_tstrict"]
                p.op("pe", lambda e, b2=b2, lhs=lhs: e.matmul(k.PS[:, 0:NE], lhsT=lhs[:, :], rhs=selb[:, b2, :], start=(b2 == 0), stop=(b2 == tb)),
                     reads=[b_selb, k.b_c], writes=[k.b_PS], inc=(b2 == tb))
            p.op("dve", lambda e: e.tensor_scalar(out=tmp[:, :], in0=k.PS[:, 0:NE], scalar1=float(CAP), scalar2=1.0e6, op0=ALU.is_ge, op1=ALU.mult),
                 reads=[k.b_PS], writes=W)
            p.op("dve", lambda e: e.tensor_tensor(out=pos[:, :], in0=k.PS[:, 0:NE], in1=cs["c_ecap"][:, :], op=ALU.add), reads=[k.b_PS, k.b_c], writes=W)
            p.op("dve", lambda e: e.tensor_tensor(out=pos[:, :], in0=pos[:, :], in1=tmp[:, :], op=ALU.add), writes=W)
            for kk in range(4):
                p.op("dve", lambda e, kk=kk: e.tensor_scalar(out=oh[:, :], in0=lg[:, :], scalar1=mx8[:, kk:kk + 1], scalar2=None, op0=ALU.is_equal), writes=W)
                p.op("dve", lambda e: e.tensor_tensor(out=tmp[:, :], in0=oh[:, :], in1=G[:, :], op=ALU.mult), writes=W)
                p.op("dve", lambda e, kk=kk: e.reduce_sum(out=k.gk[:, tb, kk:kk + 1], in_=tmp[:, :], axis=AX.X), reads=W, writes=[k.b_route])
                p.op("dve", lambda e: e.tensor_tensor(out=tmp[:, :], in0=oh[:, :], in1=pos[:, :], op=ALU.mult), writes=W)
                p.op("dve", lambda e, kk=kk: e.reduce_sum(out=pk[:, kk:kk + 1], in_=tmp[:, :], axis=AX.X), writes=W)
            p.op("dve", lambda e: e.tensor_copy(k.idx[:, tb, :], pk[:, 0:4]), reads=W, writes=[k.b_route])
            for kk in range(4):
                p.dma("pool", lambda e, kk=kk: e.indirect_dma_start(out=k.XS[:, :], out_offset=bass.IndirectOffsetOnAxis(ap=k.idx[:, tb, kk:kk + 1], axis=0),
                                                                   in_=x1b[i2][:, :], in_offset=None, bounds_check=k.bc_reg, oob_is_err=False),
                      reads=[b_x1b[i2], k.b_route], writes=[b_XS])
        p.end_stage()


def stage_moe(k, l):
    p = k.p
    nc = k.nc
    cs = k.cs
    NS = CAP
    with ExitStack() as es:
        sb = lambda name, shape, d: es.enter_context(nc.sbuf_tensor("L%d_" % l + name, shape, d))
        NW1 = 4
        w1 = [sb("m_w1_%d" % i, [128, 16, 512], BF16) for i in range(NW1)]
        b_w1 = [p.buf("m_w1_%d" % i) for i in range(NW1)]
        w2 = [k.xT[:, 0:8, :], k.xT[:, 8:16, :]]
        b_w2 = [p.buf("m_w2_0"), p.buf("m_w2_1")]
        xs = sb("m_xs", [128, 3, D], BF16)
        b_xs = p.buf("m_xs")
        xeT = sb("m_xeT", [128, 16, NS], BF16)
        b_xeT = p.buf("m_xeT")
        hT = [sb("m_hT%d" % i, [128, 8, NS], BF16) for i in range(2)]
        b_hT = [p.buf("m_hT%d" % i) for i in range(2)]
        NT = 2
        gc = [sb("m_gc%d" % i, [128, NS], F32) for i in range(NT)]
        sg = [sb("m_sg%d" % i, [128, NS], F32) for i in range(NT)]
        uc = [sb("m_uc%d" % i, [128, NS], F32) for i in range(NT)]
        b_tmp = [p.buf("m_tmp%d" % i) for i in range(NT)]
        NY = 4
        yo = [sb("m_yo%d" % i, [128, 512], F32) for i in range(NY)]
        b_yo = [p.buf("m_yo%d" % i) for i in range(NY)]
        b1 = sb("m_b1", [128, NE, 16], F32)
        b_b1 = p.buf("m_b1")
        b2 = [sb("m_b2_%d" % i, [128, D], F32) for i in range(2)]
        b_b2 = [p.buf("m_b2_%d" % i) for i in range(2)]
        b_XS = p.buf("XS_r")
        b_YS = p.buf("YS")
        print("moe stage: sbuf bytes remaining:", nc.sbuf_bytes_remaining)

        p.dma("sp", lambda e: e.dma_start(out=b1[:, :, :], in_=k.b_gu[l, :, :, :]), writes=[b_b1])

        def load_slab(g_):
            wi = g_ % NW1
            for hc in range(2):
                p.dma("act", lambda e, wi=wi, hc=hc, g_=g_: e.dma_start(out=w1[wi][:, 8 * hc:8 * hc + 8, :], in_=k.WB1[g_, :, 8 * hc:8 * hc + 8, :]),
                      reads=[k.b_WB], writes=[b_w1[wi]])

        def load_w2(e_):
            wj = e_ % 2
            for hf in range(2):
                src = k.w_dn[l, e_].rearrange("(c p) n -> p c n", p=128)[:, 4 * hf:4 * hf + 4, :]
                p.dma("pool", lambda e, wj=wj, hf=hf, src=src: e.dma_start(out=w2[wj][:, 4 * hf:4 * hf + 4, :], in_=src), writes=[b_w2[wj]])

        def load_acts(e_):
            p.dma("sp", lambda e: e.dma_start(out=xs[:, :, :], in_=k.XS[e_ * CAP:(e_ + 1) * CAP, :].rearrange("(j p) f -> p j f", p=128)),
                  reads=[b_XS], writes=[b_xs])
            p.dma("sp", lambda e: e.dma_start(out=b2[e_ % 2][:, :], in_=k.b_dn[l, e_:e_ + 1, :].broadcast_to([128, D])), writes=[b_b2[e_ % 2]])

        st = {"ev": 0, "yo": 0, "tmp": 0, "dn": 0}
        st["slab"] = 0

        def ensure_slabs(upto):
            while st["slab"] <= min(upto, 4 * NE - 1):
                load_slab(st["slab"])
                st["slab"] += 1

        ensure_slabs(3)
        load_w2(0)
        load_acts(0)
        for e_ in range(NE):
            for j in range(3):
                for cg in range(4):
                    for i in range(4):
                        c = 4 * cg + i
                        p.op("pe", lambda e, j=j, c=c, i=i: e.transpose(k.PT[:, i * 128:(i + 1) * 128], xs[:, j, c * 128:(c + 1) * 128], cs["c_identb"][:, :]),
                             reads=[b_xs, k.b_c], writes=[k.b_PT], inc=(i == 3))
                    src = k.PT[:, :].rearrange("p (a t) -> p a t", a=4)
                    dst = xeT[:, 4 * cg:4 * cg + 4, j * 128:(j + 1) * 128]
                    if st["ev"] % 2 == 0:
                        p.op("act", lambda e, src=src, dst=dst: e.activation(out=dst, in_=src, func=AF.Copy), reads=[k.b_PT], writes=[b_xeT])
                    else:
                        p.op("dve", lambda e, src=src, dst=dst: e.tensor_copy(dst, src), reads=[k.b_PT], writes=[b_xeT])
                    st["ev"] += 1
            if e_ + 1 < NE:
                load_acts(e_ + 1)
            hb = e_ % 2
            for jj in range(8):
                half = jj // 4
                if jj == 0:
                    ensure_slabs(4 * e_ + 3)
                    if e_ + 1 < NE:
                        load_w2(e_ + 1)
                elif jj == 4:
                    ensure_slabs(4 * e_ + 5)
                co = (jj % 4) * 128
                Gb, Ub = jj % 2, 2 + jj % 2
                for gu, bank in ((0, Gb), (1, Ub)):
                    wi = (4 * e_ + 2 * half + gu) % NW1
                    for c in range(16):
                        p.op("pe", lambda e, c=c, wi=wi, bank=bank: e.matmul(k.F[bank][:, 0:NS], lhsT=w1[wi][:, c, co:co + 128], rhs=xeT[:, c, :],
                                                                         start=(c == 0), stop=(c == 15)),
                             reads=[b_w1[wi], b_xeT], writes=[k.b_F[bank]], inc=(c == 15))
                ti = st["tmp"] % NT
                st["tmp"] += 1
                T = [b_tmp[ti]]
                p.op("dve", lambda e: e.tensor_scalar(out=gc[ti][:, :], in0=k.F[Gb][:, 0:NS], scalar1=b1[:, e_, jj:jj + 1], scalar2=7.0, op0=ALU.add, op1=ALU.min),
                     reads=[k.b_F[Gb], b_b1], writes=T)
                p.op("act", lambda e: e.activation(out=sg[ti][:, :], in_=gc[ti][:, :], func=AF.Sigmoid, scale=1.702), writes=T)
                p.op("dve", lambda e: e.tensor_scalar(out=uc[ti][:, :], in0=k.F[Ub][:, 0:NS], scalar1=b1[:, e_, 8 + jj:9 + jj], scalar2=7.0, op0=ALU.add, op1=ALU.min),
                     reads=[k.b_F[Ub], b_b1], writes=T)
                p.op("dve", lambda e: e.tensor_scalar(out=uc[ti][:, :], in0=uc[ti][:, :], scalar1=-7.0, scalar2=1.0, op0=ALU.max, op1=ALU.add), writes=T)
                p.op("dve", lambda e: e.tensor_tensor(out=gc[ti][:, :], in0=gc[ti][:, :], in1=sg[ti][:, :], op=ALU.mult), writes=T)
                p.op("dve", lambda e: e.tensor_tensor(out=hT[hb][:, jj, :], in0=gc[ti][:, :], in1=uc[ti][:, :], op=ALU.mult), reads=T, writes=[b_hT[hb]])
            wj = e_ % 2
            for j in range(3):
                for q in range(4):
                    bank = 4 + st["dn"] % 3
                    st["dn"] += 1
                    for jj in range(8):
                        p.op("pe", lambda e, jj=jj, j=j, q=q, bank=bank: e.matmul(k.F[bank][:, :], lhsT=hT[hb][:, jj, j * 128:(j + 1) * 128], rhs=w2[wj][:, jj, q * 512:(q + 1) * 512],
                                                                               start=(jj == 0), stop=(jj == 7)),
                             reads=[b_hT[hb], b_w2[wj]], writes=[k.b_F[bank]], inc=(jj == 7))
                    yi = st["yo"] % NY
                    st["yo"] += 1
                    p.op("dve", lambda e, q=q, bank=bank, yi=yi: e.tensor_tensor(out=yo[yi][:, :], in0=k.F[bank][:, :], in1=b2[wj][:, q * 512:(q + 1) * 512], op=ALU.add),
                         reads=[k.b_F[bank], b_b2[wj]], writes=[b_yo[yi]])
                    r0 = e_ * CAP + j * 128
                    p.dma("sp", lambda e, q=q, yi=yi, r0=r0: e.dma_start(out=k.YS[r0:r0 + 128, q * 512:(q + 1) * 512], in_=yo[yi][:, :]), reads=[b_yo[yi]], writes=[b_YS])
        p.end_stage()


def stage_ln2(k, l, dst, last):
    p = k.p
    nc = k.nc
    with ExitStack() as es:
        sb = lambda name, shape, d: es.enter_context(nc.sbuf_tensor("L%d_" % l + name, shape, d))
        yk = [[sb("l_yk%d_%d" % (s, i), [128, D], F32) for i in range(4)] for s in range(2)]
        b_yk = [[p.buf("l_yk%d_%d" % (s, i)) for i in range(4)] for s in range(2)]
        x1t = [sb("l_x1t%d" % i, [128, D], F32) for i in range(2)]
        b_x1t = [p.buf("l_x1t%d" % i) for i in range(2)]
        u = sb("l_u", [128, D], F32)
        b_u = p.buf("l_u")
        x2 = [sb("l_x2_%d" % i, [128, D], F32) for i in range(2)]
        b_x2 = [p.buf("l_x2_%d" % i) for i in range(2)]
        lng = sb("l_lng", [128, D], F32)
        lnb = sb("l_lnb", [128, D], F32)
        b_par = p.buf("l_par")
        st = sb("l_st", [128, 8], F32)
        lnsb = {"st": st, "b_st": p.buf("l_st")}
        b_YS = p.buf("YS_r")
        b_x1res = p.buf("x1res_r")
        b_dst = p.buf("dst")
        print("ln2 stage: sbuf bytes remaining:", nc.sbuf_bytes_remaining)
        p.dma("sp", lambda e: e.dma_start(out=lng[:, :], in_=k.ln2_g[l:l + 1, :].broadcast_to([128, D])), writes=[b_par])
        p.dma("sp", lambda e: e.dma_start(out=lnb[:, :], in_=k.ln2_b[l:l + 1, :].broadcast_to([128, D])), writes=[b_par])
        for tb in range(16):
            s2 = tb % 2
            t0, t1 = tb * 128, (tb + 1) * 128
            for kk in range(4):
                p.dma("pool", lambda e, kk=kk: e.indirect_dma_start(out=yk[s2][kk][:, :], out_offset=None, in_=k.YS[:, :],
                                                                   in_offset=bass.IndirectOffsetOnAxis(ap=k.idx[:, tb, kk:kk + 1], axis=0),
                                                                   bounds_check=k.bc_reg, oob_is_err=False),
                      reads=[b_YS, k.b_route], writes=[b_yk[s2][kk]])
            p.dma("sp", lambda e: e.dma_start(out=x1t[s2][:, :], in_=k.x1res[t0:t1, :]), reads=[b_x1res], writes=[b_x1t[s2]])
            p.op("act", lambda e: e.activation(out=u[:, :], in_=x1t[s2][:, :], func=AF.Copy, scale=ALPHA), reads=[b_x1t[s2]], writes=[b_u])
            for kk in range(4):
                p.op("dve", lambda e, kk=kk: e.scalar_tensor_tensor(out=u[:, :], in0=yk[s2][kk][:, :], scalar=k.gk[:, tb, kk:kk + 1], in1=u[:, :], op0=ALU.mult, op1=ALU.add),
                     reads=[b_yk[s2][kk], k.b_route], writes=[b_u])
            xo, b_xo = x2[s2], b_x2[s2]
            ln_block(k, lnsb, u, b_u, xo, b_xo, lng, lnb, b_par)
            p.dma("sp", lambda e: e.dma_start(out=dst[t0:t1, :], in_=xo[:, :]), reads=[b_xo], writes=[b_dst])
            if not last:
                transpose_block(k, xo, b_xo, tb)
        p.end_stage()


_NC_CACHE = {}


def _host_layout(inputs):
    f32 = lambda a: np.ascontiguousarray(np.asarray(a, dtype=np.float32))
    m = {}
    m["w_in"] = f32(inputs["w_in"])
    m["w_out"] = f32(inputs["w_out"])
    gm = np.concatenate([np.asarray(inputs["g_mix_a"], np.float32), np.asarray(inputs["g_mix_b"], np.float32)], axis=1)
    m["gmix"] = np.ascontiguousarray(gm.reshape(NL, 32, 64).transpose(0, 2, 1))
    for n in ["ln1_g", "ln1_b", "ln2_g", "ln2_b", "w_router", "b_router", "b_down", "w_gate_up", "w_down"]:
        m[n] = f32(inputs[n])
    m["b_gu"] = np.ascontiguousarray(np.asarray(inputs["b_gate_up"], np.float32).reshape(NL, NE, 16, 128).transpose(0, 3, 1, 2))
    m.update(make_consts())
    return m


def kernel(**inputs):
    x = np.asarray(inputs["x"], dtype=np.float32)
    nb = x.shape[0]
    shared = _host_layout(inputs)
    if "nc" not in _NC_CACHE:
        _NC_CACHE["nc"] = build(n_layers=NL)
    nc = _NC_CACHE["nc"]
    in_maps = []
    for b in range(nb):
        m = dict(shared)
        m["x"] = np.ascontiguousarray(x[b])
        in_maps.append(m)
    res = run_bass_kernel_spmd(nc, in_maps, core_ids=list(range(nb)))
    out = np.stack([np.asarray(r["out"], dtype=np.float32) for r in res.results], axis=0)
    return out
```

```python
import math
from contextlib import ExitStack

import numpy as np
import ml_dtypes

import concourse.bass as bass
import concourse.mybir as mybir
from concourse.bass_utils import run_bass_kernel_spmd

F32 = mybir.dt.float32
BF16 = mybir.dt.bfloat16
I32 = mybir.dt.int32
AF = mybir.ActivationFunctionType
ALU = mybir.AluOpType
AX = mybir.AxisListType

NL = 4
S = 2048
D = 2048
HD = 64
NH = 16
WA = 1024
NE = 32
CAP = 384
NSLOT = NE * CAP
DFF = 1024
ALPHA = (2.0 * NL) ** 0.25
PAT = ((128, 1), (512, 4), (2048, 16))
BIGD = 30000.0
NEGM = -30000.0
SAME_ENG_SYNC = False


class Buf:
    __slots__ = ("name", "w", "r", "dkey", "excl")

    def __init__(self, name, excl=False):
        self.name = name
        self.w = None
        self.r = {}
        self.dkey = None
        self.excl = excl


class Prog:
    def __init__(self, nc, es, n_dsem=90):
        self.nc = nc
        self.E = {"pe": nc.tensor, "act": nc.scalar, "dve": nc.vector, "pool": nc.gpsimd, "sp": nc.sync}
        self.semobj = {}
        self.cnt = {}
        for k in ["pe", "act", "dve", "pool"]:
            self.semobj[("e", k)] = es.enter_context(nc.semaphore("e_" + k))
            self.cnt[k] = 0
        self.seen = {k: {} for k in self.E}
        self.dfree = []
        self.dval = {}
        for i in range(n_dsem):
            key = ("d", i)
            self.semobj[key] = es.enter_context(nc.semaphore("d%d" % i))
            self.dval[key] = 0
            self.dfree.append(key)
        self.stage_bufs = []
        self.defer = set()

    def buf(self, name):
        b = Buf(name)
        self.stage_bufs.append(b)
        return b

    def _deps(self, eng, reads, writes):
        deps = {}
        me = ("e", eng)

        def add(k, v):
            if v > deps.get(k, 0):
                deps[k] = v

        for b in reads:
            if b.w is not None:
                add(*b.w)
        for b in writes:
            if b.w is not None:
                add(*b.w)
            for k, v in b.r.items():
                if k != me:
                    add(k, v)
        out = []
        seen = self.seen[eng]
        for k, v in deps.items():
            if k == me and eng == "pe":
                continue
            if k[0] == "e":
                assert v <= self.cnt[k[1]], ("wait on not-yet-emitted inc", eng, k, v, self.cnt[k[1]])
            if seen.get(k, 0) < v:
                seen[k] = v
                out.append((k, v))
        return out

    def _emit_waits(self, eng, waits):
        e = self.E[eng]
        for k, v in waits:
            e.wait_ge(self.semobj[k], v)

    def op(self, eng, fn, reads=(), writes=(), inc=True):
        if any(b.excl for b in reads):
            writes = list(writes) + [b for b in reads if b.excl]
            reads = [b for b in reads if not b.excl]
        self._emit_waits(eng, self._deps(eng, reads, writes))
        ins = fn(self.E[eng])
        if inc:
            self.cnt[eng] += 1
            ins.then_inc(self.semobj[("e", eng)], 1)
            tok = (("e", eng), self.cnt[eng])
        else:
            tok = (("e", eng), self.cnt[eng] + 1)
        for b in reads:
            if b.r.get(tok[0], 0) < tok[1]:
                b.r[tok[0]] = tok[1]
        for b in writes:
            b.w = tok
            b.r = {}
        return tok

    def dma(self, q, fn, reads=(), writes=()):
        self._emit_waits(q, self._deps(q, reads, writes))
        wb = writes[0]
        if wb.dkey is None:
            assert self.dfree, "out of DMA semaphores"
            wb.dkey = self.dfree.pop()
        key = wb.dkey
        ins = fn(self.E[q])
        self.dval[key] += 16
        ins.then_inc(self.semobj[key], 16)
        tok = (key, self.dval[key])
        for b in reads:
            if b.r.get(tok[0], 0) < tok[1]:
                b.r[tok[0]] = tok[1]
        for b in writes:
            b.w = tok
            b.r = {}
        return tok

    def barrier(self, engines=("pe", "act", "dve", "pool", "sp")):
        allv = {}
        for k in ["pe", "act", "dve", "pool"]:
            if self.cnt[k] > 0:
                allv[("e", k)] = self.cnt[k]
        for key, v in self.dval.items():
            if v > 0 and key not in self.defer:
                allv[key] = v
        for eng in engines:
            seen = self.seen[eng]
            for k, v in allv.items():
                if k == ("e", eng):
                    continue
                if seen.get(k, 0) < v:
                    seen[k] = v
                    self.E[eng].wait_ge(self.semobj[k], v)

    def end_stage(self):
        self.barrier()
        for b in self.stage_bufs:
            if b.dkey is not None:
                self.dfree.append(b.dkey)
                b.dkey = None
        self.stage_bufs = []


def make_consts():
    c = {}
    k = np.arange(128)[:, None]
    q = np.arange(128)[None, :]
    cur = np.where(q >= k, (q - k).astype(np.float32), BIGD)
    prv = np.where(k >= q, (q + 128 - k).astype(np.float32), BIGD)
    c["c_dm"] = np.concatenate([cur, prv], axis=1).astype(np.float32)
    c["c_identf"] = np.eye(128, dtype=np.float32)
    c["c_identb"] = np.eye(128, dtype=np.float32).astype(ml_dtypes.bfloat16)
    qq = np.arange(512)[None, :]
    neg = np.stack([np.where(128 * o + k < qq, 0.0, NEGM) for o in range(4)], axis=1)
    c["c_neg"] = neg.astype(ml_dtypes.bfloat16)
    kp = np.arange(128)[:, None]
    kk = np.arange(128)[None, :]
    c["c_negtri"] = np.where(kp >= kk, -1.0, 0.0).astype(ml_dtypes.bfloat16)
    c["c_negones"] = np.full((128, 128), -1.0).astype(ml_dtypes.bfloat16)
    c["c_ones"] = np.ones((128, 128), np.float32).astype(ml_dtypes.bfloat16)
    c["c_tstrict"] = np.where(kp < kk, 1.0, 0.0).astype(ml_dtypes.bfloat16)
    sel = np.zeros((128, 64), np.float32)
    sel[64, :] = 1.0
    c["c_sel65"] = sel
    c["c_onesf"] = np.ones((128, 8), np.float32)
    c["c_ecap"] = np.tile((np.arange(NE) * CAP).astype(np.float32)[None, :], (128, 1))
    return c


CONST_SPECS = {
    "c_dm": ([128, 256], F32), "c_identf": ([128, 128], F32), "c_identb": ([128, 128], BF16),
    "c_neg": ([128, 4, 512], BF16), "c_negtri": ([128, 128], BF16), "c_negones": ([128, 128], BF16),
    "c_ones": ([128, 128], BF16), "c_tstrict": ([128, 128], BF16), "c_sel65": ([128, 64], F32),
    "c_onesf": ([128, 8], F32), "c_ecap": ([128, NE], F32),
}


class K:
    pass


def build(n_layers=NL, stages=("attn", "oproj", "moe", "ln2"), debug=()):
    nc = bass.Bass("TRN2", target_bir_lowering=False)
    k = K()
    k.nc = nc
    k.debug = set(debug)
    dt = nc.dram_tensor
    k.x_in = dt("x", [S, D], F32, kind="ExternalInput").ap()
    k.w_in = dt("w_in", [NL, D, 6144], F32, kind="ExternalInput").ap()
    k.w_out = dt("w_out", [NL, D, D], F32, kind="ExternalInput").ap()
    k.gmix = dt("gmix", [NL, 64, 32], F32, kind="ExternalInput").ap()
    k.ln1_g = dt("ln1_g", [NL, D], F32, kind="ExternalInput").ap()
    k.ln1_b = dt("ln1_b", [NL, D], F32, kind="ExternalInput").ap()
    k.ln2_g = dt("ln2_g", [NL, D], F32, kind="ExternalInput").ap()
    k.ln2_b = dt("ln2_b", [NL, D], F32, kind="ExternalInput").ap()
    k.w_router = dt("w_router", [NL, D, NE], F32, kind="ExternalInput").ap()
    k.b_router = dt("b_router", [NL, NE], F32, kind="ExternalInput").ap()
    if "moe" in stages:
        k.w_gu = dt("w_gate_up", [NL, NE, D, 2 * DFF], F32, kind="ExternalInput").ap()
        k.w_dn = dt("w_down", [NL, NE, DFF, D], F32, kind="ExternalInput").ap()
    k.b_gu = dt("b_gu", [NL, 128, NE, 16], F32, kind="ExternalInput").ap()
    k.b_dn = dt("b_down", [NL, NE, D], F32, kind="ExternalInput").ap()
    k.cdram = {n: dt(n, shp, d, kind="ExternalInput").ap() for n, (shp, d) in CONST_SPECS.items()}
    k.out = dt("out", [S, D], F32, kind="ExternalOutput").ap()
    k.xres = dt("xres", [S, D], F32, kind="Internal").ap()
    k.x1res = dt("x1res", [S, D], F32, kind="Internal").ap()
    k.mixT = dt("mixT", [16, 128, S], BF16, kind="Internal").ap()
    k.XS = dt("XS", [NSLOT, D], BF16, kind="Internal").ap()
    k.YS = dt("YS", [NSLOT, D], F32, kind="Internal").ap()
    k.WB1 = dt("WB1", [NE * 4, 128, 16, 512], BF16, kind="Internal").ap()
    k.dbg = {}
    if "mix" in k.debug:
        k.dbg["mixT_o"] = dt("mixT_o", [16, 128, S], BF16, kind="ExternalOutput").ap()
        k.dbg["ss_o"] = dt("ss_o", [128, 32], F32, kind="ExternalOutput").ap()
    if "x1" in k.debug:
        k.dbg["x1_o"] = dt("x1_o", [S, D], F32, kind="ExternalOutput").ap()
        k.dbg["idx_o"] = dt("idx_o", [128, 64], I32, kind="ExternalOutput").ap()
        k.dbg["gk_o"] = dt("gk_o", [128, 64], F32, kind="ExternalOutput").ap()
    if "xs" in k.debug:
        k.dbg["xs_o"] = dt("xs_o", [NSLOT, D], BF16, kind="ExternalOutput").ap()
    if "ys" in k.debug:
        k.dbg["ys_o"] = dt("ys_o", [NSLOT, D], F32, kind="ExternalOutput").ap()

    with ExitStack() as es:
        p = Prog(nc, es)
        k.p = p
        sb = lambda name, shape, d: es.enter_context(nc.sbuf_tensor(name, shape, d))
        ps = lambda name, shape, d: es.enter_context(nc.psum_tensor(name, shape, d))
        k.xT = sb("xT", [128, 16, S], BF16)
        k.b_xT = Buf("xT")
        k.cs = {}
        k.b_c = Buf("consts")
        for n, (shp, d) in CONST_SPECS.items():
            k.cs[n] = sb("s_" + n, shp, d)
        k.ss = sb("ss", [128, 32], F32)
        k.b_ss = Buf("ss")
        k.idx = sb("idx", [128, 16, 4], I32)
        k.gk = sb("gk", [128, 16, 4], F32)
        k.b_route = Buf("route")
        k.b_WB = Buf("WB1")
        k.b_WB.dkey = p.dfree.pop()
        p.defer.add(k.b_WB.dkey)
        k.has_moe = "moe" in stages
        k.F = [ps("F%d" % i, [128, 512], F32) for i in range(7)]
        k.b_F = [Buf("F%d" % i, excl=True) for i in range(7)]
        k.PX = ps("PX", [128, 512], F32)
        k.PT = k.PX[:, 0:256].bitcast(BF16)
        k.b_PT = Buf("PX", excl=True)
        k.PS = k.PX[:, 256:512]
        k.b_PS = k.b_PT
        print("PT shape", k.PT.shape, "PS shape", k.PS.shape)

        k.bc_reg = nc.gpsimd.alloc_register("bc_reg")
        nc.gpsimd.reg_mov(k.bc_reg, NSLOT - 1)
        first = True
        for n in CONST_SPECS:
            src = k.cdram[n]
            dst = k.cs[n]
            if len(CONST_SPECS[n][0]) == 3:
                p.dma("sp", lambda e, d_=dst, s_=src: e.dma_start(out=d_[:, :, :], in_=s_[:, :, :]), writes=[k.b_c])
            else:
                p.dma("sp", lambda e, d_=dst, s_=src: e.dma_start(out=d_[:, :], in_=s_[:, :]), writes=[k.b_c])
        print("sbuf bytes remaining after persistent:", nc.sbuf_bytes_remaining)

        stage_prologue(k)
        xcur = k.x_in
        for l in range(n_layers):
            last = (l == n_layers - 1)
            if "attn" in stages:
                stage_attn(k, l)
            if "mix" in k.debug and l == 0:
                dump_dram(k, k.dbg["mixT_o"].rearrange("c p t -> (c p) t"), k.mixT.rearrange("c p t -> (c p) t"), 2048, BF16, 2048)
                dump_sbuf(k, k.dbg["ss_o"], k.ss, k.b_ss)
            if "oproj" in stages:
                stage_oproj(k, l, xcur)
            if "x1" in k.debug and l == 0:
                dump_dram(k, k.dbg["x1_o"], k.x1res, 2048, F32, 2048)
                dump_sbuf(k, k.dbg["idx_o"], k.idx[:, :, :].rearrange("p a b -> p (a b)"), k.b_route, raw=True)
                dump_sbuf(k, k.dbg["gk_o"], k.gk[:, :, :].rearrange("p a b -> p (a b)"), k.b_route, raw=True)
            if "xs" in k.debug and l == 0:
                dump_dram(k, k.dbg["xs_o"], k.XS, NSLOT, BF16, 2048)
            if "moe" in stages:
                stage_moe(k, l)
            if "ys" in k.debug and l == 0:
                dump_dram(k, k.dbg["ys_o"], k.YS, NSLOT, F32, 2048)
            if "ln2" in stages:
                stage_ln2(k, l, k.out if last else k.xres, last)
            xcur = k.xres
        p.barrier()
    return nc


def dump_dram(k, dst, src, rows, dtp, cols):
    p = k.p
    nc = k.nc
    p.barrier()
    k.ndump = getattr(k, "ndump", 0) + 1
    with nc.sbuf_tensor("dump_t%d" % k.ndump, [128, cols], dtp) as t:
        bt = Buf("dump_t")
        bo = Buf("dump_o")
        for r0 in range(0, rows, 128):
            p.dma("sp", lambda e, r0=r0: e.dma_start(out=t[:, :], in_=src[r0:r0 + 128, :]), writes=[bt])
            p.dma("sp", lambda e, r0=r0: e.dma_start(out=dst[r0:r0 + 128, :], in_=t[:, :]), reads=[bt], writes=[bo])
        p.barrier()
        if bt.dkey is not None:
            p.dfree.append(bt.dkey)
        if bo.dkey is not None:
            p.dfree.append(bo.dkey)


def dump_sbuf(k, dst, src, b, raw=False):
    p = k.p
    p.barrier()
    bo = Buf("dump_o2")
    if raw:
        p.dma("sp", lambda e: e.dma_start(out=dst[:, :], in_=src), reads=[b], writes=[bo])
    else:
        p.dma("sp", lambda e: e.dma_start(out=dst[:, :], in_=src[:, :]), reads=[b], writes=[bo])
    p.barrier()
    p.dfree.append(bo.dkey)


def transpose_block(k, src, b_src, tb, xT_dst=True, f32_dst=None, b_f32=None, fbanks=(0, 1), evac=("act", "dve")):
    p = k.p
    identf = k.cs["c_identf"]
    for g in range(4):
        fb = fbanks[g % len(fbanks)]
        F = k.F[fb]
        bF = k.b_F[fb]
        for i in range(4):
            c = 4 * g + i
            p.op("pe", lambda e, c=c, i=i, F=F: e.transpose(F[:, i * 128:(i + 1) * 128], src[:, c * 128:(c + 1) * 128], identf[:, :]),
                 reads=[b_src, k.b_c], writes=[bF], inc=(i == 3))
        Fv = F[:, :].rearrange("p (a t) -> p a t", a=4)
        if xT_dst:
            eng = evac[g % len(evac)]
            if eng == "act":
                p.op("act", lambda e, g=g, Fv=Fv: e.activation(out=k.xT[:, 4 * g:4 * g + 4, tb * 128:(tb + 1) * 128], in_=Fv, func=AF.Copy),
                     reads=[bF], writes=[k.b_xT])
            else:
                p.op("dve", lambda e, g=g, Fv=Fv: e.tensor_copy(k.xT[:, 4 * g:4 * g + 4, tb * 128:(tb + 1) * 128], Fv),
                     reads=[bF], writes=[k.b_xT])
        if f32_dst is not None:
            p.op("dve", lambda e, g=g, Fv=Fv: e.tensor_copy(f32_dst[:, 4 * g:4 * g + 4, :], Fv), reads=[bF], writes=[b_f32])


def stage_prologue(k):
    p = k.p
    nc = k.nc
    with ExitStack() as es:
        xt = [es.enter_context(nc.sbuf_tensor("pro_x%d" % i, [128, D], F32)) for i in range(3)]
        bx = [p.buf("pro_x%d" % i) for i in range(3)]
        zt = es.enter_context(nc.sbuf_tensor("pro_z", [128, D], BF16))
        bz = p.buf("pro_z")
        bxs = p.buf("XS_init")
        p.op("pool", lambda e: e.memset(zt[:, :], 0.0), writes=[bz])
        for r0 in range(0, NSLOT, 128):
            p.dma("pool", lambda e, r0=r0: e.dma_start(out=k.XS[r0:r0 + 128, :], in_=zt[:, :]), reads=[bz], writes=[bxs])
        for tb in range(16):
            t = xt[tb % 3]
            b = bx[tb % 3]
            p.dma("sp", lambda e, t=t, tb=tb: e.dma_start(out=t[:, :], in_=k.x_in[tb * 128:(tb + 1) * 128, :]), writes=[b])
            transpose_block(k, t, b, tb)
        p.end_stage()


def stage_attn(k, l):
    p = k.p
    nc = k.nc
    cs = k.cs
    with ExitStack() as es:
        sb = lambda name, shape, d: es.enter_context(nc.sbuf_tensor("L%d_" % l + name, shape, d))
        NSET = 2
        wsl = [[sb("a_w%d_%d" % (s, j), [128, 16, 128], BF16) for j in range(3)] for s in range(NSET)]
        b_wsl = [[p.buf("a_w%d_%d" % (s, j)) for j in range(3)] for s in range(NSET)]
        qkvT = [[sb("a_qkv%d_%d" % (s, j), [128, S], BF16) for j in range(3)] for s in range(NSET)]
        b_qkvT = [[p.buf("a_qkv%d_%d" % (s, j)) for j in range(3)] for s in range(NSET)]
        Vr = [[sb("a_vr%d_%d" % (s, r), [128, 16, 2, 65], BF16) for r in range(3)] for s in range(NSET)]
        b_Vr = [[p.buf("a_vr%d_%d" % (s, r)) for r in range(3)] for s in range(NSET)]
        acc = [sb("a_acc%d" % i, [128, S], F32) for i in range(2)]
        b_acc = [p.buf("a_acc%d" % i) for i in range(2)]
        NW = 4
        Pe = [sb("a_pe%d" % i, [128, 256], F32) for i in range(NW)]
        b_Pe = [p.buf("a_pe%d" % i) for i in range(NW)]
        Pm = [sb("a_pm%d" % i, [128, 256], BF16) for i in range(NW)]
        b_Pm = [p.buf("a_pm%d" % i) for i in range(NW)]
        Md = [sb("a_md%d" % i, [128, 256], F32) for i in range(2)]
        b_Md = [p.buf("a_md%d" % i) for i in range(2)]
        e32 = [sb("b_e%d" % i, [128, 512], F32) for i in range(2)]
        b_e32 = [p.buf("b_e%d" % i) for i in range(2)]
        spt = [sb("b_sp%d" % i, [128, 512], BF16) for i in range(3)]
        b_spt = [p.buf("b_sp%d" % i) for i in range(3)]
        ss32 = [sb("b_ss%d" % i, [128, 512], F32) for i in range(2)]
        b_ss32 = [p.buf("b_ss%d" % i) for i in range(2)]
        ssb = [sb("b_ssb%d" % i, [128, 512], BF16) for i in range(3)]
        b_ssb = [p.buf("b_ssb%d" % i) for i in range(3)]
        at = [sb("b_a%d" % i, [128, 512], BF16) for i in range(3)]
        b_at = [p.buf("b_a%d" % i) for i in range(3)]
        oaf = [sb("f_oaf%d" % i, [64, 512], F32) for i in range(2)]
        b_oaf = [p.buf("f_oaf%d" % i) for i in range(2)]
        oab = [sb("f_oab%d" % i, [64, 512], BF16) for i in range(2)]
        b_oab = [p.buf("f_oab%d" % i) for i in range(2)]
        sq = [sb("f_sq%d" % i, [64, 512], F32) for i in range(2)]
        b_sq = [p.buf("f_sq%d" % i) for i in range(2)]
        b_mixT = p.buf("mixT")
        gmt = sb("a_gmt", [64, 32], F32)
        b_gmt = p.buf("a_gmt")
        p.dma("sp", lambda e: e.dma_start(out=gmt[:, :], in_=k.gmix[l, :, :]), writes=[b_gmt])
        print("attn stage: sbuf bytes remaining:", nc.sbuf_bytes_remaining)

        for s in range(NSET):
            for r in range(3):
                p.op("pool", lambda e, s=s, r=r: e.memset(Vr[s][r][:, :, :, 64:65], 1.0), writes=[b_Vr[s][r]])
        p.op("pool", lambda e: e.memset(k.ss[:, :], 0.0), writes=[k.b_ss])

        st = {"fin": 0, "pe": 0, "pm": 0, "md": 0}

        def col0(pi, j):
            base = 0 if pi < 8 else 3 * WA
            return base + j * WA + (pi % 8) * 128

        def bg_pair(pi):
            s = pi % NSET
            for j in range(3):
                c0 = col0(pi, j)
                src = k.w_in[l].rearrange("(c p) n -> p c n", p=128)[:, :, c0:c0 + 128]
                p.dma("pool", lambda e, s=s, j=j, src=src: e.dma_start(out=wsl[s][j][:, :, :], in_=src), writes=[b_wsl[s][j]])
            if k.has_moe:
                for e_ in (2 * pi, 2 * pi + 1):
                    for s_ in range(4):
                        gu, half = s_ % 2, s_ // 2
                        c0 = gu * DFF + 512 * half
                        srcw = k.w_gu[l, e_].rearrange("(c p) n -> p c n", p=128)[:, :, c0:c0 + 512]
                        for hc in range(2):
                            p.dma("pool", lambda e, e_=e_, s_=s_, hc=hc, srcw=srcw: e.dma_start(out=k.WB1[4 * e_ + s_, :, 8 * hc:8 * hc + 8, :], in_=srcw[:, 8 * hc:8 * hc + 8, :]),
                                  writes=[k.b_WB])
            yield
            gi = 0
            for j in range(3):
                for n in range(4):
                    fb = gi % 2
                    gi += 1
                    F = k.F[fb]
                    bF = k.b_F[fb]
                    for c in range(16):
                        p.op("pe", lambda e, c=c, F=F, j=j, n=n: e.matmul(F[:, :], lhsT=wsl[s][j][:, c, :], rhs=k.xT[:, c, n * 512:(n + 1) * 512],
                                                                        start=(c == 0), stop=(c == 15)),
                             reads=[b_wsl[s][j], k.b_xT], writes=[bF], inc=(c == 15))
                        if c % 4 == 3:
                            yield
                    scale = 0.125 if j == 0 else 1.0
                    if gi % 2 == 0:
                        p.op("act", lambda e, F=F, j=j, n=n, scale=scale: e.activation(out=qkvT[s][j][:, n * 512:(n + 1) * 512], in_=F[:, :], func=AF.Copy, scale=scale),
                             reads=[bF], writes=[b_qkvT[s][j]])
                    else:
                        p.op("dve", lambda e, F=F, j=j, n=n, scale=scale: e.tensor_scalar(out=qkvT[s][j][:, n * 512:(n + 1) * 512], in0=F[:, :], scalar1=scale, scalar2=None, op0=ALU.mult),
                             reads=[bF], writes=[b_qkvT[s][j]])
                    yield
            npat = 3 if pi < 8 else 1
            vT = qkvT[s][2]
            for r in range(npat):
                d = PAT[r][1]
                nblk = S // d // 128
                vv = vT[:, :].rearrange("p (i d) -> p d i", d=d)
                for g in range(4):
                    for i in range(4):
                        B = 4 * g + i
                        c, n = B // nblk, B % nblk
                        p.op("pe", lambda e, i=i, c=c, n=n, vv=vv: e.transpose(k.PT[:, i * 128:(i + 1) * 128], vv[:, c, n * 128:(n + 1) * 128], cs["c_identb"][:, :]),
                             reads=[b_qkvT[s][2], k.b_c], writes=[k.b_PT], inc=(i == 3))
                    p.op("dve", lambda e, g=g, r=r: e.tensor_copy(Vr[s][r][:, 4 * g:4 * g + 4, :, 0:64],
                                                                 k.PT[:, :].rearrange("p (a h d) -> p a h d", a=4, h=2)),
                         reads=[k.b_PT], writes=[b_Vr[s][r]])
                    yield

        def run_all(gen):
            for _ in gen:
                pass

        def step(gen):
            if gen is not None:
                try:
                    next(gen)
                except StopIteration:
                    return None
            return gen

        def finalize(h_glob, n, src_kind, accb=None, b_accb=None, Fsrc=None, b_Fsrc=None):
            i = st["fin"] % 2
            st["fin"] += 1
            if src_kind == "A":
                F6 = k.F[6]
                p.op("pe", lambda e: e.matmul(F6[0:64, :], lhsT=cs["c_sel65"][0:65, :], rhs=accb[0:65, n * 512:(n + 1) * 512], start=True, stop=True),
                     reads=[b_accb, k.b_c], writes=[k.b_F[6]])
                p.op("dve", lambda e: e.reciprocal(out=sq[i][:, :], in_=F6[0:64, :]), reads=[k.b_F[6]], writes=[b_sq[i]])
                p.op("dve", lambda e: e.tensor_tensor(out=oaf[i][:, :], in0=accb[0:64, n * 512:(n + 1) * 512], in1=sq[i][:, :], op=ALU.mult),
                     reads=[b_accb, b_sq[i]], writes=[b_oaf[i]])
            else:
                p.op("dve", lambda e: e.tensor_copy(oaf[i][:, :], Fsrc[0:64, :]), reads=[b_Fsrc], writes=[b_oaf[i]])
            p.op("dve", lambda e: e.tensor_scalar(out=oab[i][:, :], in0=oaf[i][:, :], scalar1=gmt[:, h_glob:h_glob + 1], scalar2=None, op0=ALU.mult),
                 reads=[b_oaf[i], b_gmt], writes=[b_oab[i]])
            p.op("act", lambda e: e.activation(out=sq[i][:, :], in_=oaf[i][:, :], func=AF.Square), reads=[b_oaf[i]], writes=[b_sq[i]])
            for tb in range(4):
                p.op("pe", lambda e, tb=tb: e.matmul(k.PS[:, tb:tb + 1], lhsT=sq[i][:, tb * 128:(tb + 1) * 128], rhs=cs["c_onesf"][0:64, 0:1], start=True, stop=True),
                     reads=[b_sq[i], k.b_c], writes=[k.b_PS], inc=(tb == 3))
            off = 0 if h_glob < 16 else 16
            p.op("dve", lambda e: e.tensor_tensor(out=k.ss[:, off + 4 * n:off + 4 * n + 4], in0=k.ss[:, off + 4 * n:off + 4 * n + 4], in1=k.PS[:, 0:4], op=ALU.add),
                 reads=[k.b_PS, k.b_ss], writes=[k.b_ss])
            ch, ph = h_glob // 2, (h_glob % 2) * 64
            p.dma("sp", lambda e: e.dma_start(out=k.mixT[ch, ph:ph + 64, n * 512:(n + 1) * 512], in_=oab[i][:, :]), reads=[b_oab[i]], writes=[b_mixT])

        def attn_A(pi, bg):
            s = pi % NSET
            qT, kT = qkvT[s][0], qkvT[s][1]
            bq, bk = b_qkvT[s][0], b_qkvT[s][1]
            for hh in range(2):
                h = 2 * pi + hh
                slope = 2.0 ** (-8.0 * (h + 1) / NH)
                accb, b_accb = acc[hh], b_acc[hh]
                r0, r1 = hh * 64, hh * 64 + 64
                for r in range(3):
                    d = PAT[r][1]
                    nblk = S // d // 128
                    mi = st["md"] % 2
                    st["md"] += 1
                    p.op("act", lambda e, mi=mi, d=d: e.activation(out=Md[mi][:, :], in_=cs["c_dm"][:, :], func=AF.Exp, scale=-slope * d),
                         reads=[k.b_c], writes=[b_Md[mi]])
                    qv = qT[r0:r1, :].rearrange("p (i d) -> p d i", d=d)
                    kv = kT[r0:r1, :].rearrange("p (i d) -> p d i", d=d)
                    av = accb[0:65, :].rearrange("p (i d) -> p d i", d=d)
                    tiles = [(B // nblk, B % nblk) for B in range(16)]
                    pendq = []
                    for t in range(18):
                        cur = None
                        if t < 16:
                            c, m = tiles[t]
                            ncols = 256 if m + 1 < nblk else 128
                            sbk = 2 + (t % 2)
                            FS = k.F[sbk]
                            p.op("pe", lambda e, c=c, m=m, ncols=ncols, FS=FS: e.matmul(FS[:, 0:ncols], lhsT=kv[:, c, m * 128:(m + 1) * 128],
                                                                                      rhs=qv[:, c, m * 128:m * 128 + ncols], start=True, stop=True),
                                 reads=[bq, bk], writes=[k.b_F[sbk]])
                            wi = st["pe"] % NW
                            st["pe"] += 1
                            p.op("act", lambda e, wi=wi, ncols=ncols, FS=FS: e.activation(out=Pe[wi][:, 0:ncols], in_=FS[:, 0:ncols], func=AF.Exp),
                                 reads=[k.b_F[sbk]], writes=[b_Pe[wi]])
                            p.op("dve", lambda e, wi=wi, ncols=ncols, mi=mi: e.tensor_tensor(out=Pm[wi][:, 0:ncols], in0=Pe[wi][:, 0:ncols], in1=Md[mi][:, 0:ncols], op=ALU.mult),
                                 reads=[b_Pe[wi], b_Md[mi]], writes=[b_Pm[wi]])
                            cur = (t, c, m, ncols, wi)
                        if cur is not None:
                            pendq.append(cur)
                        if t >= 2 or t >= 16:
                            tt, c, m, ncols, wi = pendq.pop(0)
                            B = tt
                            g = B // 4
                            ob = 4 + (g % 2)
                            FO = k.F[ob]
                            lhs = Vr[s][r][:, B, hh, :]
                            p.op("pe", lambda e, FO=FO, lhs=lhs, wi=wi, B=B, m=m: e.matmul(FO[0:65, (B % 4) * 128:(B % 4) * 128 + 128], lhsT=lhs, rhs=Pm[wi][:, 0:128],
                                                                                       start=(m == 0), stop=True),
                                 reads=[b_Vr[s][r], b_Pm[wi]], writes=[k.b_F[ob]], inc=True)
                            if ncols == 256:
                                B2 = B + 1
                                ob2 = 4 + ((B2 // 4) % 2)
                                FO2 = k.F[ob2]
                                p.op("pe", lambda e, FO2=FO2, lhs=lhs, wi=wi, B2=B2: e.matmul(FO2[0:65, (B2 % 4) * 128:(B2 % 4) * 128 + 128], lhsT=lhs, rhs=Pm[wi][:, 128:256],
                                                                                          start=True, stop=False),
                                     reads=[b_Vr[s][r], b_Pm[wi]], writes=[k.b_F[ob2]], inc=True)
                            if B % 4 == 3:
                                if d == 1:
                                    dst = av[:, 0, g * 512:(g + 1) * 512]
                                    srcv = FO[0:65, :]
                                elif d == 4:
                                    dst = av[:, g, 0:512]
                                    srcv = FO[0:65, :]
                                else:
                                    dst = av[:, 4 * g:4 * g + 4, 0:128]
                                    srcv = FO[0:65, :].rearrange("p (a t) -> p a t", a=4)
                                if r == 0:
                                    p.op("dve", lambda e, dst=dst, srcv=srcv: e.tensor_copy(dst, srcv), reads=[k.b_F[ob]], writes=[b_accb])
                                else:
                                    p.op("dve", lambda e, dst=dst, srcv=srcv: e.tensor_tensor(out=dst, in0=dst, in1=srcv, op=ALU.add), reads=[k.b_F[ob], b_accb], writes=[b_accb])
                            bg = step(bg)
                    assert not pendq
                for n in range(4):
                    finalize(h, n, "A", accb=accb, b_accb=b_accb)
                    bg = step(bg)
            return bg

        def attn_B(pi, bg):
            s = pi % NSET
            qT, kT = qkvT[s][0], qkvT[s][1]
            bq, bk = b_qkvT[s][0], b_qkvT[s][1]
            V0, bV0 = Vr[s][0], b_Vr[s][0]
            tiles = []
            for hh in range(2):
                for m in range(4):
                    for j in range(4 * m + 3, -1, -1):
                        tiles.append((hh, m, j))
            N = len(tiles)
            info = {}

            def PEz(t):
                hh, m, j = tiles[t]
                r0, r1 = hh * 64, hh * 64 + 64
                zb = 2 + (t % 4)
                diag = j >= 4 * m
                p.op("pe", lambda e: e.matmul(k.F[zb][:, :], lhsT=kT[r0:r1, j * 128:(j + 1) * 128], rhs=qT[r0:r1, m * 512:(m + 1) * 512], start=True, stop=False),
                     reads=[bq, bk], writes=[k.b_F[zb]], inc=not diag)
                if diag:
                    p.op("pe", lambda e: e.matmul(k.F[zb][:, :], lhsT=cs["c_identb"][:, :], rhs=cs["c_neg"][:, j - 4 * m, :], start=False, stop=False),
                         reads=[k.b_c], writes=[k.b_F[zb]])

            def ACTe(t):
                zb = 2 + (t % 4)
                ei = t % 2
                p.op("act", lambda e: e.activation(out=e32[ei][:, :], in_=k.F[zb][:, :], func=AF.Exp), reads=[k.b_F[zb]], writes=[b_e32[ei]])

            def ACTsp(t):
                ei = t % 2
                si = t % 3
                p.op("act", lambda e: e.activation(out=spt[si][:, :], in_=e32[ei][:, :], func=AF.Ln, bias=1.0), reads=[b_e32[ei]], writes=[b_spt[si]])

            def SSupd(t):
                hh, m, j = tiles[t]
                if j == 0:
                    return
                first = (j == 4 * m + 3)
                gidx = (hh * 4 + m) % 2
                si = t % 3
                if first:
                    p.op("dve", lambda e: e.tensor_copy(ss32[gidx][:, :], spt[si][:, :]), reads=[b_spt[si]], writes=[b_ss32[gidx]])
                else:
                    p.op("dve", lambda e: e.tensor_tensor(out=ss32[gidx][:, :], in0=ss32[gidx][:, :], in1=spt[si][:, :], op=ALU.add),
                         reads=[b_spt[si], b_ss32[gidx]], writes=[b_ss32[gidx]])
                p.op("dve", lambda e: e.tensor_copy(ssb[si][:, :], ss32[gidx][:, :]), reads=[b_ss32[gidx]], writes=[b_ssb[si]])

            def PEB(t):
                hh, m, j = tiles[t]
                bb = 2 + (t % 4)
                si = t % 3
                first = (j == 4 * m + 3)
                FB = k.F[bb]
                p.op("pe", lambda e: e.matmul(FB[:, :], lhsT=cs["c_negtri"][:, :], rhs=spt[si][:, :], start=False, stop=first),
                     reads=[k.b_c, b_spt[si]], writes=[k.b_F[bb]], inc=first)
                if not first:
                    sp_prev = (t - 1) % 3
                    p.op("pe", lambda e: e.matmul(FB[:, :], lhsT=cs["c_negones"][:, :], rhs=ssb[sp_prev][:, :], start=False, stop=True),
                         reads=[k.b_c, b_ssb[sp_prev]], writes=[k.b_F[bb]], inc=True)

            def ACTa(t):
                bb = 2 + (t % 4)
                ai = t % 3
                p.op("act", lambda e: e.activation(out=at[ai][:, :], in_=k.F[bb][:, :], func=AF.Exp), reads=[k.b_F[bb]], writes=[b_at[ai]])

            def PEav(t):
                hh, m, j = tiles[t]
                ai = t % 3
                first = (j == 4 * m + 3)
                p.op("pe", lambda e: e.matmul(k.F[6][0:64, :], lhsT=V0[:, j, hh, 0:64], rhs=at[ai][:, :], start=first, stop=(j == 0)),
                     reads=[bV0, b_at[ai]], writes=[k.b_F[6]], inc=True)
                if j == 0:
                    finalize(16 + 2 * (pi - 8) + hh, m, "B", Fsrc=k.F[6], b_Fsrc=k.b_F[6])

            for t in range(-1, N + 1):
                if t + 1 < N:
                    PEz(t + 1)
                    ACTe(t + 1)
                if 0 <= t < N:
                    PEB(t)
                if t + 1 < N:
                    ACTsp(t + 1)
                    SSupd(t + 1)
                if 0 <= t < N:
                    ACTa(t)
                if t >= 1:
                    PEav(t - 1)
                    bg = step(bg)
            return bg

        import os
        lim = int(os.environ.get("ATTN_LIMIT", "99"))
        bg = bg_pair(0)
        run_all(bg)
        for pi in range(16):
            if lim == 0 or (lim == 1 and pi >= 1) or (lim == 2 and pi != 8):
                continue
            if lim == 2:
                run_all(bg_pair(8))
            nxt = bg_pair(pi + 1) if pi + 1 < 16 else None
            if pi < 8:
                nxt = attn_A(pi, nxt)
            else:
                nxt = attn_B(pi, nxt)
            if nxt is not None:
                run_all(nxt)
        p.end_stage()


def ln_block(k, sbufs, u, b_u, dst, b_dst, lng, lnb, b_ln):
    p = k.p
    st, b_st = sbufs["st"], sbufs["b_st"]
    p.op("dve", lambda e: e.reduce_sum(out=st[:, 0:1], in_=u[:, :], axis=AX.X), reads=[b_u], writes=[b_st])
    p.op("dve", lambda e: e.memset(st[:, 1:2], 0.0), writes=[b_st])
    p.op("act", lambda e: e.activation(out=dst[:, :], in_=u[:, :], func=AF.Square, accum_out=st[:, 1:2]), reads=[b_u], writes=[b_dst, b_st])
    p.op("dve", lambda e: e.tensor_scalar(out=st[:, 2:3], in0=st[:, 0:1], scalar1=1.0 / D, scalar2=None, op0=ALU.mult), reads=[b_st], writes=[b_st])
    p.op("dve", lambda e: e.tensor_tensor(out=st[:, 3:4], in0=st[:, 2:3], in1=st[:, 2:3], op=ALU.mult), reads=[b_st], writes=[b_st])
    p.op("dve", lambda e: e.scalar_tensor_tensor(out=st[:, 4:5], in0=st[:, 1:2], scalar=1.0 / D, in1=st[:, 3:4], op0=ALU.mult, op1=ALU.subtract),
         reads=[b_st], writes=[b_st])
    p.op("dve", lambda e: e.tensor_scalar(out=st[:, 4:5], in0=st[:, 4:5], scalar1=1e-5, scalar2=None, op0=ALU.add), reads=[b_st], writes=[b_st])
    p.op("act", lambda e: e.activation(out=st[:, 5:6], in_=st[:, 4:5], func=AF.Sqrt), reads=[b_st], writes=[b_st])
    p.op("dve", lambda e: e.reciprocal(out=st[:, 6:7], in_=st[:, 5:6]), reads=[b_st], writes=[b_st])
    p.op("dve", lambda e: e.tensor_scalar(out=dst[:, :], in0=u[:, :], scalar1=st[:, 2:3], scalar2=st[:, 6:7], op0=ALU.subtract, op1=ALU.mult),
         reads=[b_u, b_st], writes=[b_dst])
    p.op("dve", lambda e: e.tensor_tensor(out=dst[:, :], in0=dst[:, :], in1=lng[:, :], op=ALU.mult), reads=[b_dst, b_ln], writes=[b_dst])
    p.op("dve", lambda e: e.tensor_tensor(out=dst[:, :], in0=dst[:, :], in1=lnb[:, :], op=ALU.add), reads=[b_dst, b_ln], writes=[b_dst])


def stage_oproj(k, l, xcur):
    p = k.p
    nc = k.nc
    cs = k.cs
    with ExitStack() as es:
        sb = lambda name, shape, d: es.enter_context(nc.sbuf_tensor("L%d_" % l + name, shape, d))
        wout = k.xT
        b_wout = k.b_xT
        gm = sb("o_gm", [128, 16], F32)
        lng = sb("o_lng", [128, D], F32)
        lnb = sb("o_lnb", [128, D], F32)
        wr = sb("o_wr", [128, 16, NE], F32)
        brt = sb("o_brt", [128, NE], F32)
        b_par = p.buf("o_par")
        mixt = [sb("o_mixt%d" % i, [128, 16, 128], BF16) for i in range(2)]
        b_mixt = [p.buf("o_mixt%d" % i) for i in range(2)]
        xr = [sb("o_xr%d" % i, [128, D], F32) for i in range(2)]
        b_xr = [p.buf("o_xr%d" % i) for i in range(2)]
        u = sb("o_u", [128, D], F32)
        b_u = p.buf("o_u")
        x1f = [sb("o_x1f%d" % i, [128, D], F32) for i in range(2)]
        b_x1f = [p.buf("o_x1f%d" % i) for i in range(2)]
        x1b = [sb("o_x1b%d" % i, [128, D], BF16) for i in range(2)]
        b_x1b = [p.buf("o_x1b%d" % i) for i in range(2)]
        x1T = sb("o_x1T", [128, 16, 128], F32)
        b_x1T = p.buf("o_x1T")
        selb = sb("o_selb", [128, 16, NE], BF16)
        b_selb = p.buf("o_selb")
        st = sb("o_st", [128, 8], F32)
        rr = sb("o_rr", [128, 4], F32)
        b_rr = p.buf("o_rr")
        sm = {n: sb("o_" + n, [128, NE], F32) for n in ["lg", "sel", "ex", "G", "oh", "tmp", "pos"]}
        b_sm = p.buf("o_sm")
        mx8 = sb("o_mx8", [128, 8], F32)
        pk = sb("o_pk", [128, 8], F32)
        b_x1res = p.buf("x1res")
        b_XS = p.buf("XS")
        b_mixT = p.buf("mixT_r")
        lnsb = {"st": st, "b_st": p.buf("o_st")}
        print("oproj stage: sbuf bytes remaining:", nc.sbuf_bytes_remaining)

        p.dma("sp", lambda e: e.dma_start(out=lng[:, :], in_=k.ln1_g[l:l + 1, :].broadcast_to([128, D])), writes=[b_par])
        p.dma("sp", lambda e: e.dma_start(out=lnb[:, :], in_=k.ln1_b[l:l + 1, :].broadcast_to([128, D])), writes=[b_par])
        p.dma("sp", lambda e: e.dma_start(out=wr[:, :, :], in_=k.w_router[l].rearrange("(c p) n -> p c n", p=128)), writes=[b_par])
        p.dma("sp", lambda e: e.dma_start(out=brt[:, :], in_=k.b_router[l:l + 1, :].broadcast_to([128, NE])), writes=[b_par])
        for c in range(16):
            p.dma("pool", lambda e, c=c: e.dma_start(out=wout[:, c, :], in_=k.w_out[l, c * 128:(c + 1) * 128, :]), writes=[b_wout])

        ssv = k.ss[:, :].rearrange("p (a b) -> p a b", a=2)
        for tb in range(16):
            i2 = tb % 2
            t0, t1 = tb * 128, (tb + 1) * 128
            p.dma("sp", lambda e: e.dma_start(out=mixt[i2][:, :, :], in_=k.mixT[:, :, t0:t1].rearrange("c p t -> p c t")), reads=[b_mixT], writes=[b_mixt[i2]])
            p.dma("sp", lambda e: e.dma_start(out=xr[i2][:, :], in_=xcur[t0:t1, :]), writes=[b_xr[i2]])
            p.op("dve", lambda e: e.tensor_scalar(out=rr[:, 0:2], in0=ssv[:, :, tb], scalar1=1.0 / WA, scalar2=1e-6, op0=ALU.mult, op1=ALU.add),
                 reads=[k.b_ss], writes=[b_rr])
            p.op("act", lambda e: e.activation(out=rr[:, 0:2], in_=rr[:, 0:2], func=AF.Sqrt), writes=[b_rr])
            p.op("dve", lambda e: e.reciprocal(out=rr[:, 2:4], in_=rr[:, 0:2]), writes=[b_rr])
            p.op("act", lambda e: e.activation(out=u[:, :], in_=xr[i2][:, :], func=AF.Copy, scale=ALPHA), reads=[b_xr[i2]], writes=[b_u])
            for part in range(2):
                for nb in range(4):
                    for c in range(8):
                        cc = 8 * part + c
                        p.op("pe", lambda e, nb=nb, cc=cc, c=c: e.matmul(k.F[nb][:, :], lhsT=mixt[i2][:, cc, :], rhs=wout[:, cc, nb * 512:(nb + 1) * 512],
                                                                      start=(c == 0), stop=(c == 7)),
                             reads=[b_mixt[i2], b_wout], writes=[k.b_F[nb]], inc=(c == 7))
                for nb in range(4):
                    p.op("dve", lambda e, nb=nb, part=part: e.scalar_tensor_tensor(out=u[:, nb * 512:(nb + 1) * 512], in0=k.F[nb][:, :], scalar=rr[:, 2 + part:3 + part],
                                                                                 in1=u[:, nb * 512:(nb + 1) * 512], op0=ALU.mult, op1=ALU.add),
                         reads=[k.b_F[nb], b_rr], writes=[b_u])
            xf, b_xf = x1f[i2], b_x1f[i2]
            ln_block(k, lnsb, u, b_u, xf, b_xf, lng, lnb, b_par)
            p.op("act", lambda e: e.activation(out=x1b[i2][:, :], in_=xf[:, :], func=AF.Copy), reads=[b_xf], writes=[b_x1b[i2]])
            p.dma("sp", lambda e: e.dma_start(out=k.x1res[t0:t1, :], in_=xf[:, :]), reads=[b_xf], writes=[b_x1res])
            transpose_block(k, xf, b_xf, tb, xT_dst=False, f32_dst=x1T, b_f32=b_x1T, fbanks=(4, 5))
            for c in range(16):
                p.op("pe", lambda e, c=c: e.matmul(k.F[6][:, 0:NE], lhsT=x1T[:, c, :], rhs=wr[:, c, :], start=(c == 0), stop=(c == 15)),
                     reads=[b_x1T, b_par], writes=[k.b_F[6]], inc=(c == 15))
            lg, sel, ex, G, oh, tmp, pos = [sm[n] for n in ["lg", "sel", "ex", "G", "oh", "tmp", "pos"]]
            W = [b_sm]
            p.op("dve", lambda e: e.tensor_tensor(out=lg[:, :], in0=k.F[6][:, 0:NE], in1=brt[:, :], op=ALU.add), reads=[k.b_F[6], b_par], writes=W)
            p.op("dve", lambda e: e.max(out=mx8[:, :], in_=lg[:, :]), writes=W)
            p.op("dve", lambda e: e.tensor_scalar(out=sel[:, :], in0=lg[:, :], scalar1=mx8[:, 3:4], scalar2=None, op0=ALU.is_ge), writes=W)
            p.op("dve", lambda e: e.tensor_copy(selb[:, tb, :], sel[:, :]), reads=W, writes=[b_selb])
            p.op("dve", lambda e: e.tensor_scalar(out=pk[:, 4:5], in0=mx8[:, 0:1], scalar1=-1.0, scalar2=None, op0=ALU.mult), writes=W)
            p.op("act", lambda e: e.activation(out=ex[:, :], in_=lg[:, :], func=AF.Exp, bias=pk[:, 4:5]), writes=W)
            p.op("dve", lambda e: e.tensor_tensor(out=ex[:, :], in0=ex[:, :], in1=sel[:, :], op=ALU.mult), writes=W)
            p.op("dve", lambda e: e.reduce_sum(out=pk[:, 5:6], in_=ex[:, :], axis=AX.X), writes=W)
            p.op("dve", lambda e: e.reciprocal(out=pk[:, 6:7], in_=pk[:, 5:6]), writes=W)
            p.op("dve", lambda e: e.tensor_scalar(out=G[:, :], in0=ex[:, :], scalar1=pk[:, 6:7], scalar2=None, op0=ALU.mult), writes=W)
            for b2 in range(tb + 1):
                lhs = cs["c_ones"] if b2 < tb else cs["c_tstrict"]
                p.op("pe", lambda e, b2=b2, lhs=lhs: e.matmul(k.PS[:, 0:NE], lhsT=lhs[:, :], rhs=selb[:, b2, :], start=(b2 == 0), stop=(b2 == tb)),
                     reads=[b_selb, k.b_c], writes=[k.b_PS], inc=(b2 == tb))
            p.op("dve", lambda e: e.tensor_scalar(out=tmp[:, :], in0=k.PS[:, 0:NE], scalar1=float(CAP), scalar2=1.0e6, op0=ALU.is_ge, op1=ALU.mult),
                 reads=[k.b_PS], writes=W)
            p.op("dve", lambda e: e.tensor_tensor(out=pos[:, :], in0=k.PS[:, 0:NE], in1=cs["c_ecap"][:, :], op=ALU.add), reads=[k.b_PS, k.b_c], writes=W)
            p.op("dve", lambda e: e.tensor_tensor(out=pos[:, :], in0=pos[:, :], in1=tmp[:, :], op=ALU.add), writes=W)
            for kk in range(4):
                p.op("dve", lambda e, kk=kk: e.tensor_scalar(out=oh[:, :], in0=lg[:, :], scalar1=mx8[:, kk:kk + 1], scalar2=None, op0=ALU.is_equal), writes=W)
                p.op("dve", lambda e: e.tensor_tensor(out=tmp[:, :], in0=oh[:, :], in1=G[:, :], op=ALU.mult), writes=W)
                p.op("dve", lambda e, kk=kk: e.reduce_sum(out=k.gk[:, tb, kk:kk + 1], in_=tmp[:, :], axis=AX.X), reads=W, writes=[k.b_route])
                p.op("dve", lambda e: e.tensor_tensor(out=tmp[:, :], in0=oh[:, :], in1=pos[:, :], op=ALU.mult), writes=W)
                p.op("dve", lambda e, kk=kk: e.reduce_sum(out=pk[:, kk:kk + 1], in_=tmp[:, :], axis=AX.X), writes=W)
            p.op("dve", lambda e: e.tensor_copy(k.idx[:, tb, :], pk[:, 0:4]), reads=W, writes=[k.b_route])
            for kk in range(4):
                p.dma("pool", lambda e, kk=kk: e.indirect_dma_start(out=k.XS[:, :], out_offset=bass.IndirectOffsetOnAxis(ap=k.idx[:, tb, kk:kk + 1], axis=0),
                                                                   in_=x1b[i2][:, :], in_offset=None, bounds_check=k.bc_reg, oob_is_err=False),
                      reads=[b_x1b[i2], k.b_route], writes=[b_XS])
        p.end_stage()


def stage_moe(k, l):
    p = k.p
    nc = k.nc
    cs = k.cs
    NS = CAP
    with ExitStack() as es:
        sb = lambda name, shape, d: es.enter_context(nc.sbuf_tensor("L%d_" % l + name, shape, d))
        NW1 = 4
        w1 = [sb("m_w1_%d" % i, [128, 16, 512], BF16) for i in range(NW1)]
        b_w1 = [p.buf("m_w1_%d" % i) for i in range(NW1)]
        w2 = [k.xT[:, 0:8, :], k.xT[:, 8:16, :]]
        b_w2 = [p.buf("m_w2_0"), p.buf("m_w2_1")]
        xs = sb("m_xs", [128, 3, D], BF16)
        b_xs = p.buf("m_xs")
        xeT = sb("m_xeT", [128, 16, NS], BF16)
        b_xeT = p.buf("m_xeT")
        hT = [sb("m_hT%d" % i, [128, 8, NS], BF16) for i in range(2)]
        b_hT = [p.buf("m_hT%d" % i) for i in range(2)]
        NT = 2
        gc = [sb("m_gc%d" % i, [128, NS], F32) for i in range(NT)]
        sg = [sb("m_sg%d" % i, [128, NS], F32) for i in range(NT)]
        uc = [sb("m_uc%d" % i, [128, NS], F32) for i in range(NT)]
        b_tmp = [p.buf("m_tmp%d" % i) for i in range(NT)]
        NY = 4
        yo = [sb("m_yo%d" % i, [128, 512], F32) for i in range(NY)]
        b_yo = [p.buf("m_yo%d" % i) for i in range(NY)]
        b1 = sb("m_b1", [128, NE, 16], F32)
        b_b1 = p.buf("m_b1")
        b2 = [sb("m_b2_%d" % i, [128, D], F32) for i in range(2)]
        b_b2 = [p.buf("m_b2_%d" % i) for i in range(2)]
        b_XS = p.buf("XS_r")
        b_YS = p.buf("YS")
        print("moe stage: sbuf bytes remaining:", nc.sbuf_bytes_remaining)

        p.dma("sp", lambda e: e.dma_start(out=b1[:, :, :], in_=k.b_gu[l, :, :, :]), writes=[b_b1])

        def load_slab(g_):
            wi = g_ % NW1
            for hc in range(2):
                p.dma("sp", lambda e, wi=wi, hc=hc, g_=g_: e.dma_start(out=w1[wi][:, 8 * hc:8 * hc + 8, :], in_=k.WB1[g_, :, 8 * hc:8 * hc + 8, :]),
                      reads=[k.b_WB], writes=[b_w1[wi]])

        def load_w2(e_):
            wj = e_ % 2
            for hf in range(2):
                src = k.w_dn[l, e_].rearrange("(c p) n -> p c n", p=128)[:, 4 * hf:4 * hf + 4, :]
                p.dma("pool", lambda e, wj=wj, hf=hf, src=src: e.dma_start(out=w2[wj][:, 4 * hf:4 * hf + 4, :], in_=src), writes=[b_w2[wj]])

        def load_acts(e_):
            p.dma("sp", lambda e: e.dma_start(out=xs[:, :, :], in_=k.XS[e_ * CAP:(e_ + 1) * CAP, :].rearrange("(j p) f -> p j f", p=128)),
                  reads=[b_XS], writes=[b_xs])

        def load_b2(e_):
            p.dma("sp", lambda e: e.dma_start(out=b2[e_ % 2][:, :], in_=k.b_dn[l, e_:e_ + 1, :].broadcast_to([128, D])), writes=[b_b2[e_ % 2]])

        st = {"ev": 0, "yo": 0, "tmp": 0, "dn": 0}
        st["slab"] = 0

        def ensure_slabs(upto):
            while st["slab"] <= min(upto, 4 * NE - 1):
                load_slab(st["slab"])
                st["slab"] += 1

        ensure_slabs(3)
        load_w2(0)
        load_acts(0)
        load_b2(0)
        load_b2(1)
        PTB = k.F[6][:, 0:256].bitcast(BF16)

        def emit_transposes(e_):
            for j in range(3):
                for cg in range(4):
                    if st["ev"] % 2 == 0:
                        TB, bTB = k.PT, k.b_PT
                    else:
                        TB, bTB = PTB, k.b_F[6]
                    for i in range(4):
                        c = 4 * cg + i
                        p.op("pe", lambda e, j=j, c=c, i=i, TB=TB: e.transpose(TB[:, i * 128:(i + 1) * 128], xs[:, j, c * 128:(c + 1) * 128], cs["c_identb"][:, :]),
                             reads=[b_xs, k.b_c], writes=[bTB], inc=(i == 3))
                    src = TB[:, :].rearrange("p (a t) -> p a t", a=4)
                    dst = xeT[:, 4 * cg:4 * cg + 4, j * 128:(j + 1) * 128]
                    if st["ev"] % 2 == 0:
                        p.op("act", lambda e, src=src, dst=dst: e.activation(out=dst, in_=src, func=AF.Copy), reads=[bTB], writes=[b_xeT])
                    else:
                        p.op("dve", lambda e, src=src, dst=dst: e.tensor_copy(dst, src), reads=[bTB], writes=[b_xeT])
                    st["ev"] += 1
            if e_ + 1 < NE:
                load_acts(e_ + 1)

        emit_transposes(0)
        for e_ in range(NE):
            hb = e_ % 2
            for jj in range(8):
                half = jj // 4
                if jj == 0:
                    ensure_slabs(4 * e_ + 3)
                    if e_ + 1 < NE:
                        load_w2(e_ + 1)
                elif jj == 4:
                    ensure_slabs(4 * e_ + 5)
                co = (jj % 4) * 128
                Gb, Ub = jj % 2, 2 + jj % 2
                for gu, bank in ((0, Gb), (1, Ub)):
                    wi = (4 * e_ + 2 * half + gu) % NW1
                    for c in range(16):
                        p.op("pe", lambda e, c=c, wi=wi, bank=bank: e.matmul(k.F[bank][:, 0:NS], lhsT=w1[wi][:, c, co:co + 128], rhs=xeT[:, c, :],
                                                                         start=(c == 0), stop=(c == 15)),
                             reads=[b_w1[wi], b_xeT], writes=[k.b_F[bank]], inc=(c == 15))
                ti = st["tmp"] % NT
                st["tmp"] += 1
                T = [b_tmp[ti]]
                p.op("dve", lambda e: e.tensor_scalar(out=gc[ti][:, :], in0=k.F[Gb][:, 0:NS], scalar1=b1[:, e_, jj:jj + 1], scalar2=7.0, op0=ALU.add, op1=ALU.min),
                     reads=[k.b_F[Gb], b_b1], writes=T)
                p.op("act", lambda e: e.activation(out=sg[ti][:, :], in_=gc[ti][:, :], func=AF.Sigmoid, scale=1.702), writes=T)
                p.op("dve", lambda e: e.tensor_scalar(out=uc[ti][:, :], in0=k.F[Ub][:, 0:NS], scalar1=b1[:, e_, 8 + jj:9 + jj], scalar2=7.0, op0=ALU.add, op1=ALU.min),
                     reads=[k.b_F[Ub], b_b1], writes=T)
                p.op("dve", lambda e: e.tensor_scalar(out=uc[ti][:, :], in0=uc[ti][:, :], scalar1=-7.0, scalar2=1.0, op0=ALU.max, op1=ALU.add), writes=T)
                p.op("dve", lambda e: e.tensor_tensor(out=gc[ti][:, :], in0=gc[ti][:, :], in1=sg[ti][:, :], op=ALU.mult), writes=T)
                p.op("dve", lambda e: e.tensor_tensor(out=hT[hb][:, jj, :], in0=gc[ti][:, :], in1=uc[ti][:, :], op=ALU.mult), reads=T, writes=[b_hT[hb]])
            if e_ + 1 < NE:
                emit_transposes(e_ + 1)
            wj = e_ % 2
            for j in range(3):
                for q in range(4):
                    bank = 4 + st["dn"] % 2
                    st["dn"] += 1
                    for jj in range(8):
                        p.op("pe", lambda e, jj=jj, j=j, q=q, bank=bank: e.matmul(k.F[bank][:, :], lhsT=hT[hb][:, jj, j * 128:(j + 1) * 128], rhs=w2[wj][:, jj, q * 512:(q + 1) * 512],
                                                                               start=(jj == 0), stop=(jj == 7)),
                             reads=[b_hT[hb], b_w2[wj]], writes=[k.b_F[bank]], inc=(jj == 7))
                    yi = st["yo"] % NY
                    st["yo"] += 1
                    p.op("dve", lambda e, q=q, bank=bank, yi=yi: e.tensor_tensor(out=yo[yi][:, :], in0=k.F[bank][:, :], in1=b2[wj][:, q * 512:(q + 1) * 512], op=ALU.add),
                         reads=[k.b_F[bank], b_b2[wj]], writes=[b_yo[yi]])
                    r0 = e_ * CAP + j * 128
                    p.dma("sp", lambda e, q=q, yi=yi, r0=r0: e.dma_start(out=k.YS[r0:r0 + 128, q * 512:(q + 1) * 512], in_=yo[yi][:, :]), reads=[b_yo[yi]], writes=[b_YS])
            if e_ + 2 < NE:
                load_b2(e_ + 2)
        p.end_stage()


def stage_ln2(k, l, dst, last):
    p = k.p
    nc = k.nc
    with ExitStack() as es:
        sb = lambda name, shape, d: es.enter_context(nc.sbuf_tensor("L%d_" % l + name, shape, d))
        yk = [[sb("l_yk%d_%d" % (s, i), [128, D], F32) for i in range(4)] for s in range(2)]
        b_yk = [[p.buf("l_yk%d_%d" % (s, i)) for i in range(4)] for s in range(2)]
        x1t = [sb("l_x1t%d" % i, [128, D], F32) for i in range(2)]
        b_x1t = [p.buf("l_x1t%d" % i) for i in range(2)]
        u = sb("l_u", [128, D], F32)
        b_u = p.buf("l_u")
        x2 = [sb("l_x2_%d" % i, [128, D], F32) for i in range(2)]
        b_x2 = [p.buf("l_x2_%d" % i) for i in range(2)]
        lng = sb("l_lng", [128, D], F32)
        lnb = sb("l_lnb", [128, D], F32)
        b_par = p.buf("l_par")
        st = sb("l_st", [128, 8], F32)
        lnsb = {"st": st, "b_st": p.buf("l_st")}
        b_YS = p.buf("YS_r")
        b_x1res = p.buf("x1res_r")
        b_dst = p.buf("dst")
        print("ln2 stage: sbuf bytes remaining:", nc.sbuf_bytes_remaining)
        p.dma("sp", lambda e: e.dma_start(out=lng[:, :], in_=k.ln2_g[l:l + 1, :].broadcast_to([128, D])), writes=[b_par])
        p.dma("sp", lambda e: e.dma_start(out=lnb[:, :], in_=k.ln2_b[l:l + 1, :].broadcast_to([128, D])), writes=[b_par])
        for tb in range(16):
            s2 = tb % 2
            t0, t1 = tb * 128, (tb + 1) * 128
            for kk in range(4):
                p.dma("pool", lambda e, kk=kk: e.indirect_dma_start(out=yk[s2][kk][:, :], out_offset=None, in_=k.YS[:, :],
                                                                   in_offset=bass.IndirectOffsetOnAxis(ap=k.idx[:, tb, kk:kk + 1], axis=0),
                                                                   bounds_check=k.bc_reg, oob_is_err=False),
                      reads=[b_YS, k.b_route], writes=[b_yk[s2][kk]])
            p.dma("sp", lambda e: e.dma_start(out=x1t[s2][:, :], in_=k.x1res[t0:t1, :]), reads=[b_x1res], writes=[b_x1t[s2]])
            p.op("act", lambda e: e.activation(out=u[:, :], in_=x1t[s2][:, :], func=AF.Copy, scale=ALPHA), reads=[b_x1t[s2]], writes=[b_u])
            for kk in range(4):
                p.op("dve", lambda e, kk=kk: e.scalar_tensor_tensor(out=u[:, :], in0=yk[s2][kk][:, :], scalar=k.gk[:, tb, kk:kk + 1], in1=u[:, :], op0=ALU.mult, op1=ALU.add),
                     reads=[b_yk[s2][kk], k.b_route], writes=[b_u])
            xo, b_xo = x2[s2], b_x2[s2]
            ln_block(k, lnsb, u, b_u, xo, b_xo, lng, lnb, b_par)
            p.dma("sp", lambda e: e.dma_start(out=dst[t0:t1, :], in_=xo[:, :]), reads=[b_xo], writes=[b_dst])
            if not last:
                transpose_block(k, xo, b_xo, tb)
        p.end_stage()


_NC_CACHE = {}


def _host_layout(inputs):
    f32 = lambda a: np.ascontiguousarray(np.asarray(a, dtype=np.float32))
    m = {}
    m["w_in"] = f32(inputs["w_in"])
    m["w_out"] = f32(inputs["w_out"])
    gm = np.concatenate([np.asarray(inputs["g_mix_a"], np.float32), np.asarray(inputs["g_mix_b"], np.float32)], axis=1)
    m["gmix"] = np.ascontiguousarray(gm.reshape(NL, 32, 64).transpose(0, 2, 1))
    for n in ["ln1_g", "ln1_b", "ln2_g", "ln2_b", "w_router", "b_router", "b_down", "w_gate_up", "w_down"]:
        m[n] = f32(inputs[n])
    m["b_gu"] = np.ascontiguousarray(np.asarray(inputs["b_gate_up"], np.float32).reshape(NL, NE, 16, 128).transpose(0, 3, 1, 2))
    m.update(make_consts())
    return m


def kernel(**inputs):
    x = np.asarray(inputs["x"], dtype=np.float32)
    nb = x.shape[0]
    shared = _host_layout(inputs)
    if "nc" not in _NC_CACHE:
        _NC_CACHE["nc"] = build(n_layers=NL)
    nc = _NC_CACHE["nc"]
    in_maps = []
    for b in range(nb):
        m = dict(shared)
        m["x"] = np.ascontiguousarray(x[b])
        in_maps.append(m)
    res = run_bass_kernel_spmd(nc, in_maps, core_ids=list(range(nb)))
    out = np.stack([np.asarray(r["out"], dtype=np.float32) for r in res.results], axis=0)
    return out
```

```python
import math
from contextlib import ExitStack

import numpy as np
import ml_dtypes

import concourse.bass as bass
import concourse.mybir as mybir
from concourse.bass_utils import run_bass_kernel_spmd

F32 = mybir.dt.float32
BF16 = mybir.dt.bfloat16
I32 = mybir.dt.int32
AF = mybir.ActivationFunctionType
ALU = mybir.AluOpType
AX = mybir.AxisListType

NL = 4
S = 2048
D = 2048
HD = 64
NH = 16
WA = 1024
NE = 32
CAP = 384
NSLOT = NE * CAP
DFF = 1024
ALPHA = (2.0 * NL) ** 0.25
PAT = ((128, 1), (512, 4), (2048, 16))
BIGD = 30000.0
NEGM = -30000.0
SAME_ENG_SYNC = False


class Buf:
    __slots__ = ("name", "w", "r", "dkey", "excl")

    def __init__(self, name, excl=False):
        self.name = name
        self.w = None
        self.r = {}
        self.dkey = None
        self.excl = excl


class Prog:
    def __init__(self, nc, es, n_dsem=90):
        self.nc = nc
        self.E = {"pe": nc.tensor, "act": nc.scalar, "dve": nc.vector, "pool": nc.gpsimd, "sp": nc.sync}
        self.semobj = {}
        self.cnt = {}
        for k in ["pe", "act", "dve", "pool"]:
            self.semobj[("e", k)] = es.enter_context(nc.semaphore("e_" + k))
            self.cnt[k] = 0
        self.seen = {k: {} for k in self.E}
        self.dfree = []
        self.dval = {}
        for i in range(n_dsem):
            key = ("d", i)
            self.semobj[key] = es.enter_context(nc.semaphore("d%d" % i))
            self.dval[key] = 0
            self.dfree.append(key)
        self.stage_bufs = []
        self.defer = set()

    def buf(self, name):
        b = Buf(name)
        self.stage_bufs.append(b)
        return b

    def _deps(self, eng, reads, writes):
        deps = {}
        me = ("e", eng)

        def add(k, v):
            if v > deps.get(k, 0):
                deps[k] = v

        for b in reads:
            if b.w is not None:
                add(*b.w)
        for b in writes:
            if b.w is not None:
                add(*b.w)
            for k, v in b.r.items():
                if k != me:
                    add(k, v)
        out = []
        seen = self.seen[eng]
        for k, v in deps.items():
            if k == me and eng == "pe":
                continue
            if k[0] == "e":
                assert v <= self.cnt[k[1]], ("wait on not-yet-emitted inc", eng, k, v, self.cnt[k[1]])
            if seen.get(k, 0) < v:
                seen[k] = v
                out.append((k, v))
        return out

    def _emit_waits(self, eng, waits):
        e = self.E[eng]
        for k, v in waits:
            e.wait_ge(self.semobj[k], v)

    def op(self, eng, fn, reads=(), writes=(), inc=True):
        if any(b.excl for b in reads):
            writes = list(writes) + [b for b in reads if b.excl]
            reads = [b for b in reads if not b.excl]
        self._emit_waits(eng, self._deps(eng, reads, writes))
        ins = fn(self.E[eng])
        if inc:
            self.cnt[eng] += 1
            ins.then_inc(self.semobj[("e", eng)], 1)
            tok = (("e", eng), self.cnt[eng])
        else:
            tok = (("e", eng), self.cnt[eng] + 1)
        for b in reads:
            if b.r.get(tok[0], 0) < tok[1]:
                b.r[tok[0]] = tok[1]
        for b in writes:
            b.w = tok
            b.r = {}
        return tok

    def dma(self, q, fn, reads=(), writes=()):
        self._emit_waits(q, self._deps(q, reads, writes))
        wb = writes[0]
        if wb.dkey is None:
            assert self.dfree, "out of DMA semaphores"
            wb.dkey = self.dfree.pop()
        key = wb.dkey
        ins = fn(self.E[q])
        self.dval[key] += 16
        ins.then_inc(self.semobj[key], 16)
        tok = (key, self.dval[key])
        for b in reads:
            if b.r.get(tok[0], 0) < tok[1]:
                b.r[tok[0]] = tok[1]
        for b in writes:
            b.w = tok
            b.r = {}
        return tok

    def barrier(self, engines=("pe", "act", "dve", "pool", "sp")):
        allv = {}
        for k in ["pe", "act", "dve", "pool"]:
            if self.cnt[k] > 0:
                allv[("e", k)] = self.cnt[k]
        for key, v in self.dval.items():
            if v > 0 and key not in self.defer:
                allv[key] = v
        for eng in engines:
            seen = self.seen[eng]
            for k, v in allv.items():
                if k == ("e", eng):
                    continue
                if seen.get(k, 0) < v:
                    seen[k] = v
                    self.E[eng].wait_ge(self.semobj[k], v)

    def end_stage(self):
        self.barrier()
        for b in self.stage_bufs:
            if b.dkey is not None:
                self.dfree.append(b.dkey)
                b.dkey = None
        self.stage_bufs = []


def make_consts():
    c = {}
    k = np.arange(128)[:, None]
    q = np.arange(128)[None, :]
    cur = np.where(q >= k, (q - k).astype(np.float32), BIGD)
    prv = np.where(k >= q, (q + 128 - k).astype(np.float32), BIGD)
    c["c_dm"] = np.concatenate([cur, prv], axis=1).astype(np.float32)
    c["c_identf"] = np.eye(128, dtype=np.float32)
    c["c_identb"] = np.eye(128, dtype=np.float32).astype(ml_dtypes.bfloat16)
    qq = np.arange(512)[None, :]
    neg = np.stack([np.where(128 * o + k < qq, 0.0, NEGM) for o in range(4)], axis=1)
    c["c_neg"] = neg.astype(ml_dtypes.bfloat16)
    kp = np.arange(128)[:, None]
    kk = np.arange(128)[None, :]
    c["c_negtri"] = np.where(kp >= kk, -1.0, 0.0).astype(ml_dtypes.bfloat16)
    c["c_negones"] = np.full((128, 128), -1.0).astype(ml_dtypes.bfloat16)
    c["c_ones"] = np.ones((128, 128), np.float32).astype(ml_dtypes.bfloat16)
    c["c_tstrict"] = np.where(kp < kk, 1.0, 0.0).astype(ml_dtypes.bfloat16)
    sel = np.zeros((128, 64), np.float32)
    sel[64, :] = 1.0
    c["c_sel65"] = sel
    c["c_onesf"] = np.ones((128, 8), np.float32)
    c["c_ecap"] = np.tile((np.arange(NE) * CAP).astype(np.float32)[None, :], (128, 1))
    return c


CONST_SPECS = {
    "c_dm": ([128, 256], F32), "c_identf": ([128, 128], F32), "c_identb": ([128, 128], BF16),
    "c_neg": ([128, 4, 512], BF16), "c_negtri": ([128, 128], BF16), "c_negones": ([128, 128], BF16),
    "c_ones": ([128, 128], BF16), "c_tstrict": ([128, 128], BF16), "c_sel65": ([128, 64], F32),
    "c_onesf": ([128, 8], F32), "c_ecap": ([128, NE], F32),
}


class K:
    pass


def build(n_layers=NL, stages=("attn", "oproj", "moe", "ln2"), debug=()):
    nc = bass.Bass("TRN2", target_bir_lowering=False)
    k = K()
    k.nc = nc
    k.debug = set(debug)
    dt = nc.dram_tensor
    k.x_in = dt("x", [S, D], F32, kind="ExternalInput").ap()
    k.w_in = dt("w_in", [NL, D, 6144], F32, kind="ExternalInput").ap()
    k.w_out = dt("w_out", [NL, D, D], F32, kind="ExternalInput").ap()
    k.gmix = dt("gmix", [NL, 64, 32], F32, kind="ExternalInput").ap()
    k.ln1_g = dt("ln1_g", [NL, D], F32, kind="ExternalInput").ap()
    k.ln1_b = dt("ln1_b", [NL, D], F32, kind="ExternalInput").ap()
    k.ln2_g = dt("ln2_g", [NL, D], F32, kind="ExternalInput").ap()
    k.ln2_b = dt("ln2_b", [NL, D], F32, kind="ExternalInput").ap()
    k.w_router = dt("w_router", [NL, D, NE], F32, kind="ExternalInput").ap()
    k.b_router = dt("b_router", [NL, NE], F32, kind="ExternalInput").ap()
    if "moe" in stages:
        k.w_gu = dt("w_gate_up", [NL, NE, D, 2 * DFF], F32, kind="ExternalInput").ap()
        k.w_dn = dt("w_down", [NL, NE, DFF, D], F32, kind="ExternalInput").ap()
    k.b_gu = dt("b_gu", [NL, 128, NE, 16], F32, kind="ExternalInput").ap()
    k.b_dn = dt("b_down", [NL, NE, D], F32, kind="ExternalInput").ap()
    k.cdram = {n: dt(n, shp, d, kind="ExternalInput").ap() for n, (shp, d) in CONST_SPECS.items()}
    k.out = dt("out", [S, D], F32, kind="ExternalOutput").ap()
    k.xres = dt("xres", [S, D], F32, kind="Internal").ap()
    k.x1res = dt("x1res", [S, D], F32, kind="Internal").ap()
    k.mixT = dt("mixT", [16, 128, S], BF16, kind="Internal").ap()
    k.XS = dt("XS", [NSLOT, D], BF16, kind="Internal").ap()
    k.YS = dt("YS", [NSLOT, D], F32, kind="Internal").ap()
    k.WB1 = dt("WB1", [NE * 4, 128, 16, 512], BF16, kind="Internal").ap()
    k.dbg = {}
    if "mix" in k.debug:
        k.dbg["mixT_o"] = dt("mixT_o", [16, 128, S], BF16, kind="ExternalOutput").ap()
        k.dbg["ss_o"] = dt("ss_o", [128, 32], F32, kind="ExternalOutput").ap()
    if "x1" in k.debug:
        k.dbg["x1_o"] = dt("x1_o", [S, D], F32, kind="ExternalOutput").ap()
        k.dbg["idx_o"] = dt("idx_o", [128, 64], I32, kind="ExternalOutput").ap()
        k.dbg["gk_o"] = dt("gk_o", [128, 64], F32, kind="ExternalOutput").ap()
    if "xs" in k.debug:
        k.dbg["xs_o"] = dt("xs_o", [NSLOT, D], BF16, kind="ExternalOutput").ap()
    if "ys" in k.debug:
        k.dbg["ys_o"] = dt("ys_o", [NSLOT, D], F32, kind="ExternalOutput").ap()

    with ExitStack() as es:
        p = Prog(nc, es)
        k.p = p
        sb = lambda name, shape, d: es.enter_context(nc.sbuf_tensor(name, shape, d))
        ps = lambda name, shape, d: es.enter_context(nc.psum_tensor(name, shape, d))
        k.xT = sb("xT", [128, 16, S], BF16)
        k.b_xT = Buf("xT")
        k.cs = {}
        k.b_c = Buf("consts")
        for n, (shp, d) in CONST_SPECS.items():
            k.cs[n] = sb("s_" + n, shp, d)
        k.ss = sb("ss", [128, 32], F32)
        k.b_ss = Buf("ss")
        k.idx = sb("idx", [128, 16, 4], I32)
        k.gk = sb("gk", [128, 16, 4], F32)
        k.b_route = Buf("route")
        k.b_WB = Buf("WB1")
        k.b_WB.dkey = p.dfree.pop()
        p.defer.add(k.b_WB.dkey)
        k.has_moe = "moe" in stages
        k.F = [ps("F%d" % i, [128, 512], F32) for i in range(7)]
        k.b_F = [Buf("F%d" % i, excl=True) for i in range(7)]
        k.PX = ps("PX", [128, 512], F32)
        k.PT = k.PX[:, 0:256].bitcast(BF16)
        k.b_PT = Buf("PX", excl=True)
        k.PS = k.PX[:, 256:512]
        k.b_PS = k.b_PT
        print("PT shape", k.PT.shape, "PS shape", k.PS.shape)

        k.bc_reg = nc.gpsimd.alloc_register("bc_reg")
        nc.gpsimd.reg_mov(k.bc_reg, NSLOT - 1)
        first = True
        for n in CONST_SPECS:
            src = k.cdram[n]
            dst = k.cs[n]
            if len(CONST_SPECS[n][0]) == 3:
                p.dma("sp", lambda e, d_=dst, s_=src: e.dma_start(out=d_[:, :, :], in_=s_[:, :, :]), writes=[k.b_c])
            else:
                p.dma("sp", lambda e, d_=dst, s_=src: e.dma_start(out=d_[:, :], in_=s_[:, :]), writes=[k.b_c])
        print("sbuf bytes remaining after persistent:", nc.sbuf_bytes_remaining)

        stage_prologue(k)
        xcur = k.x_in
        for l in range(n_layers):
            last = (l == n_layers - 1)
            if "attn" in stages:
                stage_attn(k, l)
            if "mix" in k.debug and l == 0:
                dump_dram(k, k.dbg["mixT_o"].rearrange("c p t -> (c p) t"), k.mixT.rearrange("c p t -> (c p) t"), 2048, BF16, 2048)
                dump_sbuf(k, k.dbg["ss_o"], k.ss, k.b_ss)
            if "oproj" in stages:
                stage_oproj(k, l, xcur)
            if "x1" in k.debug and l == 0:
                dump_dram(k, k.dbg["x1_o"], k.x1res, 2048, F32, 2048)
                dump_sbuf(k, k.dbg["idx_o"], k.idx[:, :, :].rearrange("p a b -> p (a b)"), k.b_route, raw=True)
                dump_sbuf(k, k.dbg["gk_o"], k.gk[:, :, :].rearrange("p a b -> p (a b)"), k.b_route, raw=True)
            if "xs" in k.debug and l == 0:
                dump_dram(k, k.dbg["xs_o"], k.XS, NSLOT, BF16, 2048)
            if "moe" in stages:
                stage_moe(k, l)
            if "ys" in k.debug and l == 0:
                dump_dram(k, k.dbg["ys_o"], k.YS, NSLOT, F32, 2048)
            if "ln2" in stages:
                stage_ln2(k, l, k.out if last else k.xres, last)
            xcur = k.xres
        p.barrier()
    return nc


def dump_dram(k, dst, src, rows, dtp, cols):
    p = k.p
    nc = k.nc
    p.barrier()
    k.ndump = getattr(k, "ndump", 0) + 1
    with nc.sbuf_tensor("dump_t%d" % k.ndump, [128, cols], dtp) as t:
        bt = Buf("dump_t")
        bo = Buf("dump_o")
        for r0 in range(0, rows, 128):
            p.dma("sp", lambda e, r0=r0: e.dma_start(out=t[:, :], in_=src[r0:r0 + 128, :]), writes=[bt])
            p.dma("sp", lambda e, r0=r0: e.dma_start(out=dst[r0:r0 + 128, :], in_=t[:, :]), reads=[bt], writes=[bo])
        p.barrier()
        if bt.dkey is not None:
            p.dfree.append(bt.dkey)
        if bo.dkey is not None:
            p.dfree.append(bo.dkey)


def dump_sbuf(k, dst, src, b, raw=False):
    p = k.p
    p.barrier()
    bo = Buf("dump_o2")
    if raw:
        p.dma("sp", lambda e: e.dma_start(out=dst[:, :], in_=src), reads=[b], writes=[bo])
    else:
        p.dma("sp", lambda e: e.dma_start(out=dst[:, :], in_=src[:, :]), reads=[b], writes=[bo])
    p.barrier()
    p.dfree.append(bo.dkey)


def transpose_block(k, src, b_src, tb, xT_dst=True, f32_dst=None, b_f32=None, fbanks=(0, 1), evac=("act", "dve")):
    p = k.p
    identf = k.cs["c_identf"]
    for g in range(4):
        fb = fbanks[g % len(fbanks)]
        F = k.F[fb]
        bF = k.b_F[fb]
        for i in range(4):
            c = 4 * g + i
            p.op("pe", lambda e, c=c, i=i, F=F: e.transpose(F[:, i * 128:(i + 1) * 128], src[:, c * 128:(c + 1) * 128], identf[:, :]),
                 reads=[b_src, k.b_c], writes=[bF], inc=(i == 3))
        Fv = F[:, :].rearrange("p (a t) -> p a t", a=4)
        if xT_dst:
            eng = evac[g % len(evac)]
            if eng == "act":
                p.op("act", lambda e, g=g, Fv=Fv: e.activation(out=k.xT[:, 4 * g:4 * g + 4, tb * 128:(tb + 1) * 128], in_=Fv, func=AF.Copy),
                     reads=[bF], writes=[k.b_xT])
            else:
                p.op("dve", lambda e, g=g, Fv=Fv: e.tensor_copy(k.xT[:, 4 * g:4 * g + 4, tb * 128:(tb + 1) * 128], Fv),
                     reads=[bF], writes=[k.b_xT])
        if f32_dst is not None:
            p.op("dve", lambda e, g=g, Fv=Fv: e.tensor_copy(f32_dst[:, 4 * g:4 * g + 4, :], Fv), reads=[bF], writes=[b_f32])


def stage_prologue(k):
    p = k.p
    nc = k.nc
    with ExitStack() as es:
        xt = [es.enter_context(nc.sbuf_tensor("pro_x%d" % i, [128, D], F32)) for i in range(3)]
        bx = [p.buf("pro_x%d" % i) for i in range(3)]
        zt = es.enter_context(nc.sbuf_tensor("pro_z", [128, D], BF16))
        bz = p.buf("pro_z")
        bxs = p.buf("XS_init")
        p.op("pool", lambda e: e.memset(zt[:, :], 0.0), writes=[bz])
        for r0 in range(0, NSLOT, 128):
            p.dma("pool", lambda e, r0=r0: e.dma_start(out=k.XS[r0:r0 + 128, :], in_=zt[:, :]), reads=[bz], writes=[bxs])
        for tb in range(16):
            t = xt[tb % 3]
            b = bx[tb % 3]
            p.dma("sp", lambda e, t=t, tb=tb: e.dma_start(out=t[:, :], in_=k.x_in[tb * 128:(tb + 1) * 128, :]), writes=[b])
            transpose_block(k, t, b, tb)
        p.end_stage()


def stage_attn(k, l):
    p = k.p
    nc = k.nc
    cs = k.cs
    with ExitStack() as es:
        sb = lambda name, shape, d: es.enter_context(nc.sbuf_tensor("L%d_" % l + name, shape, d))
        NSET = 2
        wsl = [[sb("a_w%d_%d" % (s, j), [128, 16, 128], BF16) for j in range(3)] for s in range(NSET)]
        b_wsl = [[p.buf("a_w%d_%d" % (s, j)) for j in range(3)] for s in range(NSET)]
        qkvT = [[sb("a_qkv%d_%d" % (s, j), [128, S], BF16) for j in range(3)] for s in range(NSET)]
        b_qkvT = [[p.buf("a_qkv%d_%d" % (s, j)) for j in range(3)] for s in range(NSET)]
        Vr = [[sb("a_vr%d_%d" % (s, r), [128, 16, 2, 65], BF16) for r in range(3)] for s in range(NSET)]
        b_Vr = [[p.buf("a_vr%d_%d" % (s, r)) for r in range(3)] for s in range(NSET)]
        acc = [sb("a_acc%d" % i, [128, S], F32) for i in range(2)]
        b_acc = [p.buf("a_acc%d" % i) for i in range(2)]
        NW = 4
        Pe = [sb("a_pe%d" % i, [128, 256], F32) for i in range(NW)]
        b_Pe = [p.buf("a_pe%d" % i) for i in range(NW)]
        Pm = [sb("a_pm%d" % i, [128, 256], BF16) for i in range(NW)]
        b_Pm = [p.buf("a_pm%d" % i) for i in range(NW)]
        Md = [sb("a_md%d" % i, [128, 256], F32) for i in range(2)]
        b_Md = [p.buf("a_md%d" % i) for i in range(2)]
        e32 = [sb("b_e%d" % i, [128, 512], F32) for i in range(2)]
        b_e32 = [p.buf("b_e%d" % i) for i in range(2)]
        spt = [sb("b_sp%d" % i, [128, 512], BF16) for i in range(3)]
        b_spt = [p.buf("b_sp%d" % i) for i in range(3)]
        ss32 = [sb("b_ss%d" % i, [128, 512], F32) for i in range(2)]
        b_ss32 = [p.buf("b_ss%d" % i) for i in range(2)]
        ssb = [sb("b_ssb%d" % i, [128, 512], BF16) for i in range(3)]
        b_ssb = [p.buf("b_ssb%d" % i) for i in range(3)]
        at = [sb("b_a%d" % i, [128, 512], BF16) for i in range(3)]
        b_at = [p.buf("b_a%d" % i) for i in range(3)]
        oaf = [sb("f_oaf%d" % i, [64, 512], F32) for i in range(2)]
        b_oaf = [p.buf("f_oaf%d" % i) for i in range(2)]
        oab = [sb("f_oab%d" % i, [64, 512], BF16) for i in range(2)]
        b_oab = [p.buf("f_oab%d" % i) for i in range(2)]
        sq = [sb("f_sq%d" % i, [64, 512], F32) for i in range(2)]
        b_sq = [p.buf("f_sq%d" % i) for i in range(2)]
        b_mixT = p.buf("mixT")
        gmt = sb("a_gmt", [64, 32], F32)
        b_gmt = p.buf("a_gmt")
        p.dma("sp", lambda e: e.dma_start(out=gmt[:, :], in_=k.gmix[l, :, :]), writes=[b_gmt])
        print("attn stage: sbuf bytes remaining:", nc.sbuf_bytes_remaining)

        for s in range(NSET):
            for r in range(3):
                p.op("pool", lambda e, s=s, r=r: e.memset(Vr[s][r][:, :, :, 64:65], 1.0), writes=[b_Vr[s][r]])
        p.op("pool", lambda e: e.memset(k.ss[:, :], 0.0), writes=[k.b_ss])

        st = {"fin": 0, "pe": 0, "pm": 0, "md": 0}

        def col0(pi, j):
            base = 0 if pi < 8 else 3 * WA
            return base + j * WA + (pi % 8) * 128

        def bg_pair(pi):
            s = pi % NSET
            for j in range(3):
                c0 = col0(pi, j)
                src = k.w_in[l].rearrange("(c p) n -> p c n", p=128)[:, :, c0:c0 + 128]
                p.dma("pool", lambda e, s=s, j=j, src=src: e.dma_start(out=wsl[s][j][:, :, :], in_=src), writes=[b_wsl[s][j]])
            if k.has_moe:
                for e_ in (2 * pi, 2 * pi + 1):
                    for s_ in range(4):
                        gu, half = s_ % 2, s_ // 2
                        c0 = gu * DFF + 512 * half
                        srcw = k.w_gu[l, e_].rearrange("(c p) n -> p c n", p=128)[:, :, c0:c0 + 512]
                        for hc in range(2):
                            p.dma("pool", lambda e, e_=e_, s_=s_, hc=hc, srcw=srcw: e.dma_start(out=k.WB1[4 * e_ + s_, :, 8 * hc:8 * hc + 8, :], in_=srcw[:, 8 * hc:8 * hc + 8, :]),
                                  writes=[k.b_WB])
            yield
            gi = 0
            for j in range(3):
                for n in range(4):
                    fb = gi % 2
                    gi += 1
                    F = k.F[fb]
                    bF = k.b_F[fb]
                    for c in range(16):
                        p.op("pe", lambda e, c=c, F=F, j=j, n=n: e.matmul(F[:, :], lhsT=wsl[s][j][:, c, :], rhs=k.xT[:, c, n * 512:(n + 1) * 512],
                                                                        start=(c == 0), stop=(c == 15)),
                             reads=[b_wsl[s][j], k.b_xT], writes=[bF], inc=(c == 15))
                        if c % 4 == 3:
                            yield
                    scale = 0.125 if j == 0 else 1.0
                    if gi % 2 == 0:
                        p.op("act", lambda e, F=F, j=j, n=n, scale=scale: e.activation(out=qkvT[s][j][:, n * 512:(n + 1) * 512], in_=F[:, :], func=AF.Copy, scale=scale),
                             reads=[bF], writes=[b_qkvT[s][j]])
                    else:
                        p.op("dve", lambda e, F=F, j=j, n=n, scale=scale: e.tensor_scalar(out=qkvT[s][j][:, n * 512:(n + 1) * 512], in0=F[:, :], scalar1=scale, scalar2=None, op0=ALU.mult),
                             reads=[bF], writes=[b_qkvT[s][j]])
                    yield
            npat = 3 if pi < 8 else 1
            vT = qkvT[s][2]
            for r in range(npat):
                d = PAT[r][1]
                nblk = S // d // 128
                vv = vT[:, :].rearrange("p (i d) -> p d i", d=d)
                for g in range(4):
                    for i in range(4):
                        B = 4 * g + i
                        c, n = B // nblk, B % nblk
                        p.op("pe", lambda e, i=i, c=c, n=n, vv=vv: e.transpose(k.PT[:, i * 128:(i + 1) * 128], vv[:, c, n * 128:(n + 1) * 128], cs["c_identb"][:, :]),
                             reads=[b_qkvT[s][2], k.b_c], writes=[k.b_PT], inc=(i == 3))
                    p.op("dve", lambda e, g=g, r=r: e.tensor_copy(Vr[s][r][:, 4 * g:4 * g + 4, :, 0:64],
                                                                 k.PT[:, :].rearrange("p (a h d) -> p a h d", a=4, h=2)),
                         reads=[k.b_PT], writes=[b_Vr[s][r]])
                    yield

        def run_all(gen):
            for _ in gen:
                pass

        def step(gen):
            if gen is not None:
                try:
                    next(gen)
                except StopIteration:
                    return None
            return gen

        def finalize(h_glob, n, src_kind, accb=None, b_accb=None, Fsrc=None, b_Fsrc=None):
            i = st["fin"] % 2
            st["fin"] += 1
            if src_kind == "A":
                F6 = k.F[6]
                p.op("pe", lambda e: e.matmul(F6[0:64, :], lhsT=cs["c_sel65"][0:65, :], rhs=accb[0:65, n * 512:(n + 1) * 512], start=True, stop=True),
                     reads=[b_accb, k.b_c], writes=[k.b_F[6]])
                p.op("dve", lambda e: e.reciprocal(out=sq[i][:, :], in_=F6[0:64, :]), reads=[k.b_F[6]], writes=[b_sq[i]])
                p.op("dve", lambda e: e.tensor_tensor(out=oaf[i][:, :], in0=accb[0:64, n * 512:(n + 1) * 512], in1=sq[i][:, :], op=ALU.mult),
                     reads=[b_accb, b_sq[i]], writes=[b_oaf[i]])
            else:
                p.op("dve", lambda e: e.tensor_copy(oaf[i][:, :], Fsrc[0:64, :]), reads=[b_Fsrc], writes=[b_oaf[i]])
            p.op("dve", lambda e: e.tensor_scalar(out=oab[i][:, :], in0=oaf[i][:, :], scalar1=gmt[:, h_glob:h_glob + 1], scalar2=None, op0=ALU.mult),
                 reads=[b_oaf[i], b_gmt], writes=[b_oab[i]])
            p.op("act", lambda e: e.activation(out=sq[i][:, :], in_=oaf[i][:, :], func=AF.Square), reads=[b_oaf[i]], writes=[b_sq[i]])
            for tb in range(4):
                p.op("pe", lambda e, tb=tb: e.matmul(k.PS[:, tb:tb + 1], lhsT=sq[i][:, tb * 128:(tb + 1) * 128], rhs=cs["c_onesf"][0:64, 0:1], start=True, stop=True),
                     reads=[b_sq[i], k.b_c], writes=[k.b_PS], inc=(tb == 3))
            off = 0 if h_glob < 16 else 16
            p.op("dve", lambda e: e.tensor_tensor(out=k.ss[:, off + 4 * n:off + 4 * n + 4], in0=k.ss[:, off + 4 * n:off + 4 * n + 4], in1=k.PS[:, 0:4], op=ALU.add),
                 reads=[k.b_PS, k.b_ss], writes=[k.b_ss])
            ch, ph = h_glob // 2, (h_glob % 2) * 64
            p.dma("sp", lambda e: e.dma_start(out=k.mixT[ch, ph:ph + 64, n * 512:(n + 1) * 512], in_=oab[i][:, :]), reads=[b_oab[i]], writes=[b_mixT])

        def attn_A(pi, bg):
            s = pi % NSET
            qT, kT = qkvT[s][0], qkvT[s][1]
            bq, bk = b_qkvT[s][0], b_qkvT[s][1]
            for hh in range(2):
                h = 2 * pi + hh
                slope = 2.0 ** (-8.0 * (h + 1) / NH)
                accb, b_accb = acc[hh], b_acc[hh]
                r0, r1 = hh * 64, hh * 64 + 64
                for r in range(3):
                    d = PAT[r][1]
                    nblk = S // d // 128
                    mi = st["md"] % 2
                    st["md"] += 1
                    p.op("act", lambda e, mi=mi, d=d: e.activation(out=Md[mi][:, :], in_=cs["c_dm"][:, :], func=AF.Exp, scale=-slope * d),
                         reads=[k.b_c], writes=[b_Md[mi]])
                    qv = qT[r0:r1, :].rearrange("p (i d) -> p d i", d=d)
                    kv = kT[r0:r1, :].rearrange("p (i d) -> p d i", d=d)
                    av = accb[0:65, :].rearrange("p (i d) -> p d i", d=d)
                    tiles = [(B // nblk, B % nblk) for B in range(16)]
                    pendq = []
                    for t in range(18):
                        cur = None
                        if t < 16:
                            c, m = tiles[t]
                            ncols = 256 if m + 1 < nblk else 128
                            sbk = 2 + (t % 2)
                            FS = k.F[sbk]
                            p.op("pe", lambda e, c=c, m=m, ncols=ncols, FS=FS: e.matmul(FS[:, 0:ncols], lhsT=kv[:, c, m * 128:(m + 1) * 128],
                                                                                      rhs=qv[:, c, m * 128:m * 128 + ncols], start=True, stop=True),
                                 reads=[bq, bk], writes=[k.b_F[sbk]])
                            wi = st["pe"] % NW
                            st["pe"] += 1
                            p.op("act", lambda e, wi=wi, ncols=ncols, FS=FS: e.activation(out=Pe[wi][:, 0:ncols], in_=FS[:, 0:ncols], func=AF.Exp),
                                 reads=[k.b_F[sbk]], writes=[b_Pe[wi]])
                            p.op("dve", lambda e, wi=wi, ncols=ncols, mi=mi: e.tensor_tensor(out=Pm[wi][:, 0:ncols], in0=Pe[wi][:, 0:ncols], in1=Md[mi][:, 0:ncols], op=ALU.mult),
                                 reads=[b_Pe[wi], b_Md[mi]], writes=[b_Pm[wi]])
                            cur = (t, c, m, ncols, wi)
                        if cur is not None:
                            pendq.append(cur)
                        if t >= 2 or t >= 16:
                            tt, c, m, ncols, wi = pendq.pop(0)
                            B = tt
                            g = B // 4
                            ob = 4 + (g % 2)
                            FO = k.F[ob]
                            lhs = Vr[s][r][:, B, hh, :]
                            p.op("pe", lambda e, FO=FO, lhs=lhs, wi=wi, B=B, m=m: e.matmul(FO[0:65, (B % 4) * 128:(B % 4) * 128 + 128], lhsT=lhs, rhs=Pm[wi][:, 0:128],
                                                                                       start=(m == 0), stop=True),
                                 reads=[b_Vr[s][r], b_Pm[wi]], writes=[k.b_F[ob]], inc=True)
                            if ncols == 256:
                                B2 = B + 1
                                ob2 = 4 + ((B2 // 4) % 2)
                                FO2 = k.F[ob2]
                                p.op("pe", lambda e, FO2=FO2, lhs=lhs, wi=wi, B2=B2: e.matmul(FO2[0:65, (B2 % 4) * 128:(B2 % 4) * 128 + 128], lhsT=lhs, rhs=Pm[wi][:, 128:256],
                                                                                          start=True, stop=False),
                                     reads=[b_Vr[s][r], b_Pm[wi]], writes=[k.b_F[ob2]], inc=True)
                            if B % 4 == 3:
                                if d == 1:
                                    dst = av[:, 0, g * 512:(g + 1) * 512]
                                    srcv = FO[0:65, :]
                                elif d == 4:
                                    dst = av[:, g, 0:512]
                                    srcv = FO[0:65, :]
                                else:
                                    dst = av[:, 4 * g:4 * g + 4, 0:128]
                                    srcv = FO[0:65, :].rearrange("p (a t) -> p a t", a=4)
                                if r == 0:
                                    p.op("dve", lambda e, dst=dst, srcv=srcv: e.tensor_copy(dst, srcv), reads=[k.b_F[ob]], writes=[b_accb])
                                else:
                                    p.op("dve", lambda e, dst=dst, srcv=srcv: e.tensor_tensor(out=dst, in0=dst, in1=srcv, op=ALU.add), reads=[k.b_F[ob], b_accb], writes=[b_accb])
                            bg = step(bg)
                    assert not pendq
                for n in range(4):
                    finalize(h, n, "A", accb=accb, b_accb=b_accb)
                    bg = step(bg)
            return bg

        def attn_B(pi, bg):
            s = pi % NSET
            qT, kT = qkvT[s][0], qkvT[s][1]
            bq, bk = b_qkvT[s][0], b_qkvT[s][1]
            V0, bV0 = Vr[s][0], b_Vr[s][0]
            tiles = []
            for hh in range(2):
                for m in range(4):
                    for j in range(4 * m + 3, -1, -1):
                        tiles.append((hh, m, j))
            N = len(tiles)
            info = {}

            def PEz(t):
                hh, m, j = tiles[t]
                r0, r1 = hh * 64, hh * 64 + 64
                zb = 2 + (t % 4)
                diag = j >= 4 * m
                p.op("pe", lambda e: e.matmul(k.F[zb][:, :], lhsT=kT[r0:r1, j * 128:(j + 1) * 128], rhs=qT[r0:r1, m * 512:(m + 1) * 512], start=True, stop=False),
                     reads=[bq, bk], writes=[k.b_F[zb]], inc=not diag)
                if diag:
                    p.op("pe", lambda e: e.matmul(k.F[zb][:, :], lhsT=cs["c_identb"][:, :], rhs=cs["c_neg"][:, j - 4 * m, :], start=False, stop=False),
                         reads=[k.b_c], writes=[k.b_F[zb]])

            def ACTe(t):
                zb = 2 + (t % 4)
                ei = t % 2
                p.op("act", lambda e: e.activation(out=e32[ei][:, :], in_=k.F[zb][:, :], func=AF.Exp), reads=[k.b_F[zb]], writes=[b_e32[ei]])

            def ACTsp(t):
                ei = t % 2
                si = t % 3
                p.op("act", lambda e: e.activation(out=spt[si][:, :], in_=e32[ei][:, :], func=AF.Ln, bias=1.0), reads=[b_e32[ei]], writes=[b_spt[si]])

            def SSupd(t):
                hh, m, j = tiles[t]
                if j == 0:
                    return
                first = (j == 4 * m + 3)
                gidx = (hh * 4 + m) % 2
                si = t % 3
                if first:
                    p.op("dve", lambda e: e.tensor_copy(ss32[gidx][:, :], spt[si][:, :]), reads=[b_spt[si]], writes=[b_ss32[gidx]])
                else:
                    p.op("dve", lambda e: e.tensor_tensor(out=ss32[gidx][:, :], in0=ss32[gidx][:, :], in1=spt[si][:, :], op=ALU.add),
                         reads=[b_spt[si], b_ss32[gidx]], writes=[b_ss32[gidx]])
                p.op("dve", lambda e: e.tensor_copy(ssb[si][:, :], ss32[gidx][:, :]), reads=[b_ss32[gidx]], writes=[b_ssb[si]])

            def PEB(t):
                hh, m, j = tiles[t]
                bb = 2 + (t % 4)
                si = t % 3
                first = (j == 4 * m + 3)
                FB = k.F[bb]
                p.op("pe", lambda e: e.matmul(FB[:, :], lhsT=cs["c_negtri"][:, :], rhs=spt[si][:, :], start=False, stop=first),
                     reads=[k.b_c, b_spt[si]], writes=[k.b_F[bb]], inc=first)
                if not first:
                    sp_prev = (t - 1) % 3
                    p.op("pe", lambda e: e.matmul(FB[:, :], lhsT=cs["c_negones"][:, :], rhs=ssb[sp_prev][:, :], start=False, stop=True),
                         reads=[k.b_c, b_ssb[sp_prev]], writes=[k.b_F[bb]], inc=True)

            def ACTa(t):
                bb = 2 + (t % 4)
                ai = t % 3
                p.op("act", lambda e: e.activation(out=at[ai][:, :], in_=k.F[bb][:, :], func=AF.Exp), reads=[k.b_F[bb]], writes=[b_at[ai]])

            def PEav(t):
                hh, m, j = tiles[t]
                ai = t % 3
                first = (j == 4 * m + 3)
                p.op("pe", lambda e: e.matmul(k.F[6][0:64, :], lhsT=V0[:, j, hh, 0:64], rhs=at[ai][:, :], start=first, stop=(j == 0)),
                     reads=[bV0, b_at[ai]], writes=[k.b_F[6]], inc=True)
                if j == 0:
                    finalize(16 + 2 * (pi - 8) + hh, m, "B", Fsrc=k.F[6], b_Fsrc=k.b_F[6])

            for t in range(-1, N + 1):
                if t + 1 < N:
                    PEz(t + 1)
                    ACTe(t + 1)
                if 0 <= t < N:
                    PEB(t)
                if t + 1 < N:
                    ACTsp(t + 1)
                    SSupd(t + 1)
                if 0 <= t < N:
                    ACTa(t)
                if t >= 1:
                    PEav(t - 1)
                    bg = step(bg)
            return bg

        import os
        lim = int(os.environ.get("ATTN_LIMIT", "99"))
        bg = bg_pair(0)
        run_all(bg)
        for pi in range(16):
            if lim == 0 or (lim == 1 and pi >= 1) or (lim == 2 and pi != 8):
                continue
            if lim == 2:
                run_all(bg_pair(8))
            nxt = bg_pair(pi + 1) if pi + 1 < 16 else None
            if pi < 8:
                nxt = attn_A(pi, nxt)
            else:
                nxt = attn_B(pi, nxt)
            if nxt is not None:
                run_all(nxt)
        p.end_stage()


def ln_block(k, sbufs, u, b_u, dst, b_dst, lng, lnb, b_ln):
    p = k.p
    st, b_st = sbufs["st"], sbufs["b_st"]
    p.op("dve", lambda e: e.reduce_sum(out=st[:, 0:1], in_=u[:, :], axis=AX.X), reads=[b_u], writes=[b_st])
    p.op("dve", lambda e: e.memset(st[:, 1:2], 0.0), writes=[b_st])
    p.op("act", lambda e: e.activation(out=dst[:, :], in_=u[:, :], func=AF.Square, accum_out=st[:, 1:2]), reads=[b_u], writes=[b_dst, b_st])
    p.op("dve", lambda e: e.tensor_scalar(out=st[:, 2:3], in0=st[:, 0:1], scalar1=1.0 / D, scalar2=None, op0=ALU.mult), reads=[b_st], writes=[b_st])
    p.op("dve", lambda e: e.tensor_tensor(out=st[:, 3:4], in0=st[:, 2:3], in1=st[:, 2:3], op=ALU.mult), reads=[b_st], writes=[b_st])
    p.op("dve", lambda e: e.scalar_tensor_tensor(out=st[:, 4:5], in0=st[:, 1:2], scalar=1.0 / D, in1=st[:, 3:4], op0=ALU.mult, op1=ALU.subtract),
         reads=[b_st], writes=[b_st])
    p.op("dve", lambda e: e.tensor_scalar(out=st[:, 4:5], in0=st[:, 4:5], scalar1=1e-5, scalar2=None, op0=ALU.add), reads=[b_st], writes=[b_st])
    p.op("act", lambda e: e.activation(out=st[:, 5:6], in_=st[:, 4:5], func=AF.Sqrt), reads=[b_st], writes=[b_st])
    p.op("dve", lambda e: e.reciprocal(out=st[:, 6:7], in_=st[:, 5:6]), reads=[b_st], writes=[b_st])
    p.op("dve", lambda e: e.tensor_scalar(out=dst[:, :], in0=u[:, :], scalar1=st[:, 2:3], scalar2=st[:, 6:7], op0=ALU.subtract, op1=ALU.mult),
         reads=[b_u, b_st], writes=[b_dst])
    p.op("dve", lambda e: e.tensor_tensor(out=dst[:, :], in0=dst[:, :], in1=lng[:, :], op=ALU.mult), reads=[b_dst, b_ln], writes=[b_dst])
    p.op("dve", lambda e: e.tensor_tensor(out=dst[:, :], in0=dst[:, :], in1=lnb[:, :], op=ALU.add), reads=[b_dst, b_ln], writes=[b_dst])


def stage_oproj(k, l, xcur):
    p = k.p
    nc = k.nc
    cs = k.cs
    with ExitStack() as es:
        sb = lambda name, shape, d: es.enter_context(nc.sbuf_tensor("L%d_" % l + name, shape, d))
        wout = k.xT
        b_wout = k.b_xT
        gm = sb("o_gm", [128, 16], F32)
        lng = sb("o_lng", [128, D], F32)
        lnb = sb("o_lnb", [128, D], F32)
        wr = sb("o_wr", [128, 16, NE], F32)
        brt = sb("o_brt", [128, NE], F32)
        b_par = p.buf("o_par")
        mixt = [sb("o_mixt%d" % i, [128, 16, 128], BF16) for i in range(2)]
        b_mixt = [p.buf("o_mixt%d" % i) for i in range(2)]
        xr = [sb("o_xr%d" % i, [128, D], F32) for i in range(2)]
        b_xr = [p.buf("o_xr%d" % i) for i in range(2)]
        u = sb("o_u", [128, D], F32)
        b_u = p.buf("o_u")
        x1f = [sb("o_x1f%d" % i, [128, D], F32) for i in range(2)]
        b_x1f = [p.buf("o_x1f%d" % i) for i in range(2)]
        x1b = [sb("o_x1b%d" % i, [128, D], BF16) for i in range(2)]
        b_x1b = [p.buf("o_x1b%d" % i) for i in range(2)]
        x1T = sb("o_x1T", [128, 16, 128], F32)
        b_x1T = p.buf("o_x1T")
        selb = sb("o_selb", [128, 16, NE], BF16)
        b_selb = p.buf("o_selb")
        st = sb("o_st", [128, 8], F32)
        rr = sb("o_rr", [128, 4], F32)
        b_rr = p.buf("o_rr")
        sm = {n: sb("o_" + n, [128, NE], F32) for n in ["lg", "sel", "ex", "G", "oh", "tmp", "pos"]}
        b_sm = p.buf("o_sm")
        mx8 = sb("o_mx8", [128, 8], F32)
        pk = sb("o_pk", [128, 8], F32)
        b_x1res = p.buf("x1res")
        b_XS = p.buf("XS")
        b_mixT = p.buf("mixT_r")
        lnsb = {"st": st, "b_st": p.buf("o_st")}
        print("oproj stage: sbuf bytes remaining:", nc.sbuf_bytes_remaining)

        p.dma("sp", lambda e: e.dma_start(out=lng[:, :], in_=k.ln1_g[l:l + 1, :].broadcast_to([128, D])), writes=[b_par])
        p.dma("sp", lambda e: e.dma_start(out=lnb[:, :], in_=k.ln1_b[l:l + 1, :].broadcast_to([128, D])), writes=[b_par])
        p.dma("sp", lambda e: e.dma_start(out=wr[:, :, :], in_=k.w_router[l].rearrange("(c p) n -> p c n", p=128)), writes=[b_par])
        p.dma("sp", lambda e: e.dma_start(out=brt[:, :], in_=k.b_router[l:l + 1, :].broadcast_to([128, NE])), writes=[b_par])
        for c in range(16):
            p.dma("pool", lambda e, c=c: e.dma_start(out=wout[:, c, :], in_=k.w_out[l, c * 128:(c + 1) * 128, :]), writes=[b_wout])

        ssv = k.ss[:, :].rearrange("p (a b) -> p a b", a=2)
        for tb in range(16):
            i2 = tb % 2
            t0, t1 = tb * 128, (tb + 1) * 128
            p.dma("sp", lambda e: e.dma_start(out=mixt[i2][:, :, :], in_=k.mixT[:, :, t0:t1].rearrange("c p t -> p c t")), reads=[b_mixT], writes=[b_mixt[i2]])
            p.dma("sp", lambda e: e.dma_start(out=xr[i2][:, :], in_=xcur[t0:t1, :]), writes=[b_xr[i2]])
            p.op("dve", lambda e: e.tensor_scalar(out=rr[:, 0:2], in0=ssv[:, :, tb], scalar1=1.0 / WA, scalar2=1e-6, op0=ALU.mult, op1=ALU.add),
                 reads=[k.b_ss], writes=[b_rr])
            p.op("act", lambda e: e.activation(out=rr[:, 0:2], in_=rr[:, 0:2], func=AF.Sqrt), writes=[b_rr])
            p.op("dve", lambda e: e.reciprocal(out=rr[:, 2:4], in_=rr[:, 0:2]), writes=[b_rr])
            p.op("act", lambda e: e.activation(out=u[:, :], in_=xr[i2][:, :], func=AF.Copy, scale=ALPHA), reads=[b_xr[i2]], writes=[b_u])
            for part in range(2):
                for nb in range(4):
                    for c in range(8):
                        cc = 8 * part + c
                        p.op("pe", lambda e, nb=nb, cc=cc, c=c: e.matmul(k.F[nb][:, :], lhsT=mixt[i2][:, cc, :], rhs=wout[:, cc, nb * 512:(nb + 1) * 512],
                                                                      start=(c == 0), stop=(c == 7)),
                             reads=[b_mixt[i2], b_wout], writes=[k.b_F[nb]], inc=(c == 7))
                for nb in range(4):
                    p.op("dve", lambda e, nb=nb, part=part: e.scalar_tensor_tensor(out=u[:, nb * 512:(nb + 1) * 512], in0=k.F[nb][:, :], scalar=rr[:, 2 + part:3 + part],
                                                                                 in1=u[:, nb * 512:(nb + 1) * 512], op0=ALU.mult, op1=ALU.add),
                         reads=[k.b_F[nb], b_rr], writes=[b_u])
            xf, b_xf = x1f[i2], b_x1f[i2]
            ln_block(k, lnsb, u, b_u, xf, b_xf, lng, lnb, b_par)
            p.op("act", lambda e: e.activation(out=x1b[i2][:, :], in_=xf[:, :], func=AF.Copy), reads=[b_xf], writes=[b_x1b[i2]])
            p.dma("sp", lambda e: e.dma_start(out=k.x1res[t0:t1, :], in_=xf[:, :]), reads=[b_xf], writes=[b_x1res])
            transpose_block(k, xf, b_xf, tb, xT_dst=False, f32_dst=x1T, b_f32=b_x1T, fbanks=(4, 5))
            for c in range(16):
                p.op("pe", lambda e, c=c: e.matmul(k.F[6][:, 0:NE], lhsT=x1T[:, c, :], rhs=wr[:, c, :], start=(c == 0), stop=(c == 15)),
                     reads=[b_x1T, b_par], writes=[k.b_F[6]], inc=(c == 15))
            lg, sel, ex, G, oh, tmp, pos = [sm[n] for n in ["lg", "sel", "ex", "G", "oh", "tmp", "pos"]]
            W = [b_sm]
            p.op("dve", lambda e: e.tensor_tensor(out=lg[:, :], in0=k.F[6][:, 0:NE], in1=brt[:, :], op=ALU.add), reads=[k.b_F[6], b_par], writes=W)
            p.op("dve", lambda e: e.max(out=mx8[:, :], in_=lg[:, :]), writes=W)
            p.op("dve", lambda e: e.tensor_scalar(out=sel[:, :], in0=lg[:, :], scalar1=mx8[:, 3:4], scalar2=None, op0=ALU.is_ge), writes=W)
            p.op("dve", lambda e: e.tensor_copy(selb[:, tb, :], sel[:, :]), reads=W, writes=[b_selb])
            p.op("dve", lambda e: e.tensor_scalar(out=pk[:, 4:5], in0=mx8[:, 0:1], scalar1=-1.0, scalar2=None, op0=ALU.mult), writes=W)
            p.op("act", lambda e: e.activation(out=ex[:, :], in_=lg[:, :], func=AF.Exp, bias=pk[:, 4:5]), writes=W)
            p.op("dve", lambda e: e.tensor_tensor(out=ex[:, :], in0=ex[:, :], in1=sel[:, :], op=ALU.mult), writes=W)
            p.op("dve", lambda e: e.reduce_sum(out=pk[:, 5:6], in_=ex[:, :], axis=AX.X), writes=W)
            p.op("dve", lambda e: e.reciprocal(out=pk[:, 6:7], in_=pk[:, 5:6]), writes=W)
            p.op("dve", lambda e: e.tensor_scalar(out=G[:, :], in0=ex[:, :], scalar1=pk[:, 6:7], scalar2=None, op0=ALU.mult), writes=W)
            for b2 in range(tb + 1):
                lhs = cs["c_ones"] if b2 < tb else cs["c_tstrict"]
                p.op("pe", lambda e, b2=b2, lhs=lhs: e.matmul(k.PS[:, 0:NE], lhsT=lhs[:, :], rhs=selb[:, b2, :], start=(b2 == 0), stop=(b2 == tb)),
                     reads=[b_selb, k.b_c], writes=[k.b_PS], inc=(b2 == tb))
            p.op("dve", lambda e: e.tensor_scalar(out=tmp[:, :], in0=k.PS[:, 0:NE], scalar1=float(CAP), scalar2=1.0e6, op0=ALU.is_ge, op1=ALU.mult),
                 reads=[k.b_PS], writes=W)
            p.op("dve", lambda e: e.tensor_tensor(out=pos[:, :], in0=k.PS[:, 0:NE], in1=cs["c_ecap"][:, :], op=ALU.add), reads=[k.b_PS, k.b_c], writes=W)
            p.op("dve", lambda e: e.tensor_tensor(out=pos[:, :], in0=pos[:, :], in1=tmp[:, :], op=ALU.add), writes=W)
            for kk in range(4):
                p.op("dve", lambda e, kk=kk: e.tensor_scalar(out=oh[:, :], in0=lg[:, :], scalar1=mx8[:, kk:kk + 1], scalar2=None, op0=ALU.is_equal), writes=W)
                p.op("dve", lambda e: e.tensor_tensor(out=tmp[:, :], in0=oh[:, :], in1=G[:, :], op=ALU.mult), writes=W)
                p.op("dve", lambda e, kk=kk: e.reduce_sum(out=k.gk[:, tb, kk:kk + 1], in_=tmp[:, :], axis=AX.X), reads=W, writes=[k.b_route])
                p.op("dve", lambda e: e.tensor_tensor(out=tmp[:, :], in0=oh[:, :], in1=pos[:, :], op=ALU.mult), writes=W)
                p.op("dve", lambda e, kk=kk: e.reduce_sum(out=pk[:, kk:kk + 1], in_=tmp[:, :], axis=AX.X), writes=W)
            p.op("dve", lambda e: e.tensor_copy(k.idx[:, tb, :], pk[:, 0:4]), reads=W, writes=[k.b_route])
            for kk in range(4):
                p.dma("pool", lambda e, kk=kk: e.indirect_dma_start(out=k.XS[:, :], out_offset=bass.IndirectOffsetOnAxis(ap=k.idx[:, tb, kk:kk + 1], axis=0),
                                                                   in_=x1b[i2][:, :], in_offset=None, bounds_check=k.bc_reg, oob_is_err=False),
                      reads=[b_x1b[i2], k.b_route], writes=[b_XS])
        p.end_stage()


def stage_moe(k, l):
    p = k.p
    nc = k.nc
    cs = k.cs
    NS = CAP
    with ExitStack() as es:
        sb = lambda name, shape, d: es.enter_context(nc.sbuf_tensor("L%d_" % l + name, shape, d))
        NW1 = 4
        w1 = [sb("m_w1_%d" % i, [128, 16, 512], BF16) for i in range(NW1)]
        b_w1 = [p.buf("m_w1_%d" % i) for i in range(NW1)]
        w2 = [k.xT[:, 0:8, :], k.xT[:, 8:16, :]]
        b_w2 = [p.buf("m_w2_0"), p.buf("m_w2_1")]
        xs = sb("m_xs", [128, 3, D], BF16)
        b_xs = p.buf("m_xs")
        xeT = sb("m_xeT", [128, 16, NS], BF16)
        b_xeT = p.buf("m_xeT")
        hT = [sb("m_hT%d" % i, [128, 8, NS], BF16) for i in range(2)]
        b_hT = [p.buf("m_hT%d" % i) for i in range(2)]
        NT = 2
        gc = [sb("m_gc%d" % i, [128, NS], F32) for i in range(NT)]
        sg = [sb("m_sg%d" % i, [128, NS], F32) for i in range(NT)]
        uc = [sb("m_uc%d" % i, [128, NS], F32) for i in range(NT)]
        b_tmp = [p.buf("m_tmp%d" % i) for i in range(NT)]
        NY = 4
        yo = [sb("m_yo%d" % i, [128, 512], F32) for i in range(NY)]
        b_yo = [p.buf("m_yo%d" % i) for i in range(NY)]
        b1 = sb("m_b1", [128, NE, 16], F32)
        b_b1 = p.buf("m_b1")
        b2 = [sb("m_b2_%d" % i, [128, D], F32) for i in range(2)]
        b_b2 = [p.buf("m_b2_%d" % i) for i in range(2)]
        b_XS = p.buf("XS_r")
        b_YS = p.buf("YS")
        print("moe stage: sbuf bytes remaining:", nc.sbuf_bytes_remaining)

        p.dma("sp", lambda e: e.dma_start(out=b1[:, :, :], in_=k.b_gu[l, :, :, :]), writes=[b_b1])

        def load_slab(g_):
            wi = g_ % NW1
            for hc in range(2):
                p.dma("sp", lambda e, wi=wi, hc=hc, g_=g_: e.dma_start(out=w1[wi][:, 8 * hc:8 * hc + 8, :], in_=k.WB1[g_, :, 8 * hc:8 * hc + 8, :]),
                      reads=[k.b_WB], writes=[b_w1[wi]])

        def load_w2(e_):
            wj = e_ % 2
            for hf in range(2):
                src = k.w_dn[l, e_].rearrange("(c p) n -> p c n", p=128)[:, 4 * hf:4 * hf + 4, :]
                p.dma("pool", lambda e, wj=wj, hf=hf, src=src: e.dma_start(out=w2[wj][:, 4 * hf:4 * hf + 4, :], in_=src), writes=[b_w2[wj]])

        def load_acts(e_):
            p.dma("sp", lambda e: e.dma_start(out=xs[:, :, :], in_=k.XS[e_ * CAP:(e_ + 1) * CAP, :].rearrange("(j p) f -> p j f", p=128)),
                  reads=[b_XS], writes=[b_xs])

        def load_b2(e_):
            p.dma("sp", lambda e: e.dma_start(out=b2[e_ % 2][:, :], in_=k.b_dn[l, e_:e_ + 1, :].broadcast_to([128, D])), writes=[b_b2[e_ % 2]])

        st = {"ev": 0, "yo": 0, "tmp": 0, "dn": 0}
        st["slab"] = 0

        def ensure_slabs(upto):
            while st["slab"] <= min(upto, 4 * NE - 1):
                load_slab(st["slab"])
                st["slab"] += 1

        ensure_slabs(3)
        load_w2(0)
        load_acts(0)
        load_b2(0)
        load_b2(1)
        PTB = k.F[6][:, 0:256].bitcast(BF16)

        def emit_transposes(e_):
            for j in range(3):
                for cg in range(4):
                    if st["ev"] % 2 == 0:
                        TB, bTB = k.PT, k.b_PT
                    else:
                        TB, bTB = PTB, k.b_F[6]
                    for i in range(4):
                        c = 4 * cg + i
                        p.op("pe", lambda e, j=j, c=c, i=i, TB=TB: e.transpose(TB[:, i * 128:(i + 1) * 128], xs[:, j, c * 128:(c + 1) * 128], cs["c_identb"][:, :]),
                             reads=[b_xs, k.b_c], writes=[bTB], inc=(i == 3))
                    src = TB[:, :].rearrange("p (a t) -> p a t", a=4)
                    dst = xeT[:, 4 * cg:4 * cg + 4, j * 128:(j + 1) * 128]
                    if st["ev"] % 2 == 0:
                        p.op("act", lambda e, src=src, dst=dst: e.activation(out=dst, in_=src, func=AF.Copy), reads=[bTB], writes=[b_xeT])
                    else:
                        p.op("dve", lambda e, src=src, dst=dst: e.tensor_copy(dst, src), reads=[bTB], writes=[b_xeT])
                    st["ev"] += 1
            if e_ + 1 < NE:
                load_acts(e_ + 1)

        emit_transposes(0)
        for e_ in range(NE):
            hb = e_ % 2
            for jj in range(8):
                half = jj // 4
                if jj == 0:
                    ensure_slabs(4 * e_ + 3)
                    if e_ + 1 < NE:
                        load_w2(e_ + 1)
                elif jj == 4:
                    ensure_slabs(4 * e_ + 5)
                co = (jj % 4) * 128
                Gb, Ub = jj % 2, 2 + jj % 2
                for gu, bank in ((0, Gb), (1, Ub)):
                    wi = (4 * e_ + 2 * half + gu) % NW1
                    for c in range(16):
                        p.op("pe", lambda e, c=c, wi=wi, bank=bank: e.matmul(k.F[bank][:, 0:NS], lhsT=w1[wi][:, c, co:co + 128], rhs=xeT[:, c, :],
                                                                         start=(c == 0), stop=(c == 15)),
                             reads=[b_w1[wi], b_xeT], writes=[k.b_F[bank]], inc=(c == 15))
                ti = st["tmp"] % NT
                st["tmp"] += 1
                T = [b_tmp[ti]]
                p.op("dve", lambda e: e.tensor_scalar(out=gc[ti][:, :], in0=k.F[Gb][:, 0:NS], scalar1=b1[:, e_, jj:jj + 1], scalar2=7.0, op0=ALU.add, op1=ALU.min),
                     reads=[k.b_F[Gb], b_b1], writes=T)
                p.op("act", lambda e: e.activation(out=sg[ti][:, :], in_=gc[ti][:, :], func=AF.Sigmoid, scale=1.702), writes=T)
                p.op("dve", lambda e: e.tensor_scalar(out=uc[ti][:, :], in0=k.F[Ub][:, 0:NS], scalar1=b1[:, e_, 8 + jj:9 + jj], scalar2=7.0, op0=ALU.add, op1=ALU.min),
                     reads=[k.b_F[Ub], b_b1], writes=T)
                p.op("dve", lambda e: e.tensor_scalar(out=uc[ti][:, :], in0=uc[ti][:, :], scalar1=-7.0, scalar2=1.0, op0=ALU.max, op1=ALU.add), writes=T)
                p.op("dve", lambda e: e.tensor_tensor(out=gc[ti][:, :], in0=gc[ti][:, :], in1=sg[ti][:, :], op=ALU.mult), writes=T)
                p.op("dve", lambda e: e.tensor_tensor(out=hT[hb][:, jj, :], in0=gc[ti][:, :], in1=uc[ti][:, :], op=ALU.mult), reads=T, writes=[b_hT[hb]])
            if e_ + 1 < NE:
                emit_transposes(e_ + 1)
            wj = e_ % 2
            for j in range(3):
                for q in range(4):
                    bank = 4 + st["dn"] % 2
                    st["dn"] += 1
                    for jj in range(8):
                        p.op("pe", lambda e, jj=jj, j=j, q=q, bank=bank: e.matmul(k.F[bank][:, :], lhsT=hT[hb][:, jj, j * 128:(j + 1) * 128], rhs=w2[wj][:, jj, q * 512:(q + 1) * 512],
                                                                               start=(jj == 0), stop=(jj == 7)),
                             reads=[b_hT[hb], b_w2[wj]], writes=[k.b_F[bank]], inc=(jj == 7))
                    yi = st["yo"] % NY
                    st["yo"] += 1
                    p.op("dve", lambda e, q=q, bank=bank, yi=yi: e.tensor_tensor(out=yo[yi][:, :], in0=k.F[bank][:, :], in1=b2[wj][:, q * 512:(q + 1) * 512], op=ALU.add),
                         reads=[k.b_F[bank], b_b2[wj]], writes=[b_yo[yi]])
                    r0 = e_ * CAP + j * 128
                    p.dma("pool", lambda e, q=q, yi=yi, r0=r0: e.dma_start(out=k.YS[r0:r0 + 128, q * 512:(q + 1) * 512], in_=yo[yi][:, :]), reads=[b_yo[yi]], writes=[b_YS])
            if e_ + 2 < NE:
                load_b2(e_ + 2)
        p.end_stage()


def stage_ln2(k, l, dst, last):
    p = k.p
    nc = k.nc
    with ExitStack() as es:
        sb = lambda name, shape, d: es.enter_context(nc.sbuf_tensor("L%d_" % l + name, shape, d))
        yk = [[sb("l_yk%d_%d" % (s, i), [128, D], F32) for i in range(4)] for s in range(2)]
        b_yk = [[p.buf("l_yk%d_%d" % (s, i)) for i in range(4)] for s in range(2)]
        x1t = [sb("l_x1t%d" % i, [128, D], F32) for i in range(2)]
        b_x1t = [p.buf("l_x1t%d" % i) for i in range(2)]
        u = sb("l_u", [128, D], F32)
        b_u = p.buf("l_u")
        x2 = [sb("l_x2_%d" % i, [128, D], F32) for i in range(2)]
        b_x2 = [p.buf("l_x2_%d" % i) for i in range(2)]
        lng = sb("l_lng", [128, D], F32)
        lnb = sb("l_lnb", [128, D], F32)
        b_par = p.buf("l_par")
        st = sb("l_st", [128, 8], F32)
        lnsb = {"st": st, "b_st": p.buf("l_st")}
        b_YS = p.buf("YS_r")
        b_x1res = p.buf("x1res_r")
        b_dst = p.buf("dst")
        print("ln2 stage: sbuf bytes remaining:", nc.sbuf_bytes_remaining)
        p.dma("sp", lambda e: e.dma_start(out=lng[:, :], in_=k.ln2_g[l:l + 1, :].broadcast_to([128, D])), writes=[b_par])
        p.dma("sp", lambda e: e.dma_start(out=lnb[:, :], in_=k.ln2_b[l:l + 1, :].broadcast_to([128, D])), writes=[b_par])
        for tb in range(16):
            s2 = tb % 2
            t0, t1 = tb * 128, (tb + 1) * 128
            for kk in range(4):
                p.dma("pool", lambda e, kk=kk: e.indirect_dma_start(out=yk[s2][kk][:, :], out_offset=None, in_=k.YS[:, :],
                                                                   in_offset=bass.IndirectOffsetOnAxis(ap=k.idx[:, tb, kk:kk + 1], axis=0),
                                                                   bounds_check=k.bc_reg, oob_is_err=False),
                      reads=[b_YS, k.b_route], writes=[b_yk[s2][kk]])
            p.dma("sp", lambda e: e.dma_start(out=x1t[s2][:, :], in_=k.x1res[t0:t1, :]), reads=[b_x1res], writes=[b_x1t[s2]])
            p.op("act", lambda e: e.activation(out=u[:, :], in_=x1t[s2][:, :], func=AF.Copy, scale=ALPHA), reads=[b_x1t[s2]], writes=[b_u])
            for kk in range(4):
                p.op("dve", lambda e, kk=kk: e.scalar_tensor_tensor(out=u[:, :], in0=yk[s2][kk][:, :], scalar=k.gk[:, tb, kk:kk + 1], in1=u[:, :], op0=ALU.mult, op1=ALU.add),
                     reads=[b_yk[s2][kk], k.b_route], writes=[b_u])
            xo, b_xo = x2[s2], b_x2[s2]
            ln_block(k, lnsb, u, b_u, xo, b_xo, lng, lnb, b_par)
            p.dma("sp", lambda e: e.dma_start(out=dst[t0:t1, :], in_=xo[:, :]), reads=[b_xo], writes=[b_dst])
            if not last:
                transpose_block(k, xo, b_xo, tb)
        p.end_stage()


_NC_CACHE = {}


def _host_layout(inputs):
    f32 = lambda a: np.ascontiguousarray(np.asarray(a, dtype=np.float32))
    m = {}
    m["w_in"] = f32(inputs["w_in"])
    m["w_out"] = f32(inputs["w_out"])
    gm = np.concatenate([np.asarray(inputs["g_mix_a"], np.float32), np.asarray(inputs["g_mix_b"], np.float32)], axis=1)
    m["gmix"] = np.ascontiguousarray(gm.reshape(NL, 32, 64).transpose(0, 2, 1))
    for n in ["ln1_g", "ln1_b", "ln2_g", "ln2_b", "w_router", "b_router", "b_down", "w_gate_up", "w_down"]:
        m[n] = f32(inputs[n])
    m["b_gu"] = np.ascontiguousarray(np.asarray(inputs["b_gate_up"], np.float32).reshape(NL, NE, 16, 128).transpose(0, 3, 1, 2))
    m.update(make_consts())
    return m


def kernel(**inputs):
    x = np.asarray(inputs["x"], dtype=np.float32)
    nb = x.shape[0]
    shared = _host_layout(inputs)
    if "nc" not in _NC_CACHE:
        _NC_CACHE["nc"] = build(n_layers=NL)
    nc = _NC_CACHE["nc"]
    in_maps = []
    for b in range(nb):
        m = dict(shared)
        m["x"] = np.ascontiguousarray(x[b])
        in_maps.append(m)
    res = run_bass_kernel_spmd(nc, in_maps, core_ids=list(range(nb)))
    out = np.stack([np.asarray(r["out"], dtype=np.float32) for r in res.results], axis=0)
    return out
```

```python
import math
from contextlib import ExitStack

import numpy as np
import ml_dtypes

import concourse.bass as bass
import concourse.mybir as mybir
from concourse.bass_utils import run_bass_kernel_spmd

F32 = mybir.dt.float32
BF16 = mybir.dt.bfloat16
I32 = mybir.dt.int32
AF = mybir.ActivationFunctionType
ALU = mybir.AluOpType
AX = mybir.AxisListType

NL = 4
S = 2048
D = 2048
HD = 64
NH = 16
WA = 1024
NE = 32
CAP = 384
NSLOT = NE * CAP
DFF = 1024
ALPHA = (2.0 * NL) ** 0.25
PAT = ((128, 1), (512, 4), (2048, 16))
BIGD = 30000.0
NEGM = -30000.0
SAME_ENG_SYNC = False


class Buf:
    __slots__ = ("name", "w", "r", "dkey", "excl")

    def __init__(self, name, excl=False):
        self.name = name
        self.w = None
        self.r = {}
        self.dkey = None
        self.excl = excl


class Prog:
    def __init__(self, nc, es, n_dsem=90):
        self.nc = nc
        self.E = {"pe": nc.tensor, "act": nc.scalar, "dve": nc.vector, "pool": nc.gpsimd, "sp": nc.sync}
        self.semobj = {}
        self.cnt = {}
        for k in ["pe", "act", "dve", "pool"]:
            self.semobj[("e", k)] = es.enter_context(nc.semaphore("e_" + k))
            self.cnt[k] = 0
        self.seen = {k: {} for k in self.E}
        self.dfree = []
        self.dval = {}
        for i in range(n_dsem):
            key = ("d", i)
            self.semobj[key] = es.enter_context(nc.semaphore("d%d" % i))
            self.dval[key] = 0
            self.dfree.append(key)
        self.stage_bufs = []
        self.defer = set()

    def buf(self, name):
        b = Buf(name)
        self.stage_bufs.append(b)
        return b

    def _deps(self, eng, reads, writes):
        deps = {}
        me = ("e", eng)

        def add(k, v):
            if v > deps.get(k, 0):
                deps[k] = v

        for b in reads:
            if b.w is not None:
                add(*b.w)
        for b in writes:
            if b.w is not None:
                add(*b.w)
            for k, v in b.r.items():
                if k != me:
                    add(k, v)
        out = []
        seen = self.seen[eng]
        for k, v in deps.items():
            if k == me and eng == "pe":
                continue
            if k[0] == "e":
                assert v <= self.cnt[k[1]], ("wait on not-yet-emitted inc", eng, k, v, self.cnt[k[1]])
            if seen.get(k, 0) < v:
                seen[k] = v
                out.append((k, v))
        return out

    def _emit_waits(self, eng, waits):
        e = self.E[eng]
        for k, v in waits:
            e.wait_ge(self.semobj[k], v)

    def op(self, eng, fn, reads=(), writes=(), inc=True):
        if any(b.excl for b in reads):
            writes = list(writes) + [b for b in reads if b.excl]
            reads = [b for b in reads if not b.excl]
        self._emit_waits(eng, self._deps(eng, reads, writes))
        ins = fn(self.E[eng])
        if inc:
            self.cnt[eng] += 1
            ins.then_inc(self.semobj[("e", eng)], 1)
            tok = (("e", eng), self.cnt[eng])
        else:
            tok = (("e", eng), self.cnt[eng] + 1)
        for b in reads:
            if b.r.get(tok[0], 0) < tok[1]:
                b.r[tok[0]] = tok[1]
        for b in writes:
            b.w = tok
            b.r = {}
        return tok

    def dma(self, q, fn, reads=(), writes=()):
        self._emit_waits(q, self._deps(q, reads, writes))
        wb = writes[0]
        if wb.dkey is None:
            assert self.dfree, "out of DMA semaphores"
            wb.dkey = self.dfree.pop()
        key = wb.dkey
        ins = fn(self.E[q])
        self.dval[key] += 16
        ins.then_inc(self.semobj[key], 16)
        tok = (key, self.dval[key])
        for b in reads:
            if b.r.get(tok[0], 0) < tok[1]:
                b.r[tok[0]] = tok[1]
        for b in writes:
            b.w = tok
            b.r = {}
        return tok

    def barrier(self, engines=("pe", "act", "dve", "pool", "sp")):
        allv = {}
        for k in ["pe", "act", "dve", "pool"]:
            if self.cnt[k] > 0:
                allv[("e", k)] = self.cnt[k]
        for key, v in self.dval.items():
            if v > 0 and key not in self.defer:
                allv[key] = v
        for eng in engines:
            seen = self.seen[eng]
            for k, v in allv.items():
                if k == ("e", eng):
                    continue
                if seen.get(k, 0) < v:
                    seen[k] = v
                    self.E[eng].wait_ge(self.semobj[k], v)

    def end_stage(self):
        self.barrier()
        for b in self.stage_bufs:
            if b.dkey is not None:
                self.dfree.append(b.dkey)
                b.dkey = None
        self.stage_bufs = []


def make_consts():
    c = {}
    k = np.arange(128)[:, None]
    q = np.arange(128)[None, :]
    cur = np.where(q >= k, (q - k).astype(np.float32), BIGD)
    prv = np.where(k >= q, (q + 128 - k).astype(np.float32), BIGD)
    c["c_dm"] = np.concatenate([cur, prv], axis=1).astype(np.float32)
    c["c_identf"] = np.eye(128, dtype=np.float32)
    c["c_identb"] = np.eye(128, dtype=np.float32).astype(ml_dtypes.bfloat16)
    qq = np.arange(512)[None, :]
    neg = np.stack([np.where(128 * o + k < qq, 0.0, NEGM) for o in range(4)], axis=1)
    c["c_neg"] = neg.astype(ml_dtypes.bfloat16)
    kp = np.arange(128)[:, None]
    kk = np.arange(128)[None, :]
    c["c_negtri"] = np.where(kp >= kk, -1.0, 0.0).astype(ml_dtypes.bfloat16)
    c["c_negones"] = np.full((128, 128), -1.0).astype(ml_dtypes.bfloat16)
    c["c_ones"] = np.ones((128, 128), np.float32).astype(ml_dtypes.bfloat16)
    c["c_tstrict"] = np.where(kp < kk, 1.0, 0.0).astype(ml_dtypes.bfloat16)
    sel = np.zeros((128, 64), np.float32)
    sel[64, :] = 1.0
    c["c_sel65"] = sel
    c["c_onesf"] = np.ones((128, 8), np.float32)
    c["c_ecap"] = np.tile((np.arange(NE) * CAP).astype(np.float32)[None, :], (128, 1))
    return c


CONST_SPECS = {
    "c_dm": ([128, 256], F32), "c_identf": ([128, 128], F32), "c_identb": ([128, 128], BF16),
    "c_neg": ([128, 4, 512], BF16), "c_negtri": ([128, 128], BF16), "c_negones": ([128, 128], BF16),
    "c_ones": ([128, 128], BF16), "c_tstrict": ([128, 128], BF16), "c_sel65": ([128, 64], F32),
    "c_onesf": ([128, 8], F32), "c_ecap": ([128, NE], F32),
}


class K:
    pass


def build(n_layers=NL, stages=("attn", "oproj", "moe", "ln2"), debug=()):
    nc = bass.Bass("TRN2", target_bir_lowering=False)
    k = K()
    k.nc = nc
    k.debug = set(debug)
    dt = nc.dram_tensor
    k.x_in = dt("x", [S, D], F32, kind="ExternalInput").ap()
    k.w_in = dt("w_in", [NL, D, 6144], F32, kind="ExternalInput").ap()
    k.w_out = dt("w_out", [NL, D, D], F32, kind="ExternalInput").ap()
    k.gmix = dt("gmix", [NL, 64, 32], F32, kind="ExternalInput").ap()
    k.ln1_g = dt("ln1_g", [NL, D], F32, kind="ExternalInput").ap()
    k.ln1_b = dt("ln1_b", [NL, D], F32, kind="ExternalInput").ap()
    k.ln2_g = dt("ln2_g", [NL, D], F32, kind="ExternalInput").ap()
    k.ln2_b = dt("ln2_b", [NL, D], F32, kind="ExternalInput").ap()
    k.w_router = dt("w_router", [NL, D, NE], F32, kind="ExternalInput").ap()
    k.b_router = dt("b_router", [NL, NE], F32, kind="ExternalInput").ap()
    if "moe" in stages:
        k.w_gu = dt("w_gate_up", [NL, NE, D, 2 * DFF], F32, kind="ExternalInput").ap()
        k.w_dn = dt("w_down", [NL, NE, DFF, D], F32, kind="ExternalInput").ap()
    k.b_gu = dt("b_gu", [NL, 128, NE, 16], F32, kind="ExternalInput").ap()
    k.b_dn = dt("b_down", [NL, NE, D], F32, kind="ExternalInput").ap()
    k.cdram = {n: dt(n, shp, d, kind="ExternalInput").ap() for n, (shp, d) in CONST_SPECS.items()}
    k.out = dt("out", [S, D], F32, kind="ExternalOutput").ap()
    k.xres = dt("xres", [S, D], F32, kind="Internal").ap()
    k.x1res = dt("x1res", [S, D], F32, kind="Internal").ap()
    k.mixT = dt("mixT", [16, 128, S], BF16, kind="Internal").ap()
    k.XS = dt("XS", [NSLOT, D], BF16, kind="Internal").ap()
    k.YS = dt("YS", [NSLOT, D], F32, kind="Internal").ap()
    k.WB1 = dt("WB1", [NE * 4, 128, 16, 512], BF16, kind="Internal").ap()
    k.dbg = {}
    if "mix" in k.debug:
        k.dbg["mixT_o"] = dt("mixT_o", [16, 128, S], BF16, kind="ExternalOutput").ap()
        k.dbg["ss_o"] = dt("ss_o", [128, 32], F32, kind="ExternalOutput").ap()
    if "x1" in k.debug:
        k.dbg["x1_o"] = dt("x1_o", [S, D], F32, kind="ExternalOutput").ap()
        k.dbg["idx_o"] = dt("idx_o", [128, 64], I32, kind="ExternalOutput").ap()
        k.dbg["gk_o"] = dt("gk_o", [128, 64], F32, kind="ExternalOutput").ap()
    if "xs" in k.debug:
        k.dbg["xs_o"] = dt("xs_o", [NSLOT, D], BF16, kind="ExternalOutput").ap()
    if "ys" in k.debug:
        k.dbg["ys_o"] = dt("ys_o", [NSLOT, D], F32, kind="ExternalOutput").ap()

    with ExitStack() as es:
        p = Prog(nc, es)
        k.p = p
        sb = lambda name, shape, d: es.enter_context(nc.sbuf_tensor(name, shape, d))
        ps = lambda name, shape, d: es.enter_context(nc.psum_tensor(name, shape, d))
        k.xT = sb("xT", [128, 16, S], BF16)
        k.b_xT = Buf("xT")
        k.cs = {}
        k.b_c = Buf("consts")
        for n, (shp, d) in CONST_SPECS.items():
            k.cs[n] = sb("s_" + n, shp, d)
        k.ss = sb("ss", [128, 32], F32)
        k.b_ss = Buf("ss")
        k.idx = sb("idx", [128, 16, 4], I32)
        k.gk = sb("gk", [128, 16, 4], F32)
        k.b_route = Buf("route")
        k.b_WB = Buf("WB1")
        k.b_WB.dkey = p.dfree.pop()
        p.defer.add(k.b_WB.dkey)
        k.has_moe = "moe" in stages
        k.F = [ps("F%d" % i, [128, 512], F32) for i in range(7)]
        k.b_F = [Buf("F%d" % i, excl=True) for i in range(7)]
        k.PX = ps("PX", [128, 512], F32)
        k.PT = k.PX[:, 0:256].bitcast(BF16)
        k.b_PT = Buf("PX", excl=True)
        k.PS = k.PX[:, 256:512]
        k.b_PS = k.b_PT
        print("PT shape", k.PT.shape, "PS shape", k.PS.shape)

        k.bc_reg = nc.gpsimd.alloc_register("bc_reg")
        nc.gpsimd.reg_mov(k.bc_reg, NSLOT - 1)
        first = True
        for n in CONST_SPECS:
            src = k.cdram[n]
            dst = k.cs[n]
            if len(CONST_SPECS[n][0]) == 3:
                p.dma("sp", lambda e, d_=dst, s_=src: e.dma_start(out=d_[:, :, :], in_=s_[:, :, :]), writes=[k.b_c])
            else:
                p.dma("sp", lambda e, d_=dst, s_=src: e.dma_start(out=d_[:, :], in_=s_[:, :]), writes=[k.b_c])
        print("sbuf bytes remaining after persistent:", nc.sbuf_bytes_remaining)

        stage_prologue(k)
        xcur = k.x_in
        for l in range(n_layers):
            last = (l == n_layers - 1)
            if "attn" in stages:
                stage_attn(k, l)
            if "mix" in k.debug and l == 0:
                dump_dram(k, k.dbg["mixT_o"].rearrange("c p t -> (c p) t"), k.mixT.rearrange("c p t -> (c p) t"), 2048, BF16, 2048)
                dump_sbuf(k, k.dbg["ss_o"], k.ss, k.b_ss)
            if "oproj" in stages:
                stage_oproj(k, l, xcur)
            if "x1" in k.debug and l == 0:
                dump_dram(k, k.dbg["x1_o"], k.x1res, 2048, F32, 2048)
                dump_sbuf(k, k.dbg["idx_o"], k.idx[:, :, :].rearrange("p a b -> p (a b)"), k.b_route, raw=True)
                dump_sbuf(k, k.dbg["gk_o"], k.gk[:, :, :].rearrange("p a b -> p (a b)"), k.b_route, raw=True)
            if "xs" in k.debug and l == 0:
                dump_dram(k, k.dbg["xs_o"], k.XS, NSLOT, BF16, 2048)
            if "moe" in stages:
                stage_moe(k, l)
            if "ys" in k.debug and l == 0:
                dump_dram(k, k.dbg["ys_o"], k.YS, NSLOT, F32, 2048)
            if "ln2" in stages:
                stage_ln2(k, l, k.out if last else k.xres, last)
            xcur = k.xres
        p.barrier()
    return nc


def dump_dram(k, dst, src, rows, dtp, cols):
    p = k.p
    nc = k.nc
    p.barrier()
    k.ndump = getattr(k, "ndump", 0) + 1
    with nc.sbuf_tensor("dump_t%d" % k.ndump, [128, cols], dtp) as t:
        bt = Buf("dump_t")
        bo = Buf("dump_o")
        for r0 in range(0, rows, 128):
            p.dma("sp", lambda e, r0=r0: e.dma_start(out=t[:, :], in_=src[r0:r0 + 128, :]), writes=[bt])
            p.dma("sp", lambda e, r0=r0: e.dma_start(out=dst[r0:r0 + 128, :], in_=t[:, :]), reads=[bt], writes=[bo])
        p.barrier()
        if bt.dkey is not None:
            p.dfree.append(bt.dkey)
        if bo.dkey is not None:
            p.dfree.append(bo.dkey)


def dump_sbuf(k, dst, src, b, raw=False):
    p = k.p
    p.barrier()
    bo = Buf("dump_o2")
    if raw:
        p.dma("sp", lambda e: e.dma_start(out=dst[:, :], in_=src), reads=[b], writes=[bo])
    else:
        p.dma("sp", lambda e: e.dma_start(out=dst[:, :], in_=src[:, :]), reads=[b], writes=[bo])
    p.barrier()
    p.dfree.append(bo.dkey)


def transpose_block(k, src, b_src, tb, xT_dst=True, f32_dst=None, b_f32=None, fbanks=(0, 1), evac=("act", "dve")):
    p = k.p
    identf = k.cs["c_identf"]
    for g in range(4):
        fb = fbanks[g % len(fbanks)]
        F = k.F[fb]
        bF = k.b_F[fb]
        for i in range(4):
            c = 4 * g + i
            p.op("pe", lambda e, c=c, i=i, F=F: e.transpose(F[:, i * 128:(i + 1) * 128], src[:, c * 128:(c + 1) * 128], identf[:, :]),
                 reads=[b_src, k.b_c], writes=[bF], inc=(i == 3))
        Fv = F[:, :].rearrange("p (a t) -> p a t", a=4)
        if xT_dst:
            eng = evac[g % len(evac)]
            if eng == "act":
                p.op("act", lambda e, g=g, Fv=Fv: e.activation(out=k.xT[:, 4 * g:4 * g + 4, tb * 128:(tb + 1) * 128], in_=Fv, func=AF.Copy),
                     reads=[bF], writes=[k.b_xT])
            else:
                p.op("dve", lambda e, g=g, Fv=Fv: e.tensor_copy(k.xT[:, 4 * g:4 * g + 4, tb * 128:(tb + 1) * 128], Fv),
                     reads=[bF], writes=[k.b_xT])
        if f32_dst is not None:
            p.op("dve", lambda e, g=g, Fv=Fv: e.tensor_copy(f32_dst[:, 4 * g:4 * g + 4, :], Fv), reads=[bF], writes=[b_f32])


def stage_prologue(k):
    p = k.p
    nc = k.nc
    with ExitStack() as es:
        xt = [es.enter_context(nc.sbuf_tensor("pro_x%d" % i, [128, D], F32)) for i in range(3)]
        bx = [p.buf("pro_x%d" % i) for i in range(3)]
        zt = es.enter_context(nc.sbuf_tensor("pro_z", [128, D], BF16))
        bz = p.buf("pro_z")
        bxs = p.buf("XS_init")
        p.op("pool", lambda e: e.memset(zt[:, :], 0.0), writes=[bz])
        for r0 in range(0, NSLOT, 128):
            p.dma("pool", lambda e, r0=r0: e.dma_start(out=k.XS[r0:r0 + 128, :], in_=zt[:, :]), reads=[bz], writes=[bxs])
        for tb in range(16):
            t = xt[tb % 3]
            b = bx[tb % 3]
            p.dma("sp", lambda e, t=t, tb=tb: e.dma_start(out=t[:, :], in_=k.x_in[tb * 128:(tb + 1) * 128, :]), writes=[b])
            transpose_block(k, t, b, tb)
        p.end_stage()


def stage_attn(k, l):
    p = k.p
    nc = k.nc
    cs = k.cs
    with ExitStack() as es:
        sb = lambda name, shape, d: es.enter_context(nc.sbuf_tensor("L%d_" % l + name, shape, d))
        NSET = 2
        wsl = [[sb("a_w%d_%d" % (s, j), [128, 16, 128], BF16) for j in range(3)] for s in range(NSET)]
        b_wsl = [[p.buf("a_w%d_%d" % (s, j)) for j in range(3)] for s in range(NSET)]
        qkvT = [[sb("a_qkv%d_%d" % (s, j), [128, S], BF16) for j in range(3)] for s in range(NSET)]
        b_qkvT = [[p.buf("a_qkv%d_%d" % (s, j)) for j in range(3)] for s in range(NSET)]
        Vr = [[sb("a_vr%d_%d" % (s, r), [128, 16, 2, 65], BF16) for r in range(3)] for s in range(NSET)]
        b_Vr = [[p.buf("a_vr%d_%d" % (s, r)) for r in range(3)] for s in range(NSET)]
        acc = [sb("a_acc%d" % i, [128, S], F32) for i in range(2)]
        b_acc = [p.buf("a_acc%d" % i) for i in range(2)]
        NW = 4
        Pe = [sb("a_pe%d" % i, [128, 256], F32) for i in range(NW)]
        b_Pe = [p.buf("a_pe%d" % i) for i in range(NW)]
        Pm = [sb("a_pm%d" % i, [128, 256], BF16) for i in range(NW)]
        b_Pm = [p.buf("a_pm%d" % i) for i in range(NW)]
        Md = [sb("a_md%d" % i, [128, 256], F32) for i in range(2)]
        b_Md = [p.buf("a_md%d" % i) for i in range(2)]
        e32 = [sb("b_e%d" % i, [128, 512], F32) for i in range(2)]
        b_e32 = [p.buf("b_e%d" % i) for i in range(2)]
        spt = [sb("b_sp%d" % i, [128, 512], BF16) for i in range(3)]
        b_spt = [p.buf("b_sp%d" % i) for i in range(3)]
        ss32 = [sb("b_ss%d" % i, [128, 512], F32) for i in range(2)]
        b_ss32 = [p.buf("b_ss%d" % i) for i in range(2)]
        ssb = [sb("b_ssb%d" % i, [128, 512], BF16) for i in range(3)]
        b_ssb = [p.buf("b_ssb%d" % i) for i in range(3)]
        at = [sb("b_a%d" % i, [128, 512], BF16) for i in range(3)]
        b_at = [p.buf("b_a%d" % i) for i in range(3)]
        oaf = [sb("f_oaf%d" % i, [64, 512], F32) for i in range(2)]
        b_oaf = [p.buf("f_oaf%d" % i) for i in range(2)]
        oab = [sb("f_oab%d" % i, [64, 512], BF16) for i in range(2)]
        b_oab = [p.buf("f_oab%d" % i) for i in range(2)]
        sq = [sb("f_sq%d" % i, [64, 512], F32) for i in range(2)]
        b_sq = [p.buf("f_sq%d" % i) for i in range(2)]
        b_mixT = p.buf("mixT")
        gmt = sb("a_gmt", [64, 32], F32)
        b_gmt = p.buf("a_gmt")
        p.dma("sp", lambda e: e.dma_start(out=gmt[:, :], in_=k.gmix[l, :, :]), writes=[b_gmt])
        print("attn stage: sbuf bytes remaining:", nc.sbuf_bytes_remaining)

        for s in range(NSET):
            for r in range(3):
                p.op("pool", lambda e, s=s, r=r: e.memset(Vr[s][r][:, :, :, 64:65], 1.0), writes=[b_Vr[s][r]])
        p.op("pool", lambda e: e.memset(k.ss[:, :], 0.0), writes=[k.b_ss])

        st = {"fin": 0, "pe": 0, "pm": 0, "md": 0}

        def col0(pi, j):
            base = 0 if pi < 8 else 3 * WA
            return base + j * WA + (pi % 8) * 128

        def bg_pair(pi):
            s = pi % NSET
            for j in range(3):
                c0 = col0(pi, j)
                src = k.w_in[l].rearrange("(c p) n -> p c n", p=128)[:, :, c0:c0 + 128]
                p.dma("pool", lambda e, s=s, j=j, src=src: e.dma_start(out=wsl[s][j][:, :, :], in_=src), writes=[b_wsl[s][j]])
            if k.has_moe:
                for e_ in (2 * pi, 2 * pi + 1):
                    for s_ in range(4):
                        gu, half = s_ % 2, s_ // 2
                        c0 = gu * DFF + 512 * half
                        srcw = k.w_gu[l, e_].rearrange("(c p) n -> p c n", p=128)[:, :, c0:c0 + 512]
                        for hc in range(2):
                            p.dma("pool", lambda e, e_=e_, s_=s_, hc=hc, srcw=srcw: e.dma_start(out=k.WB1[4 * e_ + s_, :, 8 * hc:8 * hc + 8, :], in_=srcw[:, 8 * hc:8 * hc + 8, :]),
                                  writes=[k.b_WB])
            yield
            gi = 0
            for j in range(3):
                for n in range(4):
                    fb = gi % 2
                    gi += 1
                    F = k.F[fb]
                    bF = k.b_F[fb]
                    for c in range(16):
                        p.op("pe", lambda e, c=c, F=F, j=j, n=n: e.matmul(F[:, :], lhsT=wsl[s][j][:, c, :], rhs=k.xT[:, c, n * 512:(n + 1) * 512],
                                                                        start=(c == 0), stop=(c == 15)),
                             reads=[b_wsl[s][j], k.b_xT], writes=[bF], inc=(c == 15))
                        if c % 4 == 3:
                            yield
                    scale = 0.125 if j == 0 else 1.0
                    if gi % 2 == 0:
                        p.op("act", lambda e, F=F, j=j, n=n, scale=scale: e.activation(out=qkvT[s][j][:, n * 512:(n + 1) * 512], in_=F[:, :], func=AF.Copy, scale=scale),
                             reads=[bF], writes=[b_qkvT[s][j]])
                    else:
                        p.op("dve", lambda e, F=F, j=j, n=n, scale=scale: e.tensor_scalar(out=qkvT[s][j][:, n * 512:(n + 1) * 512], in0=F[:, :], scalar1=scale, scalar2=None, op0=ALU.mult),
                             reads=[bF], writes=[b_qkvT[s][j]])
                    yield
            npat = 3 if pi < 8 else 1
            vT = qkvT[s][2]
            for r in range(npat):
                d = PAT[r][1]
                nblk = S // d // 128
                vv = vT[:, :].rearrange("p (i d) -> p d i", d=d)
                for g in range(4):
                    for i in range(4):
                        B = 4 * g + i
                        c, n = B // nblk, B % nblk
                        p.op("pe", lambda e, i=i, c=c, n=n, vv=vv: e.transpose(k.PT[:, i * 128:(i + 1) * 128], vv[:, c, n * 128:(n + 1) * 128], cs["c_identb"][:, :]),
                             reads=[b_qkvT[s][2], k.b_c], writes=[k.b_PT], inc=(i == 3))
                    p.op("dve", lambda e, g=g, r=r: e.tensor_copy(Vr[s][r][:, 4 * g:4 * g + 4, :, 0:64],
                                                                 k.PT[:, :].rearrange("p (a h d) -> p a h d", a=4, h=2)),
                         reads=[k.b_PT], writes=[b_Vr[s][r]])
                    yield

        def run_all(gen):
            for _ in gen:
                pass

        def step(gen):
            if gen is not None:
                try:
                    next(gen)
                except StopIteration:
                    return None
            return gen

        def finalize(h_glob, n, src_kind, accb=None, b_accb=None, Fsrc=None, b_Fsrc=None):
            i = st["fin"] % 2
            st["fin"] += 1
            if src_kind == "A":
                F6 = k.F[6]
                p.op("pe", lambda e: e.matmul(F6[0:64, :], lhsT=cs["c_sel65"][0:65, :], rhs=accb[0:65, n * 512:(n + 1) * 512], start=True, stop=True),
                     reads=[b_accb, k.b_c], writes=[k.b_F[6]])
                p.op("dve", lambda e: e.reciprocal(out=sq[i][:, :], in_=F6[0:64, :]), reads=[k.b_F[6]], writes=[b_sq[i]])
                p.op("dve", lambda e: e.tensor_tensor(out=oaf[i][:, :], in0=accb[0:64, n * 512:(n + 1) * 512], in1=sq[i][:, :], op=ALU.mult),
                     reads=[b_accb, b_sq[i]], writes=[b_oaf[i]])
            else:
                p.op("dve", lambda e: e.tensor_copy(oaf[i][:, :], Fsrc[0:64, :]), reads=[b_Fsrc], writes=[b_oaf[i]])
            p.op("dve", lambda e: e.tensor_scalar(out=oab[i][:, :], in0=oaf[i][:, :], scalar1=gmt[:, h_glob:h_glob + 1], scalar2=None, op0=ALU.mult),
                 reads=[b_oaf[i], b_gmt], writes=[b_oab[i]])
            p.op("act", lambda e: e.activation(out=sq[i][:, :], in_=oaf[i][:, :], func=AF.Square), reads=[b_oaf[i]], writes=[b_sq[i]])
            for tb in range(4):
                p.op("pe", lambda e, tb=tb: e.matmul(k.PS[:, tb:tb + 1], lhsT=sq[i][:, tb * 128:(tb + 1) * 128], rhs=cs["c_onesf"][0:64, 0:1], start=True, stop=True),
                     reads=[b_sq[i], k.b_c], writes=[k.b_PS], inc=(tb == 3))
            off = 0 if h_glob < 16 else 16
            p.op("dve", lambda e: e.tensor_tensor(out=k.ss[:, off + 4 * n:off + 4 * n + 4], in0=k.ss[:, off + 4 * n:off + 4 * n + 4], in1=k.PS[:, 0:4], op=ALU.add),
                 reads=[k.b_PS, k.b_ss], writes=[k.b_ss])
            ch, ph = h_glob // 2, (h_glob % 2) * 64
            p.dma("sp", lambda e: e.dma_start(out=k.mixT[ch, ph:ph + 64, n * 512:(n + 1) * 512], in_=oab[i][:, :]), reads=[b_oab[i]], writes=[b_mixT])

        def attn_A(pi, bg):
            s = pi % NSET
            qT, kT = qkvT[s][0], qkvT[s][1]
            bq, bk = b_qkvT[s][0], b_qkvT[s][1]
            for hh in range(2):
                h = 2 * pi + hh
                slope = 2.0 ** (-8.0 * (h + 1) / NH)
                accb, b_accb = acc[hh], b_acc[hh]
                r0, r1 = hh * 64, hh * 64 + 64
                for r in range(3):
                    d = PAT[r][1]
                    nblk = S // d // 128
                    mi = st["md"] % 2
                    st["md"] += 1
                    p.op("act", lambda e, mi=mi, d=d: e.activation(out=Md[mi][:, :], in_=cs["c_dm"][:, :], func=AF.Exp, scale=-slope * d),
                         reads=[k.b_c], writes=[b_Md[mi]])
                    qv = qT[r0:r1, :].rearrange("p (i d) -> p d i", d=d)
                    kv = kT[r0:r1, :].rearrange("p (i d) -> p d i", d=d)
                    av = accb[0:65, :].rearrange("p (i d) -> p d i", d=d)
                    tiles = [(B // nblk, B % nblk) for B in range(16)]
                    pendq = []
                    for t in range(18):
                        cur = None
                        if t < 16:
                            c, m = tiles[t]
                            ncols = 256 if m + 1 < nblk else 128
                            sbk = 2 + (t % 2)
                            FS = k.F[sbk]
                            p.op("pe", lambda e, c=c, m=m, ncols=ncols, FS=FS: e.matmul(FS[:, 0:ncols], lhsT=kv[:, c, m * 128:(m + 1) * 128],
                                                                                      rhs=qv[:, c, m * 128:m * 128 + ncols], start=True, stop=True),
                                 reads=[bq, bk], writes=[k.b_F[sbk]])
                            wi = st["pe"] % NW
                            st["pe"] += 1
                            p.op("act", lambda e, wi=wi, ncols=ncols, FS=FS: e.activation(out=Pe[wi][:, 0:ncols], in_=FS[:, 0:ncols], func=AF.Exp),
                                 reads=[k.b_F[sbk]], writes=[b_Pe[wi]])
                            p.op("dve", lambda e, wi=wi, ncols=ncols, mi=mi: e.tensor_tensor(out=Pm[wi][:, 0:ncols], in0=Pe[wi][:, 0:ncols], in1=Md[mi][:, 0:ncols], op=ALU.mult),
                                 reads=[b_Pe[wi], b_Md[mi]], writes=[b_Pm[wi]])
                            cur = (t, c, m, ncols, wi)
                        if cur is not None:
                            pendq.append(cur)
                        if t >= 2 or t >= 16:
                            tt, c, m, ncols, wi = pendq.pop(0)
                            B = tt
                            g = B // 4
                            ob = 4 + (g % 2)
                            FO = k.F[ob]
                            lhs = Vr[s][r][:, B, hh, :]
                            p.op("pe", lambda e, FO=FO, lhs=lhs, wi=wi, B=B, m=m: e.matmul(FO[0:65, (B % 4) * 128:(B % 4) * 128 + 128], lhsT=lhs, rhs=Pm[wi][:, 0:128],
                                                                                       start=(m == 0), stop=True),
                                 reads=[b_Vr[s][r], b_Pm[wi]], writes=[k.b_F[ob]], inc=True)
                            if ncols == 256:
                                B2 = B + 1
                                ob2 = 4 + ((B2 // 4) % 2)
                                FO2 = k.F[ob2]
                                p.op("pe", lambda e, FO2=FO2, lhs=lhs, wi=wi, B2=B2: e.matmul(FO2[0:65, (B2 % 4) * 128:(B2 % 4) * 128 + 128], lhsT=lhs, rhs=Pm[wi][:, 128:256],
                                                                                          start=True, stop=False),
                                     reads=[b_Vr[s][r], b_Pm[wi]], writes=[k.b_F[ob2]], inc=True)
                            if B % 4 == 3:
                                if d == 1:
                                    dst = av[:, 0, g * 512:(g + 1) * 512]
                                    srcv = FO[0:65, :]
                                elif d == 4:
                                    dst = av[:, g, 0:512]
                                    srcv = FO[0:65, :]
                                else:
                                    dst = av[:, 4 * g:4 * g + 4, 0:128]
                                    srcv = FO[0:65, :].rearrange("p (a t) -> p a t", a=4)
                                if r == 0:
                                    p.op("dve", lambda e, dst=dst, srcv=srcv: e.tensor_copy(dst, srcv), reads=[k.b_F[ob]], writes=[b_accb])
                                else:
                                    p.op("dve", lambda e, dst=dst, srcv=srcv: e.tensor_tensor(out=dst, in0=dst, in1=srcv, op=ALU.add), reads=[k.b_F[ob], b_accb], writes=[b_accb])
                            bg = step(bg)
                    assert not pendq
                for n in range(4):
                    finalize(h, n, "A", accb=accb, b_accb=b_accb)
                    bg = step(bg)
            return bg

        def attn_B(pi, bg):
            s = pi % NSET
            qT, kT = qkvT[s][0], qkvT[s][1]
            bq, bk = b_qkvT[s][0], b_qkvT[s][1]
            V0, bV0 = Vr[s][0], b_Vr[s][0]
            tiles = []
            for hh in range(2):
                for m in range(4):
                    for j in range(4 * m + 3, -1, -1):
                        tiles.append((hh, m, j))
            N = len(tiles)
            info = {}

            def PEz(t):
                hh, m, j = tiles[t]
                r0, r1 = hh * 64, hh * 64 + 64
                zb = 2 + (t % 4)
                diag = j >= 4 * m
                p.op("pe", lambda e: e.matmul(k.F[zb][:, :], lhsT=kT[r0:r1, j * 128:(j + 1) * 128], rhs=qT[r0:r1, m * 512:(m + 1) * 512], start=True, stop=False),
                     reads=[bq, bk], writes=[k.b_F[zb]], inc=not diag)
                if diag:
                    p.op("pe", lambda e: e.matmul(k.F[zb][:, :], lhsT=cs["c_identb"][:, :], rhs=cs["c_neg"][:, j - 4 * m, :], start=False, stop=False),
                         reads=[k.b_c], writes=[k.b_F[zb]])

            def ACTe(t):
                zb = 2 + (t % 4)
                ei = t % 2
                p.op("act", lambda e: e.activation(out=e32[ei][:, :], in_=k.F[zb][:, :], func=AF.Exp), reads=[k.b_F[zb]], writes=[b_e32[ei]])

            def ACTsp(t):
                ei = t % 2
                si = t % 3
                p.op("act", lambda e: e.activation(out=spt[si][:, :], in_=e32[ei][:, :], func=AF.Ln, bias=1.0), reads=[b_e32[ei]], writes=[b_spt[si]])

            def SSupd(t):
                hh, m, j = tiles[t]
                if j == 0:
                    return
                first = (j == 4 * m + 3)
                gidx = (hh * 4 + m) % 2
                si = t % 3
                if first:
                    p.op("dve", lambda e: e.tensor_copy(ss32[gidx][:, :], spt[si][:, :]), reads=[b_spt[si]], writes=[b_ss32[gidx]])
                else:
                    p.op("dve", lambda e: e.tensor_tensor(out=ss32[gidx][:, :], in0=ss32[gidx][:, :], in1=spt[si][:, :], op=ALU.add),
                         reads=[b_spt[si], b_ss32[gidx]], writes=[b_ss32[gidx]])
                p.op("dve", lambda e: e.tensor_copy(ssb[si][:, :], ss32[gidx][:, :]), reads=[b_ss32[gidx]], writes=[b_ssb[si]])

            def PEB(t):
                hh, m, j = tiles[t]
                bb = 2 + (t % 4)
                si = t % 3
                first = (j == 4 * m + 3)
                FB = k.F[bb]
                p.op("pe", lambda e: e.matmul(FB[:, :], lhsT=cs["c_negtri"][:, :], rhs=spt[si][:, :], start=False, stop=first),
                     reads=[k.b_c, b_spt[si]], writes=[k.b_F[bb]], inc=first)
                if not first:
                    sp_prev = (t - 1) % 3
                    p.op("pe", lambda e: e.matmul(FB[:, :], lhsT=cs["c_negones"][:, :], rhs=ssb[sp_prev][:, :], start=False, stop=True),
                         reads=[k.b_c, b_ssb[sp_prev]], writes=[k.b_F[bb]], inc=True)

            def ACTa(t):
                bb = 2 + (t % 4)
                ai = t % 3
                p.op("act", lambda e: e.activation(out=at[ai][:, :], in_=k.F[bb][:, :], func=AF.Exp), reads=[k.b_F[bb]], writes=[b_at[ai]])

            def PEav(t):
                hh, m, j = tiles[t]
                ai = t % 3
                first = (j == 4 * m + 3)
                p.op("pe", lambda e: e.matmul(k.F[6][0:64, :], lhsT=V0[:, j, hh, 0:64], rhs=at[ai][:, :], start=first, stop=(j == 0)),
                     reads=[bV0, b_at[ai]], writes=[k.b_F[6]], inc=True)
                if j == 0:
                    finalize(16 + 2 * (pi - 8) + hh, m, "B", Fsrc=k.F[6], b_Fsrc=k.b_F[6])

            for t in range(-1, N + 1):
                if t + 1 < N:
                    PEz(t + 1)
                    ACTe(t + 1)
                if 0 <= t < N:
                    PEB(t)
                if t + 1 < N:
                    ACTsp(t + 1)
                    SSupd(t + 1)
                if 0 <= t < N:
                    ACTa(t)
                if t >= 1:
                    PEav(t - 1)
                    bg = step(bg)
            return bg

        import os
        lim = int(os.environ.get("ATTN_LIMIT", "99"))
        bg = bg_pair(0)
        run_all(bg)
        for pi in range(16):
            if lim == 0 or (lim == 1 and pi >= 1) or (lim == 2 and pi != 8):
                continue
            if lim == 2:
                run_all(bg_pair(8))
            nxt = bg_pair(pi + 1) if pi + 1 < 16 else None
            if pi < 8:
                nxt = attn_A(pi, nxt)
            else:
                nxt = attn_B(pi, nxt)
            if nxt is not None:
                run_all(nxt)
        p.end_stage()


def ln_block(k, sbufs, u, b_u, dst, b_dst, lng, lnb, b_ln):
    p = k.p
    st, b_st = sbufs["st"], sbufs["b_st"]
    p.op("dve", lambda e: e.reduce_sum(out=st[:, 0:1], in_=u[:, :], axis=AX.X), reads=[b_u], writes=[b_st])
    p.op("dve", lambda e: e.memset(st[:, 1:2], 0.0), writes=[b_st])
    p.op("act", lambda e: e.activation(out=dst[:, :], in_=u[:, :], func=AF.Square, accum_out=st[:, 1:2]), reads=[b_u], writes=[b_dst, b_st])
    p.op("dve", lambda e: e.tensor_scalar(out=st[:, 2:3], in0=st[:, 0:1], scalar1=1.0 / D, scalar2=None, op0=ALU.mult), reads=[b_st], writes=[b_st])
    p.op("dve", lambda e: e.tensor_tensor(out=st[:, 3:4], in0=st[:, 2:3], in1=st[:, 2:3], op=ALU.mult), reads=[b_st], writes=[b_st])
    p.op("dve", lambda e: e.scalar_tensor_tensor(out=st[:, 4:5], in0=st[:, 1:2], scalar=1.0 / D, in1=st[:, 3:4], op0=ALU.mult, op1=ALU.subtract),
         reads=[b_st], writes=[b_st])
    p.op("dve", lambda e: e.tensor_scalar(out=st[:, 4:5], in0=st[:, 4:5], scalar1=1e-5, scalar2=None, op0=ALU.add), reads=[b_st], writes=[b_st])
    p.op("act", lambda e: e.activation(out=st[:, 5:6], in_=st[:, 4:5], func=AF.Sqrt), reads=[b_st], writes=[b_st])
    p.op("dve", lambda e: e.reciprocal(out=st[:, 6:7], in_=st[:, 5:6]), reads=[b_st], writes=[b_st])
    p.op("dve", lambda e: e.tensor_scalar(out=dst[:, :], in0=u[:, :], scalar1=st[:, 2:3], scalar2=st[:, 6:7], op0=ALU.subtract, op1=ALU.mult),
         reads=[b_u, b_st], writes=[b_dst])
    p.op("dve", lambda e: e.tensor_tensor(out=dst[:, :], in0=dst[:, :], in1=lng[:, :], op=ALU.mult), reads=[b_dst, b_ln], writes=[b_dst])
    p.op("dve", lambda e: e.tensor_tensor(out=dst[:, :], in0=dst[:, :], in1=lnb[:, :], op=ALU.add), reads=[b_dst, b_ln], writes=[b_dst])


def stage_oproj(k, l, xcur):
    p = k.p
    nc = k.nc
    cs = k.cs
    with ExitStack() as es:
        sb = lambda name, shape, d: es.enter_context(nc.sbuf_tensor("L%d_" % l + name, shape, d))
        wout = k.xT
        b_wout = k.b_xT
        gm = sb("o_gm", [128, 16], F32)
        lng = sb("o_lng", [128, D], F32)
        lnb = sb("o_lnb", [128, D], F32)
        wr = sb("o_wr", [128, 16, NE], F32)
        brt = sb("o_brt", [128, NE], F32)
        b_par = p.buf("o_par")
        mixt = [sb("o_mixt%d" % i, [128, 16, 128], BF16) for i in range(2)]
        b_mixt = [p.buf("o_mixt%d" % i) for i in range(2)]
        xr = [sb("o_xr%d" % i, [128, D], F32) for i in range(2)]
        b_xr = [p.buf("o_xr%d" % i) for i in range(2)]
        u = sb("o_u", [128, D], F32)
        b_u = p.buf("o_u")
        x1f = [sb("o_x1f%d" % i, [128, D], F32) for i in range(2)]
        b_x1f = [p.buf("o_x1f%d" % i) for i in range(2)]
        x1b = [sb("o_x1b%d" % i, [128, D], BF16) for i in range(2)]
        b_x1b = [p.buf("o_x1b%d" % i) for i in range(2)]
        x1T = sb("o_x1T", [128, 16, 128], F32)
        b_x1T = p.buf("o_x1T")
        selb = sb("o_selb", [128, 16, NE], BF16)
        b_selb = p.buf("o_selb")
        st = sb("o_st", [128, 8], F32)
        rr = sb("o_rr", [128, 4], F32)
        b_rr = p.buf("o_rr")
        sm = {n: sb("o_" + n, [128, NE], F32) for n in ["lg", "sel", "ex", "G", "oh", "tmp", "pos"]}
        b_sm = p.buf("o_sm")
        mx8 = sb("o_mx8", [128, 8], F32)
        pk = sb("o_pk", [128, 8], F32)
        b_x1res = p.buf("x1res")
        b_XS = p.buf("XS")
        b_mixT = p.buf("mixT_r")
        lnsb = {"st": st, "b_st": p.buf("o_st")}
        print("oproj stage: sbuf bytes remaining:", nc.sbuf_bytes_remaining)

        p.dma("sp", lambda e: e.dma_start(out=lng[:, :], in_=k.ln1_g[l:l + 1, :].broadcast_to([128, D])), writes=[b_par])
        p.dma("sp", lambda e: e.dma_start(out=lnb[:, :], in_=k.ln1_b[l:l + 1, :].broadcast_to([128, D])), writes=[b_par])
        p.dma("sp", lambda e: e.dma_start(out=wr[:, :, :], in_=k.w_router[l].rearrange("(c p) n -> p c n", p=128)), writes=[b_par])
        p.dma("sp", lambda e: e.dma_start(out=brt[:, :], in_=k.b_router[l:l + 1, :].broadcast_to([128, NE])), writes=[b_par])
        for c in range(16):
            p.dma("pool", lambda e, c=c: e.dma_start(out=wout[:, c, :], in_=k.w_out[l, c * 128:(c + 1) * 128, :]), writes=[b_wout])

        ssv = k.ss[:, :].rearrange("p (a b) -> p a b", a=2)
        for tb in range(16):
            i2 = tb % 2
            t0, t1 = tb * 128, (tb + 1) * 128
            p.dma("sp", lambda e: e.dma_start(out=mixt[i2][:, :, :], in_=k.mixT[:, :, t0:t1].rearrange("c p t -> p c t")), reads=[b_mixT], writes=[b_mixt[i2]])
            p.dma("sp", lambda e: e.dma_start(out=xr[i2][:, :], in_=xcur[t0:t1, :]), writes=[b_xr[i2]])
            p.op("dve", lambda e: e.tensor_scalar(out=rr[:, 0:2], in0=ssv[:, :, tb], scalar1=1.0 / WA, scalar2=1e-6, op0=ALU.mult, op1=ALU.add),
                 reads=[k.b_ss], writes=[b_rr])
            p.op("act", lambda e: e.activation(out=rr[:, 0:2], in_=rr[:, 0:2], func=AF.Sqrt), writes=[b_rr])
            p.op("dve", lambda e: e.reciprocal(out=rr[:, 2:4], in_=rr[:, 0:2]), writes=[b_rr])
            p.op("act", lambda e: e.activation(out=u[:, :], in_=xr[i2][:, :], func=AF.Copy, scale=ALPHA), reads=[b_xr[i2]], writes=[b_u])
            for part in range(2):
                for nb in range(4):
                    for c in range(8):
                        cc = 8 * part + c
                        p.op("pe", lambda e, nb=nb, cc=cc, c=c: e.matmul(k.F[nb][:, :], lhsT=mixt[i2][:, cc, :], rhs=wout[:, cc, nb * 512:(nb + 1) * 512],
                                                                      start=(c == 0), stop=(c == 7)),
                             reads=[b_mixt[i2], b_wout], writes=[k.b_F[nb]], inc=(c == 7))
                for nb in range(4):
                    p.op("dve", lambda e, nb=nb, part=part: e.scalar_tensor_tensor(out=u[:, nb * 512:(nb + 1) * 512], in0=k.F[nb][:, :], scalar=rr[:, 2 + part:3 + part],
                                                                                 in1=u[:, nb * 512:(nb + 1) * 512], op0=ALU.mult, op1=ALU.add),
                         reads=[k.b_F[nb], b_rr], writes=[b_u])
            xf, b_xf = x1f[i2], b_x1f[i2]
            ln_block(k, lnsb, u, b_u, xf, b_xf, lng, lnb, b_par)
            p.op("act", lambda e: e.activation(out=x1b[i2][:, :], in_=xf[:, :], func=AF.Copy), reads=[b_xf], writes=[b_x1b[i2]])
            p.dma("sp", lambda e: e.dma_start(out=k.x1res[t0:t1, :], in_=xf[:, :]), reads=[b_xf], writes=[b_x1res])
            transpose_block(k, xf, b_xf, tb, xT_dst=False, f32_dst=x1T, b_f32=b_x1T, fbanks=(4, 5))
            for c in range(16):
                p.op("pe", lambda e, c=c: e.matmul(k.F[6][:, 0:NE], lhsT=x1T[:, c, :], rhs=wr[:, c, :], start=(c == 0), stop=(c == 15)),
                     reads=[b_x1T, b_par], writes=[k.b_F[6]], inc=(c == 15))
            lg, sel, ex, G, oh, tmp, pos = [sm[n] for n in ["lg", "sel", "ex", "G", "oh", "tmp", "pos"]]
            W = [b_sm]
            p.op("dve", lambda e: e.tensor_tensor(out=lg[:, :], in0=k.F[6][:, 0:NE], in1=brt[:, :], op=ALU.add), reads=[k.b_F[6], b_par], writes=W)
            p.op("dve", lambda e: e.max(out=mx8[:, :], in_=lg[:, :]), writes=W)
            p.op("dve", lambda e: e.tensor_scalar(out=sel[:, :], in0=lg[:, :], scalar1=mx8[:, 3:4], scalar2=None, op0=ALU.is_ge), writes=W)
            p.op("dve", lambda e: e.tensor_copy(selb[:, tb, :], sel[:, :]), reads=W, writes=[b_selb])
            p.op("dve", lambda e: e.tensor_scalar(out=pk[:, 4:5], in0=mx8[:, 0:1], scalar1=-1.0, scalar2=None, op0=ALU.mult), writes=W)
            p.op("act", lambda e: e.activation(out=ex[:, :], in_=lg[:, :], func=AF.Exp, bias=pk[:, 4:5]), writes=W)
            p.op("dve", lambda e: e.tensor_tensor(out=ex[:, :], in0=ex[:, :], in1=sel[:, :], op=ALU.mult), writes=W)
            p.op("dve", lambda e: e.reduce_sum(out=pk[:, 5:6], in_=ex[:, :], axis=AX.X), writes=W)
            p.op("dve", lambda e: e.reciprocal(out=pk[:, 6:7], in_=pk[:, 5:6]), writes=W)
            p.op("dve", lambda e: e.tensor_scalar(out=G[:, :], in0=ex[:, :], scalar1=pk[:, 6:7], scalar2=None, op0=ALU.mult), writes=W)
            for b2 in range(tb + 1):
                lhs = cs["c_ones"] if b2 < tb else cs["c_tstrict"]
                p.op("pe", lambda e, b2=b2, lhs=lhs: e.matmul(k.PS[:, 0:NE], lhsT=lhs[:, :], rhs=selb[:, b2, :], start=(b2 == 0), stop=(b2 == tb)),
                     reads=[b_selb, k.b_c], writes=[k.b_PS], inc=(b2 == tb))
            p.op("dve", lambda e: e.tensor_scalar(out=tmp[:, :], in0=k.PS[:, 0:NE], scalar1=float(CAP), scalar2=1.0e6, op0=ALU.is_ge, op1=ALU.mult),
                 reads=[k.b_PS], writes=W)
            p.op("dve", lambda e: e.tensor_tensor(out=pos[:, :], in0=k.PS[:, 0:NE], in1=cs["c_ecap"][:, :], op=ALU.add), reads=[k.b_PS, k.b_c], writes=W)
            p.op("dve", lambda e: e.tensor_tensor(out=pos[:, :], in0=pos[:, :], in1=tmp[:, :], op=ALU.add), writes=W)
            for kk in range(4):
                p.op("dve", lambda e, kk=kk: e.tensor_scalar(out=oh[:, :], in0=lg[:, :], scalar1=mx8[:, kk:kk + 1], scalar2=None, op0=ALU.is_equal), writes=W)
                p.op("dve", lambda e: e.tensor_tensor(out=tmp[:, :], in0=oh[:, :], in1=G[:, :], op=ALU.mult), writes=W)
                p.op("dve", lambda e, kk=kk: e.reduce_sum(out=k.gk[:, tb, kk:kk + 1], in_=tmp[:, :], axis=AX.X), reads=W, writes=[k.b_route])
                p.op("dve", lambda e: e.tensor_tensor(out=tmp[:, :], in0=oh[:, :], in1=pos[:, :], op=ALU.mult), writes=W)
                p.op("dve", lambda e, kk=kk: e.reduce_sum(out=pk[:, kk:kk + 1], in_=tmp[:, :], axis=AX.X), writes=W)
            p.op("dve", lambda e: e.tensor_copy(k.idx[:, tb, :], pk[:, 0:4]), reads=W, writes=[k.b_route])
            for kk in range(4):
                p.dma("pool", lambda e, kk=kk: e.indirect_dma_start(out=k.XS[:, :], out_offset=bass.IndirectOffsetOnAxis(ap=k.idx[:, tb, kk:kk + 1], axis=0),
                                                                   in_=x1b[i2][:, :], in_offset=None, bounds_check=k.bc_reg, oob_is_err=False),
                      reads=[b_x1b[i2], k.b_route], writes=[b_XS])
        p.end_stage()


def stage_moe(k, l):
    p = k.p
    nc = k.nc
    cs = k.cs
    NS = CAP
    with ExitStack() as es:
        sb = lambda name, shape, d: es.enter_context(nc.sbuf_tensor("L%d_" % l + name, shape, d))
        NW1 = 4
        w1 = [sb("m_w1_%d" % i, [128, 16, 512], BF16) for i in range(NW1)]
        b_w1 = [p.buf("m_w1_%d" % i) for i in range(NW1)]
        w2 = [k.xT[:, 0:8, :], k.xT[:, 8:16, :]]
        b_w2 = [p.buf("m_w2_0"), p.buf("m_w2_1")]
        xs = sb("m_xs", [128, 3, D], BF16)
        b_xs = p.buf("m_xs")
        xeT = sb("m_xeT", [128, 16, NS], BF16)
        b_xeT = p.buf("m_xeT")
        hT = [sb("m_hT%d" % i, [128, 8, NS], BF16) for i in range(2)]
        b_hT = [p.buf("m_hT%d" % i) for i in range(2)]
        NT = 2
        gc = [sb("m_gc%d" % i, [128, NS], F32) for i in range(NT)]
        sg = [sb("m_sg%d" % i, [128, NS], F32) for i in range(NT)]
        uc = [sb("m_uc%d" % i, [128, NS], F32) for i in range(NT)]
        b_tmp = [p.buf("m_tmp%d" % i) for i in range(NT)]
        NY = 4
        yo = [sb("m_yo%d" % i, [128, 512], F32) for i in range(NY)]
        b_yo = [p.buf("m_yo%d" % i) for i in range(NY)]
        b1 = sb("m_b1", [128, NE, 16], F32)
        b_b1 = p.buf("m_b1")
        b2 = [sb("m_b2_%d" % i, [128, D], F32) for i in range(2)]
        b_b2 = [p.buf("m_b2_%d" % i) for i in range(2)]
        b_XS = p.buf("XS_r")
        b_YS = p.buf("YS")
        print("moe stage: sbuf bytes remaining:", nc.sbuf_bytes_remaining)

        p.dma("sp", lambda e: e.dma_start(out=b1[:, :, :], in_=k.b_gu[l, :, :, :]), writes=[b_b1])

        def load_slab(g_):
            wi = g_ % NW1
            for hc in range(2):
                p.dma("sp", lambda e, wi=wi, hc=hc, g_=g_: e.dma_start(out=w1[wi][:, 8 * hc:8 * hc + 8, :], in_=k.WB1[g_, :, 8 * hc:8 * hc + 8, :]),
                      reads=[k.b_WB], writes=[b_w1[wi]])

        def load_w2(e_):
            wj = e_ % 2
            for hf in range(2):
                src = k.w_dn[l, e_].rearrange("(c p) n -> p c n", p=128)[:, 4 * hf:4 * hf + 4, :]
                p.dma("pool", lambda e, wj=wj, hf=hf, src=src: e.dma_start(out=w2[wj][:, 4 * hf:4 * hf + 4, :], in_=src), writes=[b_w2[wj]])

        def load_acts(e_):
            p.dma("sp", lambda e: e.dma_start(out=xs[:, :, :], in_=k.XS[e_ * CAP:(e_ + 1) * CAP, :].rearrange("(j p) f -> p j f", p=128)),
                  reads=[b_XS], writes=[b_xs])

        def load_b2(e_):
            p.dma("sp", lambda e: e.dma_start(out=b2[e_ % 2][:, :], in_=k.b_dn[l, e_:e_ + 1, :].broadcast_to([128, D])), writes=[b_b2[e_ % 2]])

        st = {"ev": 0, "yo": 0, "tmp": 0, "dn": 0}
        st["slab"] = 0

        def ensure_slabs(upto):
            while st["slab"] <= min(upto, 4 * NE - 1):
                load_slab(st["slab"])
                st["slab"] += 1

        ensure_slabs(3)
        load_w2(0)
        load_acts(0)
        load_b2(0)
        load_b2(1)
        PTB = k.F[6][:, 0:256].bitcast(BF16)

        def emit_transposes(e_):
            for j in range(3):
                for cg in range(4):
                    if st["ev"] % 2 == 0:
                        TB, bTB = k.PT, k.b_PT
                    else:
                        TB, bTB = PTB, k.b_F[6]
                    for i in range(4):
                        c = 4 * cg + i
                        p.op("pe", lambda e, j=j, c=c, i=i, TB=TB: e.transpose(TB[:, i * 128:(i + 1) * 128], xs[:, j, c * 128:(c + 1) * 128], cs["c_identb"][:, :]),
                             reads=[b_xs, k.b_c], writes=[bTB], inc=(i == 3))
                    src = TB[:, :].rearrange("p (a t) -> p a t", a=4)
                    dst = xeT[:, 4 * cg:4 * cg + 4, j * 128:(j + 1) * 128]
                    if st["ev"] % 2 == 0:
                        p.op("act", lambda e, src=src, dst=dst: e.activation(out=dst, in_=src, func=AF.Copy), reads=[bTB], writes=[b_xeT])
                    else:
                        p.op("dve", lambda e, src=src, dst=dst: e.tensor_copy(dst, src), reads=[bTB], writes=[b_xeT])
                    st["ev"] += 1
            if e_ + 1 < NE:
                load_acts(e_ + 1)

        emit_transposes(0)
        for e_ in range(NE):
            hb = e_ % 2
            for jj in range(8):
                half = jj // 4
                if jj == 0:
                    ensure_slabs(4 * e_ + 3)
                    if e_ + 1 < NE:
                        load_w2(e_ + 1)
                elif jj == 4:
                    ensure_slabs(4 * e_ + 5)
                co = (jj % 4) * 128
                Gb, Ub = jj % 2, 2 + jj % 2
                for gu, bank in ((0, Gb), (1, Ub)):
                    wi = (4 * e_ + 2 * half + gu) % NW1
                    for c in range(16):
                        p.op("pe", lambda e, c=c, wi=wi, bank=bank: e.matmul(k.F[bank][:, 0:NS], lhsT=w1[wi][:, c, co:co + 128], rhs=xeT[:, c, :],
                                                                         start=(c == 0), stop=(c == 15)),
                             reads=[b_w1[wi], b_xeT], writes=[k.b_F[bank]], inc=(c == 15))
                ti = st["tmp"] % NT
                st["tmp"] += 1
                T = [b_tmp[ti]]
                p.op("dve", lambda e: e.tensor_scalar(out=gc[ti][:, :], in0=k.F[Gb][:, 0:NS], scalar1=b1[:, e_, jj:jj + 1], scalar2=7.0, op0=ALU.add, op1=ALU.min),
                     reads=[k.b_F[Gb], b_b1], writes=T)
                p.op("act", lambda e: e.activation(out=sg[ti][:, :], in_=gc[ti][:, :], func=AF.Sigmoid, scale=1.702), writes=T)
                p.op("dve", lambda e: e.tensor_scalar(out=uc[ti][:, :], in0=k.F[Ub][:, 0:NS], scalar1=b1[:, e_, 8 + jj:9 + jj], scalar2=7.0, op0=ALU.add, op1=ALU.min),
                     reads=[k.b_F[Ub], b_b1], writes=T)
                p.op("dve", lambda e: e.tensor_scalar(out=uc[ti][:, :], in0=uc[ti][:, :], scalar1=-7.0, scalar2=1.0, op0=ALU.max, op1=ALU.add), writes=T)
                p.op("dve", lambda e: e.tensor_tensor(out=gc[ti][:, :], in0=gc[ti][:, :], in1=sg[ti][:, :], op=ALU.mult), writes=T)
                p.op("dve", lambda e: e.tensor_tensor(out=hT[hb][:, jj, :], in0=gc[ti][:, :], in1=uc[ti][:, :], op=ALU.mult), reads=T, writes=[b_hT[hb]])
            if e_ + 1 < NE:
                emit_transposes(e_ + 1)
            wj = e_ % 2
            for j in range(3):
                for q in range(4):
                    bank = (4, 5, 0, 1, 2, 3)[st["dn"] % 6]
                    st["dn"] += 1
                    for jj in range(8):
                        p.op("pe", lambda e, jj=jj, j=j, q=q, bank=bank: e.matmul(k.F[bank][:, :], lhsT=hT[hb][:, jj, j * 128:(j + 1) * 128], rhs=w2[wj][:, jj, q * 512:(q + 1) * 512],
                                                                               start=(jj == 0), stop=(jj == 7)),
                             reads=[b_hT[hb], b_w2[wj]], writes=[k.b_F[bank]], inc=(jj == 7))
                    yi = st["yo"] % NY
                    st["yo"] += 1
                    p.op("dve", lambda e, q=q, bank=bank, yi=yi: e.tensor_tensor(out=yo[yi][:, :], in0=k.F[bank][:, :], in1=b2[wj][:, q * 512:(q + 1) * 512], op=ALU.add),
                         reads=[k.b_F[bank], b_b2[wj]], writes=[b_yo[yi]])
                    r0 = e_ * CAP + j * 128
                    p.dma("pool", lambda e, q=q, yi=yi, r0=r0: e.dma_start(out=k.YS[r0:r0 + 128, q * 512:(q + 1) * 512], in_=yo[yi][:, :]), reads=[b_yo[yi]], writes=[b_YS])
            if e_ + 2 < NE:
                load_b2(e_ + 2)
        p.end_stage()


def stage_ln2(k, l, dst, last):
    p = k.p
    nc = k.nc
    with ExitStack() as es:
        sb = lambda name, shape, d: es.enter_context(nc.sbuf_tensor("L%d_" % l + name, shape, d))
        yk = [[sb("l_yk%d_%d" % (s, i), [128, D], F32) for i in range(4)] for s in range(2)]
        b_yk = [[p.buf("l_yk%d_%d" % (s, i)) for i in range(4)] for s in range(2)]
        x1t = [sb("l_x1t%d" % i, [128, D], F32) for i in range(2)]
        b_x1t = [p.buf("l_x1t%d" % i) for i in range(2)]
        u = sb("l_u", [128, D], F32)
        b_u = p.buf("l_u")
        x2 = [sb("l_x2_%d" % i, [128, D], F32) for i in range(2)]
        b_x2 = [p.buf("l_x2_%d" % i) for i in range(2)]
        lng = sb("l_lng", [128, D], F32)
        lnb = sb("l_lnb", [128, D], F32)
        b_par = p.buf("l_par")
        st = sb("l_st", [128, 8], F32)
        lnsb = {"st": st, "b_st": p.buf("l_st")}
        b_YS = p.buf("YS_r")
        b_x1res = p.buf("x1res_r")
        b_dst = p.buf("dst")
        print("ln2 stage: sbuf bytes remaining:", nc.sbuf_bytes_remaining)
        p.dma("sp", lambda e: e.dma_start(out=lng[:, :], in_=k.ln2_g[l:l + 1, :].broadcast_to([128, D])), writes=[b_par])
        p.dma("sp", lambda e: e.dma_start(out=lnb[:, :], in_=k.ln2_b[l:l + 1, :].broadcast_to([128, D])), writes=[b_par])
        for tb in range(16):
            s2 = tb % 2
            t0, t1 = tb * 128, (tb + 1) * 128
            for kk in range(4):
                p.dma("pool", lambda e, kk=kk: e.indirect_dma_start(out=yk[s2][kk][:, :], out_offset=None, in_=k.YS[:, :],
                                                                   in_offset=bass.IndirectOffsetOnAxis(ap=k.idx[:, tb, kk:kk + 1], axis=0),
                                                                   bounds_check=k.bc_reg, oob_is_err=False),
                      reads=[b_YS, k.b_route], writes=[b_yk[s2][kk]])
            p.dma("sp", lambda e: e.dma_start(out=x1t[s2][:, :], in_=k.x1res[t0:t1, :]), reads=[b_x1res], writes=[b_x1t[s2]])
            p.op("act", lambda e: e.activation(out=u[:, :], in_=x1t[s2][:, :], func=AF.Copy, scale=ALPHA), reads=[b_x1t[s2]], writes=[b_u])
            for kk in range(4):
                p.op("dve", lambda e, kk=kk: e.scalar_tensor_tensor(out=u[:, :], in0=yk[s2][kk][:, :], scalar=k.gk[:, tb, kk:kk + 1], in1=u[:, :], op0=ALU.mult, op1=ALU.add),
                     reads=[b_yk[s2][kk], k.b_route], writes=[b_u])
            xo, b_xo = x2[s2], b_x2[s2]
            ln_block(k, lnsb, u, b_u, xo, b_xo, lng, lnb, b_par)
            p.dma("sp", lambda e: e.dma_start(out=dst[t0:t1, :], in_=xo[:, :]), reads=[b_xo], writes=[b_dst])
            if not last:
                transpose_block(k, xo, b_xo, tb)
        p.end_stage()


_NC_CACHE = {}


def _host_layout(inputs):
    f32 = lambda a: np.ascontiguousarray(np.asarray(a, dtype=np.float32))
    m = {}
    m["w_in"] = f32(inputs["w_in"])
    m["w_out"] = f32(inputs["w_out"])
    gm = np.concatenate([np.asarray(inputs["g_mix_a"], np.float32), np.asarray(inputs["g_mix_b"], np.float32)], axis=1)
    m["gmix"] = np.ascontiguousarray(gm.reshape(NL, 32, 64).transpose(0, 2, 1))
    for n in ["ln1_g", "ln1_b", "ln2_g", "ln2_b", "w_router", "b_router", "b_down", "w_gate_up", "w_down"]:
        m[n] = f32(inputs[n])
    m["b_gu"] = np.ascontiguousarray(np.asarray(inputs["b_gate_up"], np.float32).reshape(NL, NE, 16, 128).transpose(0, 3, 1, 2))
    m.update(make_consts())
    return m


def kernel(**inputs):
    x = np.asarray(inputs["x"], dtype=np.float32)
    nb = x.shape[0]
    shared = _host_layout(inputs)
    if "nc" not in _NC_CACHE:
        _NC_CACHE["nc"] = build(n_layers=NL)
    nc = _NC_CACHE["nc"]
    in_maps = []
    for b in range(nb):
        m = dict(shared)
        m["x"] = np.ascontiguousarray(x[b])
        in_maps.append(m)
    res = run_bass_kernel_spmd(nc, in_maps, core_ids=list(range(nb)))
    out = np.stack([np.asarray(r["out"], dtype=np.float32) for r in res.results], axis=0)
    return out
```
